# Optimizing a Trainium2 kernel written in Bass

```python
import math
import jax, jax.numpy as jnp
from jax import lax
import numpy as np

D_MODEL = 1024
BATCH = 8
SEQ = 4096
DEPTH = 2

N_HEADS = 16
HEAD_DIM = D_MODEL // N_HEADS
Q_BLOCK = 128
POOL_WINDOWS = (2, 4, 8, 16)
N_POOL_GROUPS = len(POOL_WINDOWS)
POOL_CH = D_MODEL // N_POOL_GROUPS
N_EXPERTS = 32
N_EXPERT_GROUPS = 4
EXPERTS_PER_GROUP = N_EXPERTS // N_EXPERT_GROUPS
TOPK_GROUPS = 1
TOP_K = 2
D_FF_EXPERT = D_MODEL // 2
EXPERT_BLOCK = 256
N_MIXERS = 2
N_ATTN = (DEPTH + 1) // 2
N_POOL = DEPTH // 2
RMS_EPS = 1e-6

kernel_name = "fox_pool_interleaved_grouped_moe_adaln"


def rmsnorm(x, g):
    xf = x.astype(jnp.float32)
    xf = xf * lax.rsqrt(jnp.mean(xf * xf, axis=-1, keepdims=True) + RMS_EPS)
    return (xf * g.astype(jnp.float32)).astype(x.dtype)


def modulate(h, shift, scale):
    return h * (1.0 + scale[:, None, :]) + shift[:, None, :]


def fox_attention(h, w_in, f_bias, w_o):
    B, S, D = h.shape
    proj = h @ w_in
    q, k, v, f = jnp.split(proj, [D, 2 * D, 3 * D], axis=-1)
    to_heads = lambda t: t.reshape(B, S, N_HEADS, HEAD_DIM).transpose(0, 2, 1, 3)
    q, k, v = to_heads(q), to_heads(k), to_heads(v)
    log_f = jax.nn.log_sigmoid(f.astype(jnp.float32) + f_bias.astype(jnp.float32))
    F = jnp.cumsum(log_f, axis=1).transpose(0, 2, 1)
    scale = 1.0 / math.sqrt(HEAD_DIM)
    kpos = jnp.arange(S)
    n_blocks = S // Q_BLOCK

    def q_block(i):
        start = i * Q_BLOCK
        qb = lax.dynamic_slice_in_dim(q, start, Q_BLOCK, axis=2)
        Fq = lax.dynamic_slice_in_dim(F, start, Q_BLOCK, axis=2)
        logits = jnp.einsum('bhqd,bhkd->bhqk', qb, k).astype(jnp.float32) * scale
        logits = logits + Fq[..., :, None] - F[:, :, None, :]
        qpos = start + jnp.arange(Q_BLOCK)
        causal = kpos[None, :] <= qpos[:, None]
        logits = jnp.where(causal[None, None], logits, -jnp.inf)
        p = jax.nn.softmax(logits, axis=-1)
        return jnp.einsum('bhqk,bhkd->bhqd', p.astype(v.dtype), v)

    o = lax.map(q_block, jnp.arange(n_blocks))
    o = o.transpose(1, 0, 3, 2, 4).reshape(B, S, D)
    return (o @ w_o).astype(h.dtype)


def pool_mixer(h, w_pool, ch_scale):
    B, S, D = h.shape
    hf = h.astype(jnp.float32)
    cs = jnp.cumsum(hf, axis=1)
    t = jnp.arange(S)
    outs = []
    for g, win in enumerate(POOL_WINDOWS):
        sl = slice(g * POOL_CH, (g + 1) * POOL_CH)
        csg = cs[..., sl]
        shifted = jnp.pad(csg, ((0, 0), (win, 0), (0, 0)))[:, :S]
        count = jnp.minimum(t + 1, win).astype(jnp.float32)[None, :, None]
        outs.append((csg - shifted) / count - hf[..., sl])
    pooled = jnp.stack(outs, axis=2).astype(h.dtype)
    y = jnp.einsum('bsgc,gce->bsge', pooled, w_pool).reshape(B, S, D)
    return (y * ch_scale).astype(h.dtype)


def grouped_moe(h, router_w, router_bias, w1, w3, w2):
    B, S, D = h.shape
    T = B * S
    hf = h.reshape(T, D)
    scores = jax.nn.sigmoid((hf @ router_w).astype(jnp.float32))
    sel = scores + router_bias.astype(jnp.float32)
    sel_g = sel.reshape(T, N_EXPERT_GROUPS, EXPERTS_PER_GROUP)
    group_score = lax.top_k(sel_g, TOP_K)[0].sum(-1)
    _, g_idx = lax.top_k(group_score, TOPK_GROUPS)
    g_mask = jnp.any(g_idx[..., None] == jnp.arange(N_EXPERT_GROUPS), axis=1)
    e_mask = jnp.repeat(g_mask, EXPERTS_PER_GROUP, axis=1)
    _, e_idx = lax.top_k(jnp.where(e_mask, sel, -jnp.inf), TOP_K)
    gate = jnp.take_along_axis(scores, e_idx, axis=1)
    gate = gate / jnp.sum(gate, axis=-1, keepdims=True)

    A = T * TOP_K
    e_flat = e_idx.reshape(A)
    tok_flat = jnp.repeat(jnp.arange(T, dtype=jnp.int32), TOP_K)
    w_flat = gate.reshape(A)
    order = jnp.argsort(e_flat)
    e_sorted = e_flat[order]
    counts = jnp.bincount(e_flat, length=N_EXPERTS)
    padded = ((counts + EXPERT_BLOCK - 1) // EXPERT_BLOCK) * EXPERT_BLOCK
    starts = jnp.cumsum(counts) - counts
    pends = jnp.cumsum(padded)
    pstarts = pends - padded
    rank = jnp.arange(A) - starts[e_sorted]
    dest = pstarts[e_sorted] + rank
    n_blocks = A // EXPERT_BLOCK + N_EXPERTS
    P = n_blocks * EXPERT_BLOCK
    row_tok = jnp.full((P,), T, dtype=jnp.int32).at[dest].set(tok_flat[order])
    row_w = jnp.zeros((P,), jnp.float32).at[dest].set(w_flat[order])
    block_e = jnp.minimum(
        jnp.searchsorted(pends, jnp.arange(n_blocks) * EXPERT_BLOCK, side='right'),
        N_EXPERTS - 1)
    x_rows = jnp.concatenate([hf, jnp.zeros((1, D), hf.dtype)], axis=0)[row_tok]
    x_rows = x_rows.reshape(n_blocks, EXPERT_BLOCK, D)

    def expert_block(args):
        xb, e = args
        return (jax.nn.silu(xb @ w1[e]) * (xb @ w3[e])) @ w2[e]

    y = lax.map(expert_block, (x_rows, block_e)).reshape(P, D)
    out = jax.ops.segment_sum(y.astype(jnp.float32) * row_w[:, None], row_tok,
                              num_segments=T + 1)[:T]
    return out.reshape(B, S, D).astype(h.dtype)


def setup_inputs(seed: int = 0) -> dict:
    key = jax.random.key(seed)
    ks = jax.random.split(key, 20)
    D, H, E, Fd = D_MODEL, N_HEADS, N_EXPERTS, D_FF_EXPERT
    nrm = lambda k, shape, s: jax.random.normal(k, shape, jnp.float32) * s
    return {
        "x": nrm(ks[0], (BATCH, SEQ, D), 1.0),
        "c": nrm(ks[1], (BATCH, D), 1.0),
        "norm_mix_g": 1.0 + nrm(ks[2], (DEPTH, D), 0.05),
        "norm_ffn_g": 1.0 + nrm(ks[3], (DEPTH, D), 0.05),
        "ada_w": nrm(ks[4], (DEPTH, D, 6 * D), 0.5 * D ** -0.5),
        "ada_b": nrm(ks[5], (DEPTH, 6 * D), 0.01),
        "attn_w_in": nrm(ks[6], (N_ATTN, D, 3 * D + H), D ** -0.5),
        "attn_f_bias": 2.0 + nrm(ks[7], (N_ATTN, H), 0.5),
        "attn_w_o": nrm(ks[8], (N_ATTN, D, D), D ** -0.5),
        "pool_w": nrm(ks[9], (N_POOL, N_POOL_GROUPS, POOL_CH, POOL_CH), POOL_CH ** -0.5),
        "pool_scale": 0.5 + nrm(ks[10], (N_POOL, D), 0.1),
        "router_w": nrm(ks[11], (D, E), D ** -0.5),
        "router_bias": nrm(ks[12], (E,), 0.01),
        "exp_w1": nrm(ks[13], (DEPTH, E, D, Fd), D ** -0.5),
        "exp_w3": nrm(ks[14], (DEPTH, E, D, Fd), D ** -0.5),
        "exp_w2": nrm(ks[15], (DEPTH, E, Fd, D), Fd ** -0.5),
        "norm_final_g": 1.0 + nrm(ks[16], (D,), 0.05),
    }


def reference(x, c, norm_mix_g, norm_ffn_g, ada_w, ada_b, attn_w_in, attn_f_bias,
              attn_w_o, pool_w, pool_scale, router_w, router_bias, exp_w1, exp_w3,
              exp_w2, norm_final_g):
    c_act = jax.nn.silu(c)
    for i in range(DEPTH):
        mod = c_act @ ada_w[i] + ada_b[i]
        sh_m, sc_m, gt_m, sh_f, sc_f, gt_f = jnp.split(mod, 6, axis=-1)
        h = modulate(rmsnorm(x, norm_mix_g[i]), sh_m, sc_m)
        if i % N_MIXERS == 0:
            j = i // N_MIXERS
            y = fox_attention(h, attn_w_in[j], attn_f_bias[j], attn_w_o[j])
        else:
            j = i // N_MIXERS
            y = pool_mixer(h, pool_w[j], pool_scale[j])
        x = x + gt_m[:, None, :] * y
        h = modulate(rmsnorm(x, norm_ffn_g[i]), sh_f, sc_f)
        y = grouped_moe(h, router_w, router_bias, exp_w1[i], exp_w3[i], exp_w2[i])
        x = x + gt_f[:, None, :] * y
    return rmsnorm(x, norm_final_g)
```

```python
import numpy as np
from contextlib import ExitStack
import concourse.bass as bass
import concourse.mybir as mybir
from concourse.bass_utils import run_bass_kernel_spmd

F32 = mybir.dt.float32
BF16 = mybir.dt.bfloat16
I32 = mybir.dt.int32
ALU = mybir.AluOpType
AF = mybir.ActivationFunctionType
AX = mybir.AxisListType

S = 4096
D = 1024
NT = S // 128
NH = 16
E = 32
FF = 512
BLK = 512
NBLK = 2 * S // BLK + E
NS = BLK // 128
EPS = 1e-6
MASKV = -30000.0
DBG = {}


class SemC:
    def __init__(self, h):
        self.h = h
        self.n = 0


class Phase:
    ENGS = ("pe", "act", "dve", "pool", "sp")

    def __init__(self, nc, name):
        self.nc = nc
        self.name = name
        self.es = ExitStack()
        self.q = {e: [] for e in self.ENGS}
        self.dsems = []
        self.allsems = []
        self.last = {}

    def __enter__(self):
        self.es.__enter__()
        self.esem = {e: self.sem("e_" + e) for e in self.ENGS[:4]}
        return self

    def sem(self, nm):
        sc = SemC(self.es.enter_context(self.nc.semaphore(f"{self.name}_{nm}")))
        self.allsems.append(sc)
        return sc

    def dsem(self, nm):
        s = self.sem(nm)
        self.dsems.append(s)
        return s

    def sb(self, nm, shape, dt):
        return self.es.enter_context(self.nc.sbuf_tensor(f"{self.name}_{nm}", list(shape), dt))

    def ps(self, nm, shape, dt=F32):
        return self.es.enter_context(self.nc.psum_tensor(f"{self.name}_{nm}", list(shape), dt))

    def op(self, eng, fn, waits=(), sig=None, inc=1, chain=True):
        is_compute = eng in ("act", "dve", "pool") and inc == 1
        if is_compute:
            sig = self.esem[eng]
            if chain and self.last.get(eng) is not None:
                waits = list(waits) + [self.last[eng]]
        if sig is True:
            sig = self.esem[eng]
        ev = None
        if sig is not None:
            sig.n += inc
            ev = (sig, sig.n)
        self.q[eng].append((fn, self._collapse(waits), sig, inc))
        if is_compute:
            self.last[eng] = ev
        return ev

    @staticmethod
    def _collapse(waits):
        best = {}
        for w in waits:
            if w is None:
                continue
            s_, v = w
            if id(s_) not in best or best[id(s_)][1] < v:
                best[id(s_)] = (s_, v)
        return list(best.values())

    def dma(self, eng, out, in_, waits=(), sem=None, **kw):
        return self.op(eng, lambda e: e.dma_start(out=out, in_=in_, **kw), waits, sig=sem, inc=16)

    def dma1(self, eng, out, in_, waits=(), **kw):
        self._n1 = getattr(self, "_n1", 0) + 1
        return self.dma(eng, out, in_, waits, sem=self.dsem(f"u{self._n1}"), **kw)

    def wait(self, eng, waits):
        self.q[eng].append((None, self._collapse(waits), None, 0))

    def __exit__(self, *a):
        if a[0] is None:
            self.wait("sp", [(s, s.n) for s in self.dsems if s.n > 0])
            self.run()
            with self.nc.Block() as blk2:
                def clr(e):
                    e.dma_reset()
                    for sc in self.allsems:
                        if sc.n > 0:
                            e.sem_clear(sc.h)
                blk2.gpsimd(clr)
        return self.es.__exit__(*a)

    def run(self):
        nc = self.nc
        with nc.Block() as blk:
            def mk(engname):
                def body(e):
                    waited = {}
                    own = self.esem.get(engname)
                    for fn, waits, sig, inc in self.q[engname]:
                        for (s, v) in waits:
                            if waited.get(id(s), 0) >= v:
                                continue
                            e.wait_ge(s.h, v)
                            waited[id(s)] = v
                        if fn is None:
                            continue
                        ins = fn(e)
                        if sig is not None:
                            ins.then_inc(sig.h, inc)
                return body
            blk.tensor(mk("pe"))
            blk.scalar(mk("act"))
            blk.vector(mk("dve"))
            blk.gpsimd(mk("pool"))
            blk.sync(mk("sp"))


def ph_mod(nc, T):
    with Phase(nc, "mod") as P:
        ccol = P.sb("ccol", [128, 8], F32)
        cact = P.sb("cact", [128, 8], F32)
        bias = [P.sb(f"bias{l}", [1, 6144], F32) for l in range(2)]
        row = [P.sb(f"row{l}", [1, 6144], F32) for l in range(2)]
        wb = [P.sb(f"wb{i}", [128, 8, 512], F32) for i in range(3)]
        pst = [P.ps(f"ps{i}", [128, 512]) for i in range(2)]
        s_w = [P.dsem(f"w{i}") for i in range(3)]
        s_out = P.dsem("out")
        ev_c = P.dma1("sp", ccol[:], T["c_col"].ap())
        ev_b = [P.dma1("sp", bias[l][:], T["ada_b"][l:l + 1, :]) for l in range(2)]
        ev_act = P.op("act", lambda e: e.activation(out=cact[:], in_=ccol[:], func=AF.Silu), [ev_c], sig=True)
        pe_done = [None] * 3
        dve_done = [None] * 2
        n = 0
        for l in range(2):
            for nt in range(12):
                k = n % 3
                pp = n % 2
                src = T["ada_w"][l:l + 1, :, nt * 512:(nt + 1) * 512].rearrange("o (kc p) n -> p (o kc) n", p=128)
                ev_w = P.dma("sp" if n % 2 == 0 else "act", wb[k][:], src, waits=[pe_done[k]], sem=s_w[k])
                ev = None
                for kc in range(8):
                    ev = P.op("pe",
                              lambda e, kc=kc, k=k, pp=pp: e.matmul(pst[pp][0:1, :], lhsT=cact[:, kc:kc + 1],
                                                                   rhs=wb[k][:, kc, :], start=(kc == 0), stop=(kc == 7)),
                              waits=[ev_w, ev_act, dve_done[pp]] if kc == 0 else [],
                              sig=(True if kc == 7 else None))
                pe_done[k] = ev
                dve_done[pp] = P.op("dve",
                                    lambda e, l=l, nt=nt, pp=pp: e.tensor_tensor(
                                        out=row[l][0:1, nt * 512:(nt + 1) * 512], in0=pst[pp][0:1, :],
                                        in1=bias[l][0:1, nt * 512:(nt + 1) * 512], op=ALU.add),
                                    [ev, ev_b[l]], sig=True)
                n += 1
            P.dma("sp", T["mod_d"][l:l + 1, :], row[l][:], waits=[dve_done[(n - 1) % 2]], sem=s_out)


def load_mod_cols(P, T, l, which, sem):
    t = P.sb(f"mc_{l}_{which}", [128, 8], F32)
    src = T["mod_d"][l, which * 1024:(which + 1) * 1024].rearrange("(k p) -> p k", p=128)
    ev = P.dma("sp", t[:], src, sem=sem, allow_slow_non_contiguous=True)
    return t, ev


def make_ab(P, T, l, gname, sh_idx, sc_idx, sem=None):
    sem = P.dsem(f"ab_{gname}{l}")
    sh, ev1 = load_mod_cols(P, T, l, sh_idx, sem)
    sc, ev2 = load_mod_cols(P, T, l, sc_idx, sem)
    g = P.sb(f"g_{gname}{l}", [128, 8], F32)
    ev3 = P.dma("sp", g[:], T[gname][:, l, :], sem=sem)
    a = P.sb(f"a_{gname}{l}", [128, 8], F32)
    ev = P.op("dve", lambda e: e.scalar_tensor_tensor(out=a[:], in0=sc[:], scalar=1.0, in1=g[:],
                                                      op0=ALU.add, op1=ALU.mult), [ev1, ev2, ev3], sig=True)
    return a, sh, ev


def ph_qkv(nc, T):
    with Phase(nc, "qkv") as P:
        w_bf = P.sb("w_bf", [128, 8, 3088], BF16)
        ident = P.sb("ident", [128, 128], BF16)
        xt = [P.sb(f"xt{i}", [128, 1024], F32) for i in range(3)]
        xn = [P.sb(f"xn{i}", [128, 1024], BF16) for i in range(4)]
        junk = P.sb("junk", [128, 1024], BF16)
        ss = P.sb("ss", [128, 32], F32)
        sd = P.sb("sd", [128, 32], F32)
        rstd = P.sb("rstd", [128, 32], F32)
        hT = [P.sb(f"hT{i}", [128, 8, 512], BF16) for i in range(2)]
        qk_st = [P.sb(f"qkst{i}", [128, 512], BF16) for i in range(4)]
        v_st = [P.sb(f"vst{i}", [128, 1024], BF16) for i in range(2)]
        fl = P.sb("fl", [16, 4096], F32)
        tp = [P.ps(f"tp{i}", [128, 8, 128], BF16) for i in range(2)]
        ps_qk = [P.ps(f"psqk{i}", [128, 512]) for i in range(2)]
        ps_v = [P.ps(f"psv{i}", [128, 512]) for i in range(2)]
        ps_f = P.ps("psf", [128, 512])
        s_c = P.dsem("c")
        s_w = P.dsem("w")
        s_x = [P.dsem(f"x{i}") for i in range(3)]
        s_qk = [P.dsem(f"qk{i}") for i in range(4)]
        s_v = [P.dsem(f"v{i}") for i in range(2)]
        s_f = P.dsem("f")

        a_m, b_m, ev_ab = make_ab(P, T, 0, "gmix_col", 0, 1, s_c)
        ev_id = P.dma1("pool", ident[:], T["ident"].ap())
        wsrc = T["w_in"].ap().rearrange("(kc p) n -> p kc n", p=128)
        ev_ws = []
        for kc in range(8):
            ev_ws.append(P.dma1("pool", w_bf[:, kc, :], wsrc[:, kc, :]))

        xt_free = [[] for _ in range(3)]
        xn_rd = [None] * 4
        hT_free = [None] * 2
        hT_ready = {}
        xn_ready = {}

        def A(Tq):
            for sub in range(4):
                st = Tq * 4 + sub
                xb = xt[st % 3]
                ev_x = P.dma("sp", xb[:], T["x"][st * 128:(st + 1) * 128, :], waits=xt_free[st % 3], sem=s_x[st % 3])
                e1 = P.op("act", lambda e, xb=xb, st=st: e.activation(out=junk[:], in_=xb[:], func=AF.Square,
                                                                      accum_out=ss[:, st:st + 1]), [ev_x], sig=True)
                e2 = P.op("act", lambda e, st=st: e.activation(out=sd[:, st:st + 1], in_=ss[:, st:st + 1], func=AF.Sqrt,
                                                               bias=EPS, scale=1.0 / D), [], sig=True)
                P.op("dve", lambda e, st=st: e.reciprocal(out=rstd[:, st:st + 1], in_=sd[:, st:st + 1]), [e2])
                xb2 = xn[st % 4]
                e3 = P.op("dve", lambda e, xb=xb, xb2=xb2, st=st: e.tensor_scalar(
                    out=xb2[:], in0=xb[:], scalar1=rstd[:, st:st + 1], scalar2=None, op0=ALU.mult),
                    [xn_rd[st % 4]], sig=True)
                xt_free[st % 3] = [e1, e3]
                xn_ready[st] = e3

        tp_free_evs = [[], []]
        ps_qk_free = [None] * 2
        ps_v_free = [None] * 2
        ps_f_free = [None]
        qk_st_free = [None] * 4
        v_st_free = [None] * 2
        cnt = {"qk": 0, "v": 0}

        def C(Tq):
            hb = hT[Tq % 2]
            rdy = hT_ready[Tq]
            ev = None
            for oc in range(16):
                n = cnt["qk"]
                cnt["qk"] += 1
                pb = ps_qk[n % 2]
                for kc in range(8):
                    ev = P.op("pe", lambda e, pb=pb, kc=kc, oc=oc: e.matmul(
                        pb[:, :], lhsT=w_bf[:, kc, oc * 128:(oc + 1) * 128], rhs=hb[:, kc, :],
                        start=(kc == 0), stop=(kc == 7)),
                        (list(rdy) + ev_ws + [ps_qk_free[n % 2]]) if kc == 0 else [], sig=(True if kc == 7 else None))
                stb = qk_st[n % 4]
                if n % 2 == 0:
                    ev2 = P.op("act", lambda e, stb=stb, pb=pb: e.activation(out=stb[:], in_=pb[:, :], func=AF.Copy),
                               [ev, qk_st_free[n % 4]], sig=True)
                else:
                    ev2 = P.op("dve", lambda e, stb=stb, pb=pb: e.tensor_copy(out=stb[:], in_=pb[:, :]),
                               [ev, qk_st_free[n % 4]], sig=True)
                ps_qk_free[n % 2] = ev2
                dst_t = T["QT_d"] if oc < 8 else T["KT_d"]
                r0 = (oc % 8) * 128
                qk_st_free[n % 4] = P.dma("sp", dst_t[r0:r0 + 128, Tq * 512:(Tq + 1) * 512], stb[:], waits=[ev2],
                                          sem=s_qk[n % 4])
            for sub in range(4):
                vb = v_st[sub % 2]
                evs = []
                for nh in range(2):
                    n = cnt["v"]
                    cnt["v"] += 1
                    pb = ps_v[n % 2]
                    for kc in range(8):
                        ev = P.op("pe", lambda e, pb=pb, kc=kc, nh=nh, sub=sub: e.matmul(
                            pb[:, :], lhsT=hb[:, kc, sub * 128:(sub + 1) * 128],
                            rhs=w_bf[:, kc, 2048 + nh * 512:2048 + (nh + 1) * 512], start=(kc == 0), stop=(kc == 7)),
                            [ps_v_free[n % 2]] if kc == 0 else [], sig=(True if kc == 7 else None))
                    if nh == 0:
                        ev2 = P.op("act", lambda e, vb=vb, pb=pb: e.activation(out=vb[:, 0:512], in_=pb[:, :], func=AF.Copy),
                                   [ev, v_st_free[sub % 2]], sig=True)
                    else:
                        ev2 = P.op("dve", lambda e, vb=vb, pb=pb: e.tensor_copy(out=vb[:, 512:1024], in_=pb[:, :]),
                                   [ev, v_st_free[sub % 2]], sig=True)
                    ps_v_free[n % 2] = ev2
                    evs.append(ev2)
                kt = Tq * 4 + sub
                v_st_free[sub % 2] = P.dma(
                    "sp", T["V_d"][:, :, kt, :].rearrange("h p d -> p h d"),
                    vb[:].rearrange("p (h d) -> p h d", h=16), waits=evs, sem=s_v[sub % 2])
            for kc in range(8):
                ev = P.op("pe", lambda e, kc=kc: e.matmul(ps_f[0:16, :], lhsT=w_bf[:, kc, 3072:3088], rhs=hb[:, kc, :],
                                                          start=(kc == 0), stop=(kc == 7)),
                          [ps_f_free[0]] if kc == 0 else [], sig=(True if kc == 7 else None))
            hT_free[Tq % 2] = ev
            ps_f_free[0] = P.op("dve", lambda e: e.tensor_copy(out=fl[:, Tq * 512:(Tq + 1) * 512], in_=ps_f[0:16, :]),
                                [ev], sig=True)

        def B2(Tq):
            hb = hT[Tq % 2]
            last = []
            for sub in range(4):
                st = Tq * 4 + sub
                tpb = tp[st % 2]
                ev = None
                for kc in range(8):
                    ev = P.op("pe", lambda e, tpb=tpb, kc=kc, xb2=xn[st % 4]: e.transpose(
                        tpb[:, kc, :], xb2[:, kc * 128:(kc + 1) * 128], ident[:]),
                        ([xn_ready[st], ev_id] + tp_free_evs[st % 2]) if kc == 0 else [],
                        sig=(True if kc == 7 else None))
                xn_rd[st % 4] = ev
                ea = ed = None
                for kc in range(8):
                    dst = hb[:, kc, sub * 128:(sub + 1) * 128]
                    ed = P.op("dve", lambda e, dst=dst, tpb=tpb, kc=kc: e.tensor_scalar(
                        out=dst, in0=tpb[:, kc, :], scalar1=a_m[:, kc:kc + 1], scalar2=b_m[:, kc:kc + 1],
                        op0=ALU.mult, op1=ALU.add), [ev, ev_ab, hT_free[Tq % 2]], sig=True)
                    ea = ed
                tp_free_evs[st % 2] = [ea, ed]
                last = [ea, ed]
            hT_ready[Tq] = last

        A(0)
        if not DBG.get("qkv_noB"):
            B2(0)
        for Tq in range(8):
            if Tq + 1 < 8:
                A(Tq + 1)
            if not DBG.get("qkv_noC") and not DBG.get("qkv_noB"):
                C(Tq)
            if Tq + 1 < 8 and not DBG.get("qkv_noB"):
                B2(Tq + 1)
        if not DBG.get("qkv_noC") and not DBG.get("qkv_noB"):
            P.dma("sp", T["FL_d"].ap(), fl[:], waits=[ps_f_free[0]], sem=s_f)


def ph_fgate(nc, T):
    with Phase(nc, "fg") as P:
        fl = P.sb("fl", [16, 4096], F32)
        u = P.sb("u", [16, 4096], F32)
        lf = P.sb("lf", [16, 4096], F32)
        ones = P.sb("ones", [16, 4096], F32)
        G = P.sb("G", [16, 4096], F32)
        fb = P.sb("fb", [16, 1], F32)
        nfb = P.sb("nfb", [16, 1], F32)
        parts = [P.sb(f"part{i}", [16, 4096], BF16) for i in range(3)]
        nparts = [P.sb(f"npart{i}", [16, 4096], BF16) for i in range(3)]
        s_in = P.dsem("in")
        s_out = P.dsem("out")
        e_fl = P.dma1("sp", fl[:], T["FL_d"].ap())
        e_fb = P.dma1("sp", fb[:], T["fb_col"].ap())
        e0 = P.op("dve", lambda e: e.tensor_scalar(out=nfb[:], in0=fb[:], scalar1=-1.0, scalar2=None, op0=ALU.mult),
                  [e_fb], sig=True)
        P.op("dve", lambda e: e.memset(ones[:], 1.0))
        e0b = P.op("dve", lambda e: e.tensor_scalar(out=fl[:], in0=fl[:], scalar1=fb[:, 0:1], scalar2=None, op0=ALU.add),
                   [e_fl, e_fb], sig=True)
        e1 = P.op("act", lambda e: e.activation(out=u[:], in_=fl[:], func=AF.Exp, scale=-1.0), [e0b], sig=True)
        e2 = P.op("act", lambda e: e.activation(out=lf[:], in_=u[:], func=AF.Ln, bias=1.0, scale=1.0), [], sig=True)
        P.op("dve", lambda e: e.tensor_tensor_scan(out=G[:], data0=ones[:], data1=lf[:], initial=0.0,
                                                   op0=ALU.mult, op1=ALU.add), [e2])
        P.op("dve", lambda e: e.tensor_scalar(out=u[:], in0=G[:], scalar1=8.0, scalar2=None, op0=ALU.mult))
        P.op("dve", lambda e: e.tensor_copy(out=parts[0][:], in_=u[:]))
        P.op("dve", lambda e: e.tensor_tensor(out=lf[:], in0=u[:], in1=parts[0][:], op=ALU.subtract))
        P.op("dve", lambda e: e.tensor_copy(out=parts[1][:], in_=lf[:]))
        P.op("dve", lambda e: e.tensor_tensor(out=u[:], in0=lf[:], in1=parts[1][:], op=ALU.subtract))
        P.op("dve", lambda e: e.tensor_copy(out=parts[2][:], in_=u[:]))
        ev = None
        for i in range(3):
            ev = P.op("dve", lambda e, i=i: e.tensor_scalar(out=nparts[i][:], in0=parts[i][:], scalar1=-1.0,
                                                            scalar2=None, op0=ALU.mult), [], sig=True)
        for i in range(3):
            P.dma("sp", T["KF_d"][:, i, :], parts[i][:], waits=[ev], sem=s_out)
            P.dma("sp", T["QF_d"][:, i, :], nparts[i][:], waits=[ev], sem=s_out)


def ph_attn(nc, T, OT_all):
    with Phase(nc, "att") as P:
        Qa = [P.sb(f"Qa{i}", [70, 4096], BF16) for i in range(2)]
        Ka = [P.sb(f"Ka{i}", [70, 4096], BF16) for i in range(2)]
        Va = [P.sb(f"Va{i}", [128, 32, 128], BF16) for i in range(2)]
        Pt = [P.sb(f"Pt{i}", [128, 512], BF16) for i in range(3)]
        rec = [P.sb(f"rec{i}", [128, 512], F32) for i in range(2)]
        ident = P.sb("ident", [128, 128], BF16)
        mtri = P.sb("mtri", [128, 128], BF16)
        s_ps = [P.ps(f"s{i}", [128, 512]) for i in range(3)]
        o_ps = [P.ps(f"o{i}", [128, 512]) for i in range(2)]
        s_c = P.dsem("c")
        s_h = [P.dsem(f"h{i}") for i in range(2)]
        e_id = P.dma1("pool", ident[:], T["ident"].ap())
        e_mt = P.dma1("pool", mtri[:], T["mtri"].ap())
        e_ms = None
        for i in range(2):
            P.op("dve", lambda e, i=i: e.memset(Qa[i][64:70, :], 1.0))
            P.op("dve", lambda e, i=i: e.memset(Ka[i][64:70, :], 1.0))
            e_ms = P.op("dve", lambda e, i=i: e.memset(Va[i][:, :, 64:128], 1.0), [], sig=True)

        head_done = [None] * NH

        def load_head(h):
            p = h % 2
            w = [e_ms, head_done[h - 2] if h >= 2 else None]
            P.dma("sp", Qa[p][0:64, :], T["QT_d"][h * 64:(h + 1) * 64, :], waits=w, sem=s_h[p])
            P.dma("sp", Qa[p][64:67, :], T["QF_d"][h, :, :], sem=s_h[p])
            P.dma("sp", Ka[p][0:64, :], T["KT_d"][h * 64:(h + 1) * 64, :], sem=s_h[p])
            P.dma("sp", Ka[p][67:70, :], T["KF_d"][h, :, :], sem=s_h[p])
            return P.dma("sp", Va[p][:, :, 0:64], T["V_d"][h, :, :, :], sem=s_h[p])

        tiles = []
        for h in range(NH):
            for j in range(8):
                for i in range(4 * j + 4):
                    tiles.append((h, j, i))
        NTL = len(tiles)
        head_ev = {0: load_head(0), 1: load_head(1)}
        exp_ev = [None] * NTL
        pv_ev = {}
        s_ev = [None] * NTL
        norm_ev = {}
        last_pv = {}

        def rec_S(n):
            h, j, i = tiles[n]
            p = h % 2
            sb_ = s_ps[n % 3]
            r = i - 4 * j
            w = [head_ev[h], exp_ev[n - 3] if n >= 3 else None, e_id, e_mt]
            if r < 0:
                s_ev[n] = P.op("pe", lambda e: e.matmul(sb_[:, :], lhsT=Ka[p][0:70, i * 128:(i + 1) * 128],
                                                        rhs=Qa[p][0:70, j * 512:(j + 1) * 512], start=True, stop=True),
                               w, sig=True)
            else:
                c0 = 128 * r
                P.op("pe", lambda e: e.matmul(sb_[:, c0:c0 + 128], lhsT=ident[:], rhs=mtri[:], start=True, stop=False), w)
                ev = P.op("pe", lambda e: e.matmul(sb_[:, c0:c0 + 128], lhsT=Ka[p][0:70, i * 128:(i + 1) * 128],
                                                   rhs=Qa[p][0:70, j * 512 + c0:j * 512 + c0 + 128], start=False, stop=True),
                          [], sig=(True if r == 3 else None))
                if r < 3:
                    ev = P.op("pe", lambda e: e.matmul(sb_[:, c0 + 128:512], lhsT=Ka[p][0:70, i * 128:(i + 1) * 128],
                                                       rhs=Qa[p][0:70, j * 512 + c0 + 128:(j + 1) * 512], start=True, stop=True),
                              [], sig=True)
                s_ev[n] = ev

        def rec_exp(n):
            h, j, i = tiles[n]
            r = i - 4 * j
            c0 = 0 if r < 0 else 128 * r
            sb_ = s_ps[n % 3]
            pt = Pt[n % 3]
            exp_ev[n] = P.op("act", lambda e: e.activation(out=pt[:, c0:512], in_=sb_[:, c0:512], func=AF.Exp, scale=0.125),
                             [s_ev[n], pv_ev.get(n - 3)], sig=True, chain=False)

        def rec_PV(n):
            h, j, i = tiles[n]
            p = h % 2
            r = i - 4 * j
            c0 = 0 if r < 0 else 128 * r
            pt = Pt[n % 3]
            qn = h * 8 + j
            ob = o_ps[qn % 2]
            first = (i == 0)
            lastk = (i == 4 * j + 3)
            w = [exp_ev[n]]
            if first and qn >= 2:
                w.append(norm_ev[qn - 2])
            ev = P.op("pe", lambda e: e.matmul(ob[:, c0:512], lhsT=Va[p][:, i, :], rhs=pt[:, c0:512], start=first, stop=lastk),
                      w, sig=(True if lastk else None))
            if lastk:
                rb = rec[qn % 2]
                P.op("dve", lambda e: e.reciprocal(out=rb[64:128, :], in_=ob[64:128, :]), [ev])
                po = (h % 2) * 64
                norm_ev[qn] = P.op("dve", lambda e: e.tensor_tensor(
                    out=OT_all[po:po + 64, h // 2, j * 512:(j + 1) * 512], in0=ob[0:64, :], in1=rb[64:128, :], op=ALU.mult),
                    [], sig=True)
                if j == 7:
                    head_done[h] = ev
                    if h + 2 < NH:
                        head_ev[h + 2] = load_head(h + 2)

        LA = 2
        for n in range(min(LA, NTL)):
            rec_S(n)
        for n in range(NTL):
            rec_exp(n)
            if n + LA < NTL:
                rec_S(n + LA)
            rec_PV(n)


def ph_oproj(nc, T, OT_all):
    with Phase(nc, "op") as P:
        wo = P.sb("wo", [128, 8, 1024], BF16)
        gt = P.sb("gt", [128, 1024], F32)
        xt = [P.sb(f"xt{i}", [128, 1024], F32) for i in range(3)]
        t1 = [P.sb(f"t1{i}", [128, 1024], F32) for i in range(2)]
        x1 = [P.sb(f"x1{i}", [128, 1024], F32) for i in range(2)]
        y_ps = [P.ps(f"y{i}", [128, 1024]) for i in range(2)]
        s_c = P.dsem("c")
        s_x = [P.dsem(f"x{i}") for i in range(3)]
        s_o = [P.dsem(f"o{i}") for i in range(2)]
        e_w = P.dma1("pool", wo[:], T["w_o"].ap().rearrange("(c p) n -> p c n", p=128))
        e_g = P.dma1("sp", gt[:], T["mod_d"][0, 2048:3072].partition_broadcast(128))
        xt_free = [None] * 3
        t1_free = [None] * 2
        x1_free = [None] * 2
        y_free = [None] * 2
        for tt in range(NT):
            xb = xt[tt % 3]
            e_x = P.dma("sp", xb[:], T["x"][tt * 128:(tt + 1) * 128, :], waits=[xt_free[tt % 3]], sem=s_x[tt % 3])
            yb = y_ps[tt % 2]
            ev = None
            for nh in range(2):
                for c in range(8):
                    ev = P.op("pe", lambda e, nh=nh, c=c, yb=yb, tt=tt: e.matmul(
                        yb[:, nh * 512:(nh + 1) * 512], lhsT=OT_all[:, c, tt * 128:(tt + 1) * 128],
                        rhs=wo[:, c, nh * 512:(nh + 1) * 512], start=(c == 0), stop=(c == 7)),
                        [e_w, y_free[tt % 2]] if (c == 0 and nh == 0) else [], sig=(True if (c == 7 and nh == 1) else None))
            tb = t1[tt % 2]
            e_t = P.op("dve", lambda e, tb=tb, yb=yb: e.tensor_tensor(out=tb[:], in0=yb[:, :], in1=gt[:], op=ALU.mult),
                       [ev, e_g, t1_free[tt % 2]], sig=True)
            y_free[tt % 2] = e_t
            ob = x1[tt % 2]
            e_a = P.op("dve", lambda e, tb=tb, xb=xb, ob=ob: e.tensor_tensor(out=ob[:], in0=tb[:], in1=xb[:], op=ALU.add),
                       [e_t, e_x, x1_free[tt % 2]], sig=True)
            t1_free[tt % 2] = e_a
            xt_free[tt % 3] = e_a
            x1_free[tt % 2] = P.dma("sp", T["X1_d"][tt * 128:(tt + 1) * 128, :], ob[:], waits=[e_a], sem=s_o[tt % 2])


def ph_front(nc, T, l, R, Xin):
    with Phase(nc, f"fr{l}") as P:
        s_c = P.dsem("c")
        s_x = [P.dsem(f"x{i}") for i in range(3)]
        s_sc = P.dsem("sc")
        a_f, b_f, ev_ab = make_ab(P, T, l, "gffn_col", 3, 4, s_c)
        rw = P.sb("rw", [128, 8, 32], F32)
        rwp = P.sb("rwp", [128, 8, 32], F32)
        rconst = P.sb("rconst", [1, 32], F32)
        ones_row = P.sb("ones_row", [1, 128], F32)
        identf = P.sb("identf", [128, 128], F32)
        ltri = P.sb("ltri", [128, 128], F32)
        onesf = P.sb("onesf", [128, 128], F32)
        rbias = P.sb("rbias", [128, 32], F32)
        thr = P.sb("thr", [128, 16], F32)
        biota = P.sb("biota", [128, 64], F32)
        ones32 = P.sb("ones32", [128, 32], F32)
        xt = [P.sb(f"xt{i}", [128, 1024], F32) for i in range(3)]
        xnf = [P.sb(f"xnf{i}", [128, 1024], F32) for i in range(2)]
        xnT = [P.sb(f"xnT{i}", [128, 8, 128], F32) for i in range(2)]
        junk = P.sb("junk", [128, 1024], BF16)
        xn_all = P.sb("xn_all", [128, 32, 1024], BF16)
        ss = P.sb("ss", [128, 32], F32)
        sd = P.sb("sd", [128, 32], F32)
        rstd = P.sb("rstd", [128, 32], F32)
        Mall = P.sb("Mall", [128, 32, 32], F32)
        Gall = P.sb("Gall", [128, 32, 32], F32)
        posl = P.sb("posl", [128, 32, 32], F32)
        CSb = P.sb("CSb", [128, 32, 32], F32)
        carry = P.sb("carry", [128, 32, 32], F32)
        W1 = P.sb("W1", [128, 32, 32], F32)
        W2 = P.sb("W2", [128, 32, 32], F32)
        cmpb = P.sb("cmpb", [128, 64, 32], F32)
        scores = [P.sb(f"scores{i}", [128, 32], F32) for i in range(2)]
        sel = P.sb("sel", [128, 32], F32)
        mx = P.sb("mx", [128, 4, 8], F32)
        gs = P.sb("gs", [128, 4], F32)
        gmax = P.sb("gmax", [128, 1], F32)
        gm = P.sb("gm", [128, 4], F32)
        pen = P.sb("pen", [128, 4], F32)
        selm = P.sb("selm", [128, 32], F32)
        t8 = P.sb("t8", [128, 8], F32)
        sm = P.sb("sm", [128, 32], F32)
        den = P.sb("den", [128, 1], F32)
        rden = P.sb("rden", [128, 1], F32)
        cnt = P.sb("cnt", [128, 32], F32)
        nb = P.sb("nb", [128, 32], F32)
        pend = P.sb("pend", [128, 32], F32)
        pst = P.sb("pst", [128, 32], F32)
        da1 = P.sb("da1", [128, 32], F32)
        sumD = P.sb("sumD", [128, 32], F32)
        db1 = P.sb("db1", [128, 32], F32)
        gsum = P.sb("gsum", [128, 32], F32)
        bef = P.sb("bef", [128, 64], F32)
        tpf = P.ps("tpf", [128, 8, 128], F32)
        lg_ps = [P.ps(f"lg{i}", [128, 32]) for i in range(2)]
        pos_ps = [P.ps(f"pos{i}", [128, 32]) for i in range(2)]
        cs_ps = P.ps("cs", [128, 1024])

        ZN = NBLK * BLK // (128 * 16)
        zt = P.sb("zt", [128, ZN * 1024], BF16)
        s_z = P.dsem("z")
        e_zm = P.op("pool", lambda e: e.memset(zt[:], 0.0), [], sig=True)
        xgv = T["XG_d"].ap().rearrange("(p j n) d -> p j (n d)", p=128, j=16)
        e_z = None
        for j in range(16):
            e_z = P.dma("sp", xgv[:, j, :], zt[:], waits=[e_zm], sem=s_z)
        e_c = []
        for (t_, nm) in ((rw, "rw"), (identf, "identf"), (ltri, "ltri"), (onesf, "ones"), (rbias, "rbias_row"),
                         (thr, "thr"), (biota, "biota")):
            e_c.append(P.dma("sp", t_[:], T[nm].ap(), sem=s_c))
        P.op("dve", lambda e: e.memset(ones_row[:], 1.0))
        P.op("dve", lambda e: e.memset(ones32[:], 1.0))
        ev = None
        for kc in range(8):
            ev = P.op("dve", lambda e, kc=kc: e.tensor_scalar(out=rwp[:, kc, :], in0=rw[:, kc, :], scalar1=a_f[:, kc:kc + 1],
                                                              scalar2=None, op0=ALU.mult), e_c + [ev_ab], sig=True)
        e_rwp = ev
        for kc in range(8):
            ev = P.op("pe", lambda e, kc=kc: e.matmul(cs_ps[0:1, 0:32], lhsT=b_f[:, kc:kc + 1], rhs=rw[:, kc, :],
                                                      start=(kc == 0), stop=(kc == 7)), e_c + [ev_ab] if kc == 0 else [],
                      sig=(True if kc == 7 else None))
        e_rc = P.op("dve", lambda e: e.tensor_copy(out=rconst[:], in_=cs_ps[0:1, 0:32]), [ev], sig=True)

        FS = DBG.get("front_stop", 99)
        xt_free = [[] for _ in range(3)]
        xnf_rd = [None] * 2
        xnT_rd = [None] * 2
        tpf_free = []
        lg_free = [None] * 2
        sc_free = [None] * 2
        pos_free = [None] * 2
        sig_ev = {}
        xnf_rd2 = [None] * 2

        def stageA(tt):
            nonlocal tpf_free
            xb = xt[tt % 3]
            e_x = P.dma("sp", xb[:], Xin[tt * 128:(tt + 1) * 128, :], waits=xt_free[tt % 3], sem=s_x[tt % 3])
            e1 = P.op("act", lambda e: e.activation(out=junk[:], in_=xb[:], func=AF.Square, accum_out=ss[:, tt:tt + 1]),
                      [e_x], sig=True)
            e2 = P.op("act", lambda e: e.activation(out=sd[:, tt:tt + 1], in_=ss[:, tt:tt + 1], func=AF.Sqrt,
                                                    bias=EPS, scale=1.0 / D), [], sig=True)
            P.op("dve", lambda e: e.reciprocal(out=rstd[:, tt:tt + 1], in_=sd[:, tt:tt + 1]), [e2])
            xf = xnf[tt % 2]
            e3 = P.op("dve", lambda e: e.tensor_scalar(out=xf[:], in0=xb[:], scalar1=rstd[:, tt:tt + 1], scalar2=None,
                                                       op0=ALU.mult), [xnf_rd[tt % 2], xnf_rd2[tt % 2]], sig=True)
            e4 = P.op("act", lambda e: e.activation(out=xn_all[:, tt, :], in_=xf[:], func=AF.Copy), [e3], sig=True)
            xt_free[tt % 3] = [e3, e1]
            xnf_rd2[tt % 2] = e4
            ev = None
            for kc in range(8):
                ev = P.op("pe", lambda e, kc=kc: e.transpose(tpf[:, kc, :], xf[:, kc * 128:(kc + 1) * 128], identf[:]),
                          ([e3] + e_c + tpf_free) if kc == 0 else [], sig=(True if kc == 7 else None))
            xnf_rd[tt % 2] = ev
            xT = xnT[tt % 2]
            ea = P.op("act", lambda e: e.activation(out=xT[:, 0:4, :], in_=tpf[:, 0:4, :], func=AF.Copy),
                      [ev, xnT_rd[tt % 2]], sig=True)
            ed = P.op("dve", lambda e: e.tensor_copy(out=xT[:, 4:8, :], in_=tpf[:, 4:8, :]),
                      [ev, xnT_rd[tt % 2]], sig=True)
            tpf_free = [ea, ed]
            lg = lg_ps[tt % 2]
            for kc in range(8):
                P.op("pe", lambda e, kc=kc: e.matmul(lg[:, :], lhsT=xT[:, kc, :], rhs=rwp[:, kc, :], start=(kc == 0), stop=False),
                     [ea, ed, e_rwp, e_rc, lg_free[tt % 2]] if kc == 0 else [])
            ev = P.op("pe", lambda e: e.matmul(lg[:, :], lhsT=ones_row[0:1, :], rhs=rconst[0:1, :], start=False, stop=True),
                      [], sig=True)
            xnT_rd[tt % 2] = ev
            sc = scores[tt % 2]
            e_s = P.op("act", lambda e: e.activation(out=sc[:], in_=lg[:, :], func=AF.Sigmoid), [ev, sc_free[tt % 2]], sig=True)
            lg_free[tt % 2] = e_s
            sig_ev[tt] = e_s

        def stageB(tt):
            sc = scores[tt % 2]
            e_s = sig_ev[tt]
            P.op("dve", lambda e: e.tensor_tensor(out=sel[:], in0=sc[:], in1=rbias[:], op=ALU.add), [e_s])
            for g in range(4):
                P.op("dve", lambda e, g=g: e.max(out=mx[:, g, :], in_=sel[:, g * 8:(g + 1) * 8]))
            P.op("dve", lambda e: e.tensor_tensor(out=gs[:], in0=mx[:, :, 0], in1=mx[:, :, 1], op=ALU.add))
            P.op("dve", lambda e: e.tensor_reduce(out=gmax[:], in_=gs[:], axis=AX.X, op=ALU.max))
            P.op("dve", lambda e: e.tensor_scalar(out=gm[:], in0=gs[:], scalar1=gmax[:, 0:1], scalar2=None, op0=ALU.is_equal))
            P.op("dve", lambda e: e.tensor_scalar(out=pen[:], in0=gm[:], scalar1=-1.0, scalar2=1e9, op0=ALU.add, op1=ALU.mult))
            P.op("dve", lambda e: e.tensor_tensor(out=selm[:].rearrange("p (g k) -> p g k", g=4),
                                                  in0=sel[:].rearrange("p (g k) -> p g k", g=4),
                                                  in1=pen[:].unsqueeze(2).to_broadcast([128, 4, 8]), op=ALU.add))
            P.op("dve", lambda e: e.max(out=t8[:], in_=selm[:]))
            e_M = P.op("dve", lambda e: e.tensor_scalar(out=Mall[:, tt, :], in0=selm[:], scalar1=t8[:, 1:2], scalar2=None,
                                                        op0=ALU.is_ge), [], sig=True)
            P.op("dve", lambda e: e.tensor_tensor(out=sm[:], in0=Mall[:, tt, :], in1=sc[:], op=ALU.mult))
            P.op("dve", lambda e: e.tensor_reduce(out=den[:], in_=sm[:], axis=AX.X, op=ALU.add))
            P.op("dve", lambda e: e.reciprocal(out=rden[:], in_=den[:]))
            sc_free[tt % 2] = P.op("dve", lambda e: e.tensor_scalar(out=Gall[:, tt, :], in0=sm[:], scalar1=rden[:, 0:1],
                                                                  scalar2=None, op0=ALU.mult), [], sig=True)
            pp = pos_ps[tt % 2]
            ev = P.op("pe", lambda e: e.matmul(pp[:, :], lhsT=ltri[:], rhs=Mall[:, tt, :], start=True, stop=True),
                      [e_M, pos_free[tt % 2]], sig=True)
            pos_free[tt % 2] = P.op("act", lambda e: e.activation(out=posl[:, tt, :], in_=pp[:, :], func=AF.Copy), [ev], sig=True)

        stageA(0)
        for tt in range(NT):
            if tt + 1 < NT:
                stageA(tt + 1)
            stageB(tt)
        if FS <= 4:
            return
        e_pos = pos_free[(NT - 1) % 2]
        Mf = Mall[:].rearrange("p a b -> p (a b)")
        ev = None
        for h2 in range(2):
            ev = P.op("pe", lambda e, h2=h2: e.matmul(cs_ps[:, h2 * 512:(h2 + 1) * 512], lhsT=onesf[:],
                                                      rhs=Mf[:, h2 * 512:(h2 + 1) * 512], start=True, stop=True),
                      [sc_free[(NT - 1) % 2]], sig=(True if h2 == 1 else None))
        P.op("dve", lambda e: e.tensor_copy(out=CSb[:].rearrange("p a b -> p (a b)"), in_=cs_ps[:, :]), [ev, e_pos])
        P.op("dve", lambda e: e.memset(carry[:, 0, :], 0.0))
        for tt in range(1, NT):
            P.op("dve", lambda e, tt=tt: e.tensor_tensor(out=carry[:, tt, :], in0=carry[:, tt - 1, :], in1=CSb[:, tt - 1, :],
                                                         op=ALU.add))
        P.op("dve", lambda e: e.tensor_tensor(out=cnt[:], in0=carry[:, NT - 1, :], in1=CSb[:, NT - 1, :], op=ALU.add))
        P.op("dve", lambda e: e.tensor_tensor(out=W1[:, :, 0:16], in0=cnt[:].unsqueeze(2).to_broadcast([128, 32, 16]),
                                              in1=thr[:].unsqueeze(1).to_broadcast([128, 32, 16]), op=ALU.is_gt))
        P.op("dve", lambda e: e.tensor_reduce(out=nb[:], in_=W1[:, :, 0:16], axis=AX.X, op=ALU.add))
        P.op("dve", lambda e: e.tensor_tensor_scan(out=pend[:], data0=ones32[:], data1=nb[:], initial=0.0,
                                                   op0=ALU.mult, op1=ALU.add))
        P.op("dve", lambda e: e.tensor_tensor(out=pst[:], in0=pend[:], in1=nb[:], op=ALU.subtract))
        P.op("dve", lambda e: e.tensor_scalar(out=pst[:], in0=pst[:], scalar1=float(BLK), scalar2=None, op0=ALU.mult))
        P.op("dve", lambda e: e.tensor_tensor(out=W1[:], in0=posl[:], in1=carry[:], op=ALU.add))
        P.op("dve", lambda e: e.tensor_tensor(out=W1[:], in0=W1[:], in1=pst[:].unsqueeze(1).to_broadcast([128, 32, 32]),
                                              op=ALU.add))
        P.op("dve", lambda e: e.scalar_tensor_tensor(out=W1[:], in0=W1[:], scalar=1.0, in1=Mall[:], op0=ALU.add, op1=ALU.mult))
        P.op("dve", lambda e: e.tensor_reduce(out=da1[:], in_=W1[:], axis=AX.X, op=ALU.max))
        P.op("dve", lambda e: e.tensor_reduce(out=sumD[:], in_=W1[:], axis=AX.X, op=ALU.add))
        P.op("dve", lambda e: e.tensor_tensor(out=db1[:], in0=sumD[:], in1=da1[:], op=ALU.subtract))
        dtmp = P.sb("dtmp", [128, 32], F32)
        for (src_, nm_) in ((da1, "destA"), (db1, "destB")):
            P.op("dve", lambda e, src_=src_: e.tensor_scalar(out=dtmp[:], in0=src_[:], scalar1=-1.0, scalar2=0.0,
                                                            op0=ALU.add, op1=ALU.max))
            P.op("dve", lambda e, nm_=nm_: e.tensor_scalar(out=R[nm_][:], in0=dtmp[:], scalar1=float(NBLK * BLK - 1),
                                                         scalar2=None, op0=ALU.min))
        P.op("dve", lambda e: e.tensor_tensor(out=W2[:], in0=W1[:], in1=da1[:].unsqueeze(2).to_broadcast([128, 32, 32]),
                                              op=ALU.is_equal))
        P.op("dve", lambda e: e.tensor_tensor(out=W2[:], in0=W2[:], in1=Gall[:], op=ALU.mult))
        P.op("dve", lambda e: e.tensor_reduce(out=R["gA"][:], in_=W2[:], axis=AX.X, op=ALU.add))
        P.op("dve", lambda e: e.tensor_reduce(out=gsum[:], in_=Gall[:], axis=AX.X, op=ALU.add))
        P.op("dve", lambda e: e.tensor_tensor(out=R["gB"][:], in0=gsum[:], in1=R["gA"][:], op=ALU.subtract))
        P.op("dve", lambda e: e.tensor_tensor(out=cmpb[:], in0=pend[:].unsqueeze(1).to_broadcast([128, 64, 32]),
                                              in1=biota[:].unsqueeze(2).to_broadcast([128, 64, 32]), op=ALU.is_le))
        P.op("dve", lambda e: e.tensor_reduce(out=bef[:], in_=cmpb[:], axis=AX.X, op=ALU.add))
        e_tab = P.op("dve", lambda e: e.tensor_scalar(out=R["blk_e"][:], in0=bef[:], scalar1=31.0, scalar2=None, op0=ALU.min),
                     [], sig=True)
        piota = P.sb("piota", [128, 1], F32)
        e_pi = P.dma1("sp", piota[:], T["piota"].ap())
        e_tab = P.op("dve", lambda e: e.tensor_scalar(out=R["idxW"][:], in0=R["blk_e"][:], scalar1=128.0, scalar2=piota[:, 0:1],
                                                      op0=ALU.mult, op1=ALU.add), [e_pi], sig=True)
        if FS <= 5:
            return
        if "Ri_dbg" in T and l == DBG.get("dump_layer", 0):
            s_dbg = P.dsem("dbg")
            P.dma("sp", T["Ri_dbg"][0, :, :], R["destA"][:], waits=[e_tab], sem=s_dbg)
            P.dma("sp", T["Ri_dbg"][1, :, :], R["destB"][:], waits=[e_tab], sem=s_dbg)
            P.dma("sp", T["Rf_dbg"][0, :, 0:32], R["gA"][:], waits=[e_tab], sem=s_dbg)
            P.dma("sp", T["Rf_dbg"][1, :, 0:32], R["gB"][:], waits=[e_tab], sem=s_dbg)
            P.dma("sp", T["Rf_dbg"][2, :, :], R["blk_e"][:], waits=[e_tab], sem=s_dbg)
        if FS <= 6:
            return
        breg = {}

        def bc(e):
            if "r" not in breg:
                breg["r"] = e.alloc_register("bc_" + P.name)
                e.reg_mov(breg["r"], NBLK * BLK - 1)
            return breg["r"]
        s_sc4 = [s_sc, P.dsem("sc2"), P.dsem("sc3"), P.dsem("sc4")]
        sc_prev = [None] * 4
        for tt in range(NT):
            for i_, nm in enumerate(("destA", "destB")):
                k_ = (tt % 2) * 2 + i_
                sc_prev[k_] = P.op("pool", lambda e, tt=tt, nm=nm: e.indirect_dma_start(
                    out=T["XG_d"][:, :], out_offset=bass.IndirectOffsetOnAxis(ap=R[nm][:, tt:tt + 1], axis=0),
                    in_=xn_all[:, tt, :], in_offset=None),
                    [e_tab, P.last.get("pool"), P.last.get("act"), e_z, sc_prev[k_]], sig=s_sc4[k_], inc=16)


def ph_moe(nc, T, l, R):
    with Phase(nc, f"moe{l}") as P:
        s_c = P.dsem("c")
        s_w = [[P.dsem(f"w{i}_{j}") for j in range(3)] for i in range(2)]
        s_xr = [P.dsem(f"xr{i}") for i in range(2)]
        s_y = [P.dsem(f"y{i}") for i in range(2)]
        sh = P.sb("sh", [128, 8], F32)
        sc = P.sb("sc", [128, 8], F32)
        g = P.sb("g", [128, 8], F32)
        a_f = P.sb("a_f", [128, 8], F32)
        e1 = P.dma("sp", sh[:], T["mod_d"][l, 3 * 1024:4 * 1024].rearrange("(p k) -> p k", k=8), sem=s_c)
        e2 = P.dma("sp", sc[:], T["mod_d"][l, 4 * 1024:5 * 1024].rearrange("(p k) -> p k", k=8), sem=s_c)
        e3 = P.dma("sp", g[:], T["gffn_pk"][:, l, :], sem=s_c)
        ev_ab = P.op("dve", lambda e: e.scalar_tensor_tensor(out=a_f[:], in0=sc[:], scalar=1.0, in1=g[:],
                                                            op0=ALU.add, op1=ALU.mult), [e1, e2, e3], sig=True)
        b_f = sh
        ident = P.sb("ident", [128, 128], BF16)
        e_id = P.dma1("pool", ident[:], T["ident"].ap())
        w1b = [P.sb(f"w1b{i}", [128, 8, 512], BF16) for i in range(2)]
        w3b = [P.sb(f"w3b{i}", [128, 8, 512], BF16) for i in range(2)]
        w2b = [P.sb(f"w2b{i}", [128, 4, 1024], BF16) for i in range(2)]
        xr = [P.sb(f"xr{i}", [128, NS, 1024], BF16) for i in range(2)]
        hT = [P.sb(f"hT{i}", [128, 8, BLK], BF16) for i in range(2)]
        sa = [P.sb(f"sa{i}", [128, BLK], F32) for i in range(2)]
        hid = [P.sb(f"hid{i}", [128, 4, BLK], BF16) for i in range(2)]
        yst = [P.sb(f"yst{i}", [128, NS, 1024], F32) for i in range(2)]
        tp = P.ps("tp", [128, 16, 128], BF16)
        a_ps = [P.ps(f"a{i}", [128, 512]) for i in range(2)]
        b_ps = [P.ps(f"b{i}", [128, 512]) for i in range(2)]
        y_ps = [P.ps(f"y{i}", [128, 512]) for i in range(2)]
        w1v = T[f"w1_{l}"].ap().rearrange("e (p k) n -> (e p) (k n)", k=8)
        w3v = T[f"w3_{l}"].ap().rearrange("e (p k) n -> (e p) (k n)", k=8)
        w2v = T[f"w2_{l}"].ap().rearrange("e (p k) n -> (e p) (k n)", k=4)

        wb_free = [None] * 2
        xr_free = [None] * 2
        hT_free = [None] * 2
        tp_free = []
        ab_free = [None] * 2
        sa_free = [None] * 2
        hid_free = [None] * 2
        yps_free = [None] * 2
        yst_free = [None] * 2
        ny = 0
        nab = 0
        breg = {}

        def bc(e):
            if "r" not in breg:
                breg["r"] = e.alloc_register("bcw_" + P.name)
                e.reg_mov(breg["r"], E * 128 - 1)
            return breg["r"]

        def load_w(b):
            pb = b % 2
            evs_ = []
            prev = w_ev.get(b - 1) or []
            for j_, (src, dst) in enumerate(((w1v, w1b[pb]), (w3v, w3b[pb]), (w2v, w2b[pb]))):
                flat = dst[:].rearrange("p k n -> p (k n)")
                evs_.append(P.op("pool", lambda e, src=src, flat=flat: e.indirect_dma_start(
                    out=flat, out_offset=None, in_=src,
                    in_offset=bass.IndirectOffsetOnAxis(ap=R["idxW"][:, b:b + 1], axis=0),
                    ), [wb_free[pb]] + list(prev), sig=s_w[pb][j_], inc=16))
            return evs_

        def load_x(b):
            pb = b % 2
            return P.dma("sp", xr[pb][:], T["XG_d"][b * BLK:(b + 1) * BLK, :].rearrange("(s p) d -> p s d", p=128),
                         waits=[xr_free[pb]], sem=s_xr[pb])

        w_ev = {}
        w_ev[0] = load_w(0)
        x_ev = {0: load_x(0)}

        def do_block(b):
            nonlocal ny, nab, tp_free
            pb = b % 2
            if b + 1 < NBLK:
                w_ev[b + 1] = load_w(b + 1)
                x_ev[b + 1] = load_x(b + 1)
            xb = xr[pb]
            hb = hT[pb]
            tpv = tp[:].rearrange("p (s k) t -> p s k t", s=2)
            ev = None
            ea = ed = None
            for g_ in range(NS // 2):
                for s2 in range(2):
                    s_ = g_ * 2 + s2
                    xv = xb[:, s_, :].rearrange("p (q k) -> p k q", k=8)
                    for kc in range(8):
                        first = (s2 == 0 and kc == 0)
                        lastt = (s2 == 1 and kc == 7)
                        ev = P.op("pe", lambda e, s2=s2, kc=kc, xv=xv: e.transpose(tp[:, s2 * 8 + kc, :], xv[:, kc, :], ident[:]),
                                  ([x_ev[b], e_id] + tp_free) if first else [], sig=(True if lastt else None))
                for kc in range(8):
                    dst = hb[:, kc, g_ * 256:(g_ + 1) * 256].rearrange("p (s t) -> p s t", s=2)
                    ed = P.op("dve", lambda e, dst=dst, kc=kc: e.tensor_scalar(out=dst, in0=tpv[:, :, kc, :],
                                                                              scalar1=a_f[:, kc:kc + 1], scalar2=b_f[:, kc:kc + 1],
                                                                              op0=ALU.mult, op1=ALU.add),
                              [ev, ev_ab, hT_free[pb]], sig=True)
                    ea = ed
                tp_free = [ea, ed]
            xr_free[pb] = ev
            hd = hid[pb]
            w1s = w1b[pb][:].rearrange("p k (q f) -> p k f q", f=4)
            w3s = w3b[pb][:].rearrange("p k (q f) -> p k f q", f=4)
            e_h = None
            for fc in range(4):
                apb = a_ps[nab % 2]
                bpb = b_ps[nab % 2]
                for kc in range(8):
                    P.op("pe", lambda e, apb=apb, kc=kc, fc=fc: e.matmul(apb[:, 0:BLK], lhsT=w1s[:, kc, fc, :],
                                                                        rhs=hb[:, kc, :], start=(kc == 0), stop=(kc == 7)),
                         ([ea, ed, ab_free[nab % 2]] + w_ev[b]) if kc == 0 else [])
                for kc in range(8):
                    ev = P.op("pe", lambda e, bpb=bpb, kc=kc, fc=fc: e.matmul(bpb[:, 0:BLK], lhsT=w3s[:, kc, fc, :],
                                                                             rhs=hb[:, kc, :], start=(kc == 0), stop=(kc == 7)),
                              [], sig=(True if kc == 7 else None))
                sab = sa[nab % 2]
                e_s = P.op("act", lambda e, sab=sab, apb=apb: e.activation(out=sab[:], in_=apb[:, 0:BLK], func=AF.Silu),
                           [ev, sa_free[nab % 2]], sig=True)
                e_h = P.op("dve", lambda e, sab=sab, bpb=bpb, fc=fc: e.tensor_tensor(out=hd[:, fc, :], in0=sab[:], in1=bpb[:, 0:BLK],
                                                                                    op=ALU.mult),
                           [e_s, ev, hid_free[pb]], sig=True)
                ab_free[nab % 2] = e_h
                sa_free[nab % 2] = e_h
                nab += 1
            hT_free[pb] = ev
            ysb = yst[pb]
            evs = []
            for sub in range(NS):
                for nh in range(2):
                    yp = y_ps[ny % 2]
                    for fc in range(4):
                        ev = P.op("pe", lambda e, yp=yp, fc=fc, sub=sub, nh=nh: e.matmul(
                            yp[:, :], lhsT=hd[:, fc, sub * 128:(sub + 1) * 128], rhs=w2b[pb][:, fc, nh * 512:(nh + 1) * 512],
                            start=(fc == 0), stop=(fc == 3)),
                            [e_h, yps_free[ny % 2]] if fc == 0 else [], sig=(True if fc == 3 else None))
                    dst = ysb[:, sub, nh * 512:(nh + 1) * 512]
                    if ny % 2 == 0:
                        e_y = P.op("act", lambda e, dst=dst, yp=yp: e.activation(out=dst, in_=yp[:, :], func=AF.Copy),
                                   [ev, yst_free[pb]], sig=True)
                    else:
                        e_y = P.op("dve", lambda e, dst=dst, yp=yp: e.tensor_copy(out=dst, in_=yp[:, :]),
                                   [ev, yst_free[pb]], sig=True)
                    yps_free[ny % 2] = e_y
                    evs.append(e_y)
                    ny += 1
            hid_free[pb] = ev
            wb_free[pb] = ev
            yst_free[pb] = P.dma("sp", T["YG_d"][b * BLK:(b + 1) * BLK, :].rearrange("(s p) d -> p s d", p=128), ysb[:],
                                 waits=evs[-2:], sem=s_y[pb])

        for b in range(NBLK):
            do_block(b)


def ph_comb(nc, T, l, R, Xin, Xout, final):
    with Phase(nc, f"cb{l}") as P:
        s_c = P.dsem("c")
        s_x = [P.dsem(f"x{i}") for i in range(3)]
        s_g = [[P.dsem(f"g{i}_{j}") for j in range(2)] for i in range(4)]
        s_o = [P.dsem(f"o{i}") for i in range(2)]
        gt = P.sb("gt", [128, 1024], F32)
        e_g = P.dma1("sp", gt[:], T["mod_d"][l, 5 * 1024:6 * 1024].partition_broadcast(128))
        gfin = None
        if final:
            gfin = P.sb("gfin", [128, 1024], F32)
            e_g2 = P.dma1("sp", gfin[:], T["gfin_row"].ap())
            junk = P.sb("junk", [128, 1024], BF16)
            ss = P.sb("ss", [128, 32], F32)
            sd = P.sb("sd", [128, 32], F32)
            rstd = P.sb("rstd", [128, 32], F32)
            ob = [P.sb(f"ob{i}", [128, 1024], F32) for i in range(2)]
        xt = [P.sb(f"xt{i}", [128, 1024], F32) for i in range(3)]
        ya = [P.sb(f"ya{i}", [128, 1024], F32) for i in range(4)]
        yb = [P.sb(f"yb{i}", [128, 1024], F32) for i in range(4)]
        tm = [P.sb(f"tm{i}", [128, 1024], F32) for i in range(2)]
        xo = [P.sb(f"xo{i}", [128, 1024], F32) for i in range(2)]
        xt_free = [None] * 3
        y_free = [None] * 4
        tm_free = [None] * 2
        xo_free = [None] * 2
        ob_free = [None] * 2
        gath = {}

        def issue_gather(tt):
            q = tt % 4
            evs_ = []
            for j_, (nm, dst) in enumerate((("destA", ya[q]), ("destB", yb[q]))):
                evs_.append(P.op("pool", lambda e, nm=nm, dst=dst: e.indirect_dma_start(
                    out=dst[:, :], out_offset=None, in_=T["YG_d"][:, :],
                    in_offset=bass.IndirectOffsetOnAxis(ap=R[nm][:, tt:tt + 1], axis=0),
                    ), [y_free[q]], sig=s_g[q][j_], inc=16))
            gath[tt] = evs_

        for t0 in range(3):
            issue_gather(t0)
        breg = {}

        def bc(e):
            if "r" not in breg:
                breg["r"] = e.alloc_register("bc_" + P.name)
                e.reg_mov(breg["r"], NBLK * BLK - 1)
            return breg["r"]
        for tt in range(NT):
            p = tt % 2
            xb = xt[tt % 3]
            e_x = P.dma("sp", xb[:], Xin[tt * 128:(tt + 1) * 128, :], waits=[xt_free[tt % 3]], sem=s_x[tt % 3])
            if tt + 3 < NT:
                issue_gather(tt + 3)
            e_gas = gath[tt]
            q = tt % 4
            tb = tm[p]
            P.op("dve", lambda e, tb=tb, q=q, tt=tt: e.tensor_scalar(out=tb[:], in0=ya[q][:], scalar1=R["gA"][:, tt:tt + 1],
                                                                   scalar2=None, op0=ALU.mult), e_gas + [tm_free[p]])
            P.op("dve", lambda e, tb=tb, q=q, tt=tt: e.scalar_tensor_tensor(out=tb[:], in0=yb[q][:], scalar=R["gB"][:, tt:tt + 1],
                                                                          in1=tb[:], op0=ALU.mult, op1=ALU.add))
            e_t = P.op("dve", lambda e, tb=tb: e.tensor_tensor(out=tb[:], in0=tb[:], in1=gt[:], op=ALU.mult), [e_g], sig=True)
            y_free[q] = e_t
            xob = xo[p]
            e_a = P.op("dve", lambda e, tb=tb, xb=xb, xob=xob: e.tensor_tensor(out=xob[:], in0=tb[:], in1=xb[:], op=ALU.add),
                       [e_t, e_x, xo_free[p]], sig=True)
            tm_free[p] = e_a
            xt_free[tt % 3] = e_a
            if not final:
                xo_free[p] = P.dma("sp", Xout[tt * 128:(tt + 1) * 128, :], xob[:], waits=[e_a], sem=s_o[p])
            else:
                e1 = P.op("act", lambda e, xob=xob, tt=tt: e.activation(out=junk[:], in_=xob[:], func=AF.Square,
                                                                        accum_out=ss[:, tt:tt + 1]), [e_a], sig=True)
                e2 = P.op("act", lambda e, tt=tt: e.activation(out=sd[:, tt:tt + 1], in_=ss[:, tt:tt + 1], func=AF.Sqrt,
                                                               bias=EPS, scale=1.0 / D), [], sig=True)
                P.op("dve", lambda e, tt=tt: e.reciprocal(out=rstd[:, tt:tt + 1], in_=sd[:, tt:tt + 1]), [e2])
                obb = ob[p]
                e_o = P.op("dve", lambda e, obb=obb, xob=xob, tt=tt: e.scalar_tensor_tensor(
                    out=obb[:], in0=xob[:], scalar=rstd[:, tt:tt + 1], in1=gfin[:], op0=ALU.mult, op1=ALU.mult),
                    [e_g2, ob_free[p]], sig=True)
                xo_free[p] = e_o
                ob_free[p] = P.dma("sp", Xout[tt * 128:(tt + 1) * 128, :], obb[:], waits=[e_o], sem=s_o[p])


def ph_pool(nc, T, Xin, Xout):
    with Phase(nc, "pl") as P:
        s_c = P.dsem("c")
        s_x = [P.dsem(f"x{i}") for i in range(6)]
        s_o = [P.dsem(f"o{i}") for i in range(2)]
        a_m, b_m, ev_ab = make_ab(P, T, 1, "gmix_col", 0, 1, s_c)
        identf = P.sb("identf", [128, 128], F32)
        wp = P.sb("wp", [128, 4, 2, 256], BF16)
        cg = P.sb("cg", [128, 1024], F32)
        gtr = P.sb("gtr", [128, 1024], F32)
        invfix = P.sb("invfix", [128, 4, 16], F32)
        fx = P.sb("fx", [128, 2, 16], F32)
        xt = [P.sb(f"xt{i}", [128, 1024], F32) for i in range(6)]
        xnf = [P.sb(f"xnf{i}", [128, 1024], F32) for i in range(2)]
        junk = P.sb("junk", [128, 1024], BF16)
        ss = P.sb("ss", [128, 32], F32)
        sd = P.sb("sd", [128, 32], F32)
        rstd = P.sb("rstd", [128, 32], F32)
        hT = [P.sb(f"hT{i}", [128, 8, 528], F32) for i in range(2)]
        S1 = P.sb("S1", [128, 8, 528], F32)
        S2 = P.sb("S2", [128, 6, 528], F32)
        S3 = P.sb("S3", [128, 4, 528], F32)
        S4 = P.sb("S4", [128, 2, 528], F32)
        pl = [P.sb(f"pl{i}", [128, 8, 512], BF16) for i in range(2)]
        t1 = [P.sb(f"t1{i}", [128, 1024], F32) for i in range(2)]
        x3 = [P.sb(f"x3{i}", [128, 1024], F32) for i in range(2)]
        tpf = [P.ps(f"tpf{i}", [128, 8, 128], F32) for i in range(2)]
        y_ps = [P.ps(f"y{i}", [128, 1024]) for i in range(2)]

        e_id = P.dma1("sp", identf[:], T["identf"].ap())
        e_wp = P.dma1("pool", wp[:], T["pool_w"].ap().rearrange("g (cc p) e -> p g cc e", p=128))
        e_a = P.dma1("sp", cg[:], T["pscale_row"].ap())
        e_b = P.dma1("sp", gtr[:], T["mod_d"][1, 2048:3072].partition_broadcast(128))
        e_if = P.dma1("sp", invfix[:], T["invfix"].ap())
        e_cg = P.op("dve", lambda e: e.tensor_tensor(out=cg[:], in0=cg[:], in1=gtr[:], op=ALU.mult), [e_a, e_b], sig=True)

        xt_free = [None] * 6
        xnf_rd = [None] * 2
        tpf_free = [[], []]
        hT_rd = [[], []]
        pl_rd = [None] * 2
        y_free = [None] * 2
        t1_free = [None] * 2
        x3_free = [None] * 2
        x_ev = {}
        halo_ev = [P.op("pool", lambda e: e.memset(hT[0][:, :, 0:16], 0.0), [], sig=True)]

        def super_tile(Tq):
            H = hT[Tq % 2]
            evs_h = []
            def front_sub(sub):
                nonlocal evs_h
                st = Tq * 4 + sub
                xb = xt[st % 6]
                e_x = P.dma("sp", xb[:], Xin[st * 128:(st + 1) * 128, :], waits=[xt_free[st % 6]], sem=s_x[st % 6])
                x_ev[st] = e_x
                e1 = P.op("act", lambda e: e.activation(out=junk[:], in_=xb[:], func=AF.Square, accum_out=ss[:, st:st + 1]),
                          [e_x], sig=True)
                e2 = P.op("act", lambda e: e.activation(out=sd[:, st:st + 1], in_=ss[:, st:st + 1], func=AF.Sqrt,
                                                        bias=EPS, scale=1.0 / D), [], sig=True)
                P.op("dve", lambda e: e.reciprocal(out=rstd[:, st:st + 1], in_=sd[:, st:st + 1]), [e2])
                xf = xnf[st % 2]
                e3 = P.op("dve", lambda e: e.tensor_scalar(out=xf[:], in0=xb[:], scalar1=rstd[:, st:st + 1], scalar2=None,
                                                           op0=ALU.mult), [xnf_rd[st % 2]], sig=True)
                tpb = tpf[st % 2]
                ev = None
                for kc in range(8):
                    ev = P.op("pe", lambda e, kc=kc: e.transpose(tpb[:, kc, :], xf[:, kc * 128:(kc + 1) * 128], identf[:]),
                              ([e3, e_id] + tpf_free[st % 2]) if kc == 0 else [], sig=(True if kc == 7 else None))
                xnf_rd[st % 2] = ev
                ea = ed = None
                for kc in range(8):
                    dst = H[:, kc, 16 + sub * 128:16 + (sub + 1) * 128]
                    ed = P.op("dve", lambda e, kc=kc, dst=dst: e.tensor_scalar(out=dst, in0=tpb[:, kc, :],
                                                                              scalar1=a_m[:, kc:kc + 1], scalar2=b_m[:, kc:kc + 1],
                                                                              op0=ALU.mult, op1=ALU.add),
                              [ev, ev_ab] + hT_rd[Tq % 2], sig=True)
                    ea = ed
                tpf_free[st % 2] = [ea, ed]
                evs_h = [ea, ed]

            for sub in range(4):
                front_sub(sub)
            P.op("dve", lambda e: e.tensor_tensor(out=S1[:, :, 1:528], in0=H[:, :, 1:528], in1=H[:, :, 0:527], op=ALU.add),
                 evs_h + [halo_ev[0]])
            e_s1 = P.op("dve", lambda e: e.tensor_tensor(out=S2[:, :, 3:528], in0=S1[:, 2:8, 3:528], in1=S1[:, 2:8, 1:526],
                                                         op=ALU.add), [], sig=True)
            P.op("pool", lambda e: e.tensor_tensor(out=S3[:, :, 7:528], in0=S2[:, 2:6, 7:528], in1=S2[:, 2:6, 3:524], op=ALU.add),
                 [e_s1])
            e_s4 = P.op("pool", lambda e: e.tensor_tensor(out=S4[:, :, 15:528], in0=S3[:, 2:4, 15:528], in1=S3[:, 2:4, 7:520],
                                                          op=ALU.add), [], sig=True)
            plb = pl[Tq % 2]
            srcs = [(S1, 0, 0.5), (S2, 0, 0.25), (S3, 0, 0.125), (S4, 0, 0.0625)]
            e_p = None
            for g in range(4):
                Sx, c0, sc_ = srcs[g]
                e_p = P.op("dve", lambda e, g=g, Sx=Sx, sc_=sc_: e.scalar_tensor_tensor(
                    out=plb[:, 2 * g:2 * g + 2, :], in0=Sx[:, 0:2, 16:528], scalar=sc_, in1=H[:, 2 * g:2 * g + 2, 16:528],
                    op0=ALU.mult, op1=ALU.subtract), [e_s4, pl_rd[Tq % 2]], sig=True)
                if Tq == 0:
                    P.op("dve", lambda e, g=g, Sx=Sx: e.tensor_tensor(
                        out=fx[:], in0=Sx[:, 0:2, 16:32], in1=invfix[:, g, :].unsqueeze(1).to_broadcast([128, 2, 16]),
                        op=ALU.mult), [e_if])
                    e_p = P.op("dve", lambda e, g=g: e.tensor_tensor(out=plb[:, 2 * g:2 * g + 2, 0:16], in0=fx[:],
                                                                    in1=H[:, 2 * g:2 * g + 2, 16:32], op=ALU.subtract),
                               [], sig=True)
            if Tq + 1 < 8:
                Hn = hT[(Tq + 1) % 2]
                halo_ev[0] = P.op("pool", lambda e: e.tensor_copy(out=Hn[:, :, 0:16], in_=H[:, :, 512:528]),
                                  evs_h + hT_rd[(Tq + 1) % 2], sig=True)
                hT_rd[Tq % 2] = [e_p, halo_ev[0]]
            else:
                hT_rd[Tq % 2] = [e_p]
            def back_sub(sub):
                st = Tq * 4 + sub
                yp = y_ps[st % 2]
                ev = None
                for g in range(4):
                    for cc in range(2):
                        ev = P.op("pe", lambda e, g=g, cc=cc: e.matmul(yp[:, g * 256:(g + 1) * 256],
                                                                      lhsT=plb[:, 2 * g + cc, sub * 128:(sub + 1) * 128],
                                                                      rhs=wp[:, g, cc, :], start=(cc == 0), stop=(cc == 1)),
                                  [e_p, e_wp, y_free[st % 2]] if (g == 0 and cc == 0) else [],
                                  sig=(True if (g == 3 and cc == 1) else None))
                if sub == 3:
                    pl_rd[Tq % 2] = ev
                tb = t1[st % 2]
                e_t = P.op("dve", lambda e: e.tensor_tensor(out=tb[:], in0=yp[:, :], in1=cg[:], op=ALU.mult),
                           [ev, e_cg, t1_free[st % 2]], sig=True)
                y_free[st % 2] = e_t
                xb = xt[st % 6]
                xo = x3[st % 2]
                e_a2 = P.op("pool", lambda e: e.tensor_tensor(out=xo[:], in0=tb[:], in1=xb[:], op=ALU.add),
                            [e_t, x_ev[st], x3_free[st % 2]], sig=True)
                t1_free[st % 2] = e_a2
                xt_free[st % 6] = e_a2
                x3_free[st % 2] = P.dma("sp", Xout[st * 128:(st + 1) * 128, :], xo[:], waits=[e_a2], sem=s_o[st % 2])

            for sub in range(4):
                back_sub(sub)

        for Tq in range(8):
            super_tile(Tq)


IN_SPECS = {
    "x": ([S, D], F32), "c_col": ([128, 8], F32), "ada_w": ([2, D, 6 * D], F32), "ada_b": ([2, 6 * D], F32),
    "gmix_col": ([128, 2, 8], F32), "gffn_col": ([128, 2, 8], F32), "w_in": ([D, 3 * D + NH], F32),
    "fb_col": ([NH, 1], F32), "w_o": ([D, D], F32), "pool_w": ([4, 256, 256], F32), "pscale_row": ([128, D], F32),
    "rw": ([128, 8, E], F32), "rbias_row": ([128, E], F32),
    "w1_0": ([E, D, FF], F32), "w1_1": ([E, D, FF], F32), "w3_0": ([E, D, FF], F32), "w3_1": ([E, D, FF], F32),
    "w2_0": ([E, FF, D], F32), "w2_1": ([E, FF, D], F32), "gfin_row": ([128, D], F32),
    "ident": ([128, 128], F32), "identf": ([128, 128], F32), "mtri": ([128, 128], F32), "ltri": ([128, 128], F32),
    "ones": ([128, 128], F32), "thr": ([128, 16], F32), "biota": ([128, 64], F32), "invfix": ([128, 4, 16], F32),
    "piota": ([128, 1], F32), "gffn_pk": ([128, 2, 8], F32),
}
SCRATCH = {
    "mod_d": ([2, 6 * D], F32), "QT_d": ([D, S], BF16), "KT_d": ([D, S], BF16), "V_d": ([NH, 128, NT, 64], BF16),
    "FL_d": ([NH, S], F32), "QF_d": ([NH, 3, S], BF16), "KF_d": ([NH, 3, S], BF16),
    "X1_d": ([S, D], F32), "X2_d": ([S, D], F32), "X3_d": ([S, D], F32),
    "XG_d": ([NBLK * BLK, D], BF16), "YG_d": ([NBLK * BLK, D], F32),
}


def build(upto=99, debug=(), only_inputs=None):
    nc = bass.Bass("TRN2", target_bir_lowering=False)
    T = {}
    for k, (shp, dt) in IN_SPECS.items():
        if only_inputs is not None and k not in only_inputs:
            continue
        T[k] = nc.dram_tensor(k, shp, dt, kind="ExternalInput")
    for k, (shp, dt) in SCRATCH.items():
        if k in DBG.get("as_input", ()):
            continue
        T[k] = nc.dram_tensor(k, shp, dt, kind=("ExternalOutput" if k in debug else "Internal"))
    T["out"] = nc.dram_tensor("out", [S, D], F32, kind="ExternalOutput")
    if "Ri_dbg" in debug:
        T["Ri_dbg"] = nc.dram_tensor("Ri_dbg", [2, 128, 32], I32, kind="ExternalOutput")
        T["Rf_dbg"] = nc.dram_tensor("Rf_dbg", [3, 128, 64], F32, kind="ExternalOutput")
    for k in DBG.get("as_input", ()):
        shp, dt = SCRATCH[k]
        T[k] = nc.dram_tensor(k, shp, dt, kind="ExternalInput")
    with ExitStack() as es:
        ph_mod(nc, T)
        if upto < 1:
            return nc
        if not DBG.get("skip_l0mix"):
            ph_qkv(nc, T)
            if not DBG.get("no_fgate"):
                ph_fgate(nc, T)
            if upto < 2:
                return nc
            with nc.sbuf_tensor("OT_all", [128, 8, S], BF16) as OT_all:
                ph_attn(nc, T, OT_all)
                ph_oproj(nc, T, OT_all)
        if upto < 3:
            return nc
        R = {
            "destA": es.enter_context(nc.sbuf_tensor("R_destA", [128, 32], I32)),
            "destB": es.enter_context(nc.sbuf_tensor("R_destB", [128, 32], I32)),
            "gA": es.enter_context(nc.sbuf_tensor("R_gA", [128, 32], F32)),
            "gB": es.enter_context(nc.sbuf_tensor("R_gB", [128, 32], F32)),
            "blk_e": es.enter_context(nc.sbuf_tensor("R_blk_e", [128, 64], F32)),
            "idxW": es.enter_context(nc.sbuf_tensor("R_idxW", [128, 64], I32)),
        }
        if "X2_d" not in DBG.get("as_input", ()) and "X3_d" not in DBG.get("as_input", ()):
            ph_front(nc, T, 0, R, T["X1_d"])
            if upto < 4:
                return nc
            ph_moe(nc, T, 0, R)
            ph_comb(nc, T, 0, R, T["X1_d"], T["X2_d"], final=False)
        if upto < 5:
            return nc
        if "X3_d" not in DBG.get("as_input", ()):
            ph_pool(nc, T, T["X2_d"], T["X3_d"])
        if upto < 6:
            return nc
        ph_front(nc, T, 1, R, T["X3_d"])
        ph_moe(nc, T, 1, R)
        ph_comb(nc, T, 1, R, T["X3_d"], T["out"], final=True)
    return nc


def _col(v):
    return np.ascontiguousarray(np.asarray(v, np.float32).reshape(8, 128).T)


def make_in_maps(inp):
    f = lambda a: np.ascontiguousarray(np.asarray(a, np.float32))
    shared = {
        "ada_w": f(inp["ada_w"]), "ada_b": f(inp["ada_b"]),
        "gmix_col": np.ascontiguousarray(np.stack([_col(inp["norm_mix_g"][l]) for l in range(2)], axis=1)),
        "gffn_col": np.ascontiguousarray(np.stack([_col(inp["norm_ffn_g"][l]) for l in range(2)], axis=1)),
        "w_in": f(inp["attn_w_in"][0]), "fb_col": f(inp["attn_f_bias"][0]).reshape(NH, 1), "w_o": f(inp["attn_w_o"][0]),
        "pool_w": f(inp["pool_w"][0]), "pscale_row": np.ascontiguousarray(np.broadcast_to(f(inp["pool_scale"][0]), (128, D))),
        "rw": np.ascontiguousarray(f(inp["router_w"]).reshape(8, 128, E).transpose(1, 0, 2)),
        "rbias_row": np.ascontiguousarray(np.broadcast_to(f(inp["router_bias"]), (128, E))),
        "w1_0": f(inp["exp_w1"][0]), "w1_1": f(inp["exp_w1"][1]), "w3_0": f(inp["exp_w3"][0]), "w3_1": f(inp["exp_w3"][1]),
        "w2_0": f(inp["exp_w2"][0]), "w2_1": f(inp["exp_w2"][1]),
        "gfin_row": np.ascontiguousarray(np.broadcast_to(f(inp["norm_final_g"]), (128, D))),
    }
    ii = np.arange(128)
    shared["ident"] = np.eye(128, dtype=np.float32)
    shared["identf"] = np.eye(128, dtype=np.float32)
    shared["mtri"] = np.where(ii[None, :] >= ii[:, None], 0.0, MASKV).astype(np.float32)
    shared["ltri"] = (ii[:, None] < ii[None, :]).astype(np.float32)
    shared["ones"] = np.ones((128, 128), np.float32)
    shared["thr"] = np.ascontiguousarray(np.broadcast_to((np.arange(16) * BLK).astype(np.float32), (128, 16)))
    shared["biota"] = np.ascontiguousarray(np.broadcast_to(np.arange(64).astype(np.float32), (128, 64)))
    fix = np.zeros((4, 16), np.float32)
    for g, w in enumerate((2, 4, 8, 16)):
        fix[g] = 1.0 / np.minimum(np.arange(16) + 1, w)
    shared["invfix"] = np.ascontiguousarray(np.broadcast_to(fix, (128, 4, 16)))
    shared["piota"] = np.arange(128, dtype=np.float32).reshape(128, 1)
    shared["gffn_pk"] = np.ascontiguousarray(np.stack([f(inp["norm_ffn_g"][l]).reshape(128, 8) for l in range(2)], axis=1))
    maps = []
    for b in range(8):
        m = dict(shared)
        m["x"] = f(inp["x"][b])
        m["c_col"] = _col(inp["c"][b])
        maps.append(m)
    return maps


_NC_CACHE = {}


def kernel(**inputs):
    if "nc" not in _NC_CACHE:
        _NC_CACHE["nc"] = build()
    nc = _NC_CACHE["nc"]
    in_maps = make_in_maps(inputs)
    res = run_bass_kernel_spmd(nc, in_maps, core_ids=list(range(8)))
    return np.stack([np.asarray(r["out"], np.float32) for r in res.results], axis=0)
```

```python
import numpy as np
from contextlib import ExitStack
import concourse.bass as bass
import concourse.mybir as mybir
from concourse.bass_utils import run_bass_kernel_spmd

F32 = mybir.dt.float32
BF16 = mybir.dt.bfloat16
I32 = mybir.dt.int32
ALU = mybir.AluOpType
AF = mybir.ActivationFunctionType
AX = mybir.AxisListType

S = 4096
D = 1024
NT = S // 128
NH = 16
E = 32
FF = 512
BLK = 256
NBLK = 2 * S // BLK + E
EPS = 1e-6
MASKV = -30000.0
DBG = {}


class SemC:
    def __init__(self, h):
        self.h = h
        self.n = 0


class Phase:
    ENGS = ("pe", "act", "dve", "pool", "sp")

    def __init__(self, nc, name):
        self.nc = nc
        self.name = name
        self.es = ExitStack()
        self.q = {e: [] for e in self.ENGS}
        self.dsems = []
        self.allsems = []
        self.last = {}

    def __enter__(self):
        self.es.__enter__()
        self.esem = {e: self.sem("e_" + e) for e in self.ENGS[:4]}
        return self

    def sem(self, nm):
        sc = SemC(self.es.enter_context(self.nc.semaphore(f"{self.name}_{nm}")))
        self.allsems.append(sc)
        return sc

    def dsem(self, nm):
        s = self.sem(nm)
        self.dsems.append(s)
        return s

    def sb(self, nm, shape, dt):
        return self.es.enter_context(self.nc.sbuf_tensor(f"{self.name}_{nm}", list(shape), dt))

    def ps(self, nm, shape, dt=F32):
        return self.es.enter_context(self.nc.psum_tensor(f"{self.name}_{nm}", list(shape), dt))

    def op(self, eng, fn, waits=(), sig=None, inc=1, chain=True):
        is_compute = eng in ("act", "dve", "pool") and inc == 1
        if is_compute:
            sig = self.esem[eng]
            if chain and self.last.get(eng) is not None:
                waits = list(waits) + [self.last[eng]]
        if sig is True:
            sig = self.esem[eng]
        ev = None
        if sig is not None:
            sig.n += inc
            ev = (sig, sig.n)
        self.q[eng].append((fn, self._collapse(waits), sig, inc))
        if is_compute:
            self.last[eng] = ev
        return ev

    @staticmethod
    def _collapse(waits):
        best = {}
        for w in waits:
            if w is None:
                continue
            s_, v = w
            if id(s_) not in best or best[id(s_)][1] < v:
                best[id(s_)] = (s_, v)
        return list(best.values())

    def dma(self, eng, out, in_, waits=(), sem=None, **kw):
        return self.op(eng, lambda e: e.dma_start(out=out, in_=in_, **kw), waits, sig=sem, inc=16)

    def dma1(self, eng, out, in_, waits=(), **kw):
        self._n1 = getattr(self, "_n1", 0) + 1
        return self.dma(eng, out, in_, waits, sem=self.dsem(f"u{self._n1}"), **kw)

    def wait(self, eng, waits):
        self.q[eng].append((None, self._collapse(waits), None, 0))

    def __exit__(self, *a):
        if a[0] is None:
            self.wait("sp", [(s, s.n) for s in self.dsems if s.n > 0])
            self.run()
            with self.nc.Block() as blk2:
                def clr(e):
                    e.dma_reset()
                    for sc in self.allsems:
                        if sc.n > 0:
                            e.sem_clear(sc.h)
                blk2.gpsimd(clr)
        return self.es.__exit__(*a)

    def run(self):
        nc = self.nc
        with nc.Block() as blk:
            def mk(engname):
                def body(e):
                    waited = {}
                    own = self.esem.get(engname)
                    for fn, waits, sig, inc in self.q[engname]:
                        for (s, v) in waits:
                            if waited.get(id(s), 0) >= v:
                                continue
                            e.wait_ge(s.h, v)
                            waited[id(s)] = v
                        if fn is None:
                            continue
                        ins = fn(e)
                        if sig is not None:
                            ins.then_inc(sig.h, inc)
                return body
            blk.tensor(mk("pe"))
            blk.scalar(mk("act"))
            blk.vector(mk("dve"))
            blk.gpsimd(mk("pool"))
            blk.sync(mk("sp"))


def ph_mod(nc, T):
    with Phase(nc, "mod") as P:
        ccol = P.sb("ccol", [128, 8], F32)
        cact = P.sb("cact", [128, 8], F32)
        bias = [P.sb(f"bias{l}", [1, 6144], F32) for l in range(2)]
        row = [P.sb(f"row{l}", [1, 6144], F32) for l in range(2)]
        wb = [P.sb(f"wb{i}", [128, 8, 512], F32) for i in range(3)]
        pst = [P.ps(f"ps{i}", [128, 512]) for i in range(2)]
        s_w = [P.dsem(f"w{i}") for i in range(3)]
        s_out = P.dsem("out")
        ev_c = P.dma1("sp", ccol[:], T["c_col"].ap())
        ev_b = [P.dma1("sp", bias[l][:], T["ada_b"][l:l + 1, :]) for l in range(2)]
        ev_act = P.op("act", lambda e: e.activation(out=cact[:], in_=ccol[:], func=AF.Silu), [ev_c], sig=True)
        pe_done = [None] * 3
        dve_done = [None] * 2
        n = 0
        for l in range(2):
            for nt in range(12):
                k = n % 3
                pp = n % 2
                src = T["ada_w"][l:l + 1, :, nt * 512:(nt + 1) * 512].rearrange("o (kc p) n -> p (o kc) n", p=128)
                ev_w = P.dma("sp" if n % 2 == 0 else "act", wb[k][:], src, waits=[pe_done[k]], sem=s_w[k])
                ev = None
                for kc in range(8):
                    ev = P.op("pe",
                              lambda e, kc=kc, k=k, pp=pp: e.matmul(pst[pp][0:1, :], lhsT=cact[:, kc:kc + 1],
                                                                   rhs=wb[k][:, kc, :], start=(kc == 0), stop=(kc == 7)),
                              waits=[ev_w, ev_act, dve_done[pp]] if kc == 0 else [],
                              sig=(True if kc == 7 else None))
                pe_done[k] = ev
                dve_done[pp] = P.op("dve",
                                    lambda e, l=l, nt=nt, pp=pp: e.tensor_tensor(
                                        out=row[l][0:1, nt * 512:(nt + 1) * 512], in0=pst[pp][0:1, :],
                                        in1=bias[l][0:1, nt * 512:(nt + 1) * 512], op=ALU.add),
                                    [ev, ev_b[l]], sig=True)
                n += 1
            P.dma("sp", T["mod_d"][l:l + 1, :], row[l][:], waits=[dve_done[(n - 1) % 2]], sem=s_out)


def load_mod_cols(P, T, l, which, sem):
    t = P.sb(f"mc_{l}_{which}", [128, 8], F32)
    src = T["mod_d"][l, which * 1024:(which + 1) * 1024].rearrange("(k p) -> p k", p=128)
    ev = P.dma("sp", t[:], src, sem=sem, allow_slow_non_contiguous=True)
    return t, ev


def make_ab(P, T, l, gname, sh_idx, sc_idx, sem=None):
    sem = P.dsem(f"ab_{gname}{l}")
    sh, ev1 = load_mod_cols(P, T, l, sh_idx, sem)
    sc, ev2 = load_mod_cols(P, T, l, sc_idx, sem)
    g = P.sb(f"g_{gname}{l}", [128, 8], F32)
    ev3 = P.dma("sp", g[:], T[gname][:, l, :], sem=sem)
    a = P.sb(f"a_{gname}{l}", [128, 8], F32)
    ev = P.op("dve", lambda e: e.scalar_tensor_tensor(out=a[:], in0=sc[:], scalar=1.0, in1=g[:],
                                                      op0=ALU.add, op1=ALU.mult), [ev1, ev2, ev3], sig=True)
    return a, sh, ev


def ph_qkv(nc, T):
    with Phase(nc, "qkv") as P:
        w_bf = P.sb("w_bf", [128, 8, 3088], BF16)
        ident = P.sb("ident", [128, 128], BF16)
        xt = [P.sb(f"xt{i}", [128, 1024], F32) for i in range(3)]
        xn = [P.sb(f"xn{i}", [128, 1024], BF16) for i in range(4)]
        junk = P.sb("junk", [128, 1024], BF16)
        ss = P.sb("ss", [128, 32], F32)
        sd = P.sb("sd", [128, 32], F32)
        rstd = P.sb("rstd", [128, 32], F32)
        hT = [P.sb(f"hT{i}", [128, 8, 512], BF16) for i in range(2)]
        qk_st = [P.sb(f"qkst{i}", [128, 512], BF16) for i in range(4)]
        v_st = [P.sb(f"vst{i}", [128, 1024], BF16) for i in range(2)]
        fl = P.sb("fl", [16, 4096], F32)
        tp = [P.ps(f"tp{i}", [128, 8, 128], BF16) for i in range(2)]
        ps_qk = [P.ps(f"psqk{i}", [128, 512]) for i in range(2)]
        ps_v = [P.ps(f"psv{i}", [128, 512]) for i in range(2)]
        ps_f = P.ps("psf", [128, 512])
        s_c = P.dsem("c")
        s_w = P.dsem("w")
        s_x = [P.dsem(f"x{i}") for i in range(3)]
        s_qk = [P.dsem(f"qk{i}") for i in range(4)]
        s_v = [P.dsem(f"v{i}") for i in range(2)]
        s_f = P.dsem("f")

        a_m, b_m, ev_ab = make_ab(P, T, 0, "gmix_col", 0, 1, s_c)
        ev_id = P.dma1("pool", ident[:], T["ident"].ap())
        wsrc = T["w_in"].ap().rearrange("(kc p) n -> p kc n", p=128)
        ev_ws = []
        for kc in range(8):
            ev_ws.append(P.dma1("pool", w_bf[:, kc, :], wsrc[:, kc, :]))

        xt_free = [[] for _ in range(3)]
        xn_rd = [None] * 4
        hT_free = [None] * 2
        hT_ready = {}
        xn_ready = {}

        def A(Tq):
            for sub in range(4):
                st = Tq * 4 + sub
                xb = xt[st % 3]
                ev_x = P.dma("sp", xb[:], T["x"][st * 128:(st + 1) * 128, :], waits=xt_free[st % 3], sem=s_x[st % 3])
                e1 = P.op("act", lambda e, xb=xb, st=st: e.activation(out=junk[:], in_=xb[:], func=AF.Square,
                                                                      accum_out=ss[:, st:st + 1]), [ev_x], sig=True)
                e2 = P.op("act", lambda e, st=st: e.activation(out=sd[:, st:st + 1], in_=ss[:, st:st + 1], func=AF.Sqrt,
                                                               bias=EPS, scale=1.0 / D), [], sig=True)
                P.op("dve", lambda e, st=st: e.reciprocal(out=rstd[:, st:st + 1], in_=sd[:, st:st + 1]), [e2])
                xb2 = xn[st % 4]
                e3 = P.op("dve", lambda e, xb=xb, xb2=xb2, st=st: e.tensor_scalar(
                    out=xb2[:], in0=xb[:], scalar1=rstd[:, st:st + 1], scalar2=None, op0=ALU.mult),
                    [xn_rd[st % 4]], sig=True)
                xt_free[st % 3] = [e1, e3]
                xn_ready[st] = e3

        tp_free_evs = [[], []]
        ps_qk_free = [None] * 2
        ps_v_free = [None] * 2
        ps_f_free = [None]
        qk_st_free = [None] * 4
        v_st_free = [None] * 2
        cnt = {"qk": 0, "v": 0}

        def C(Tq, part):
            hb = hT[Tq % 2]
            rdy = hT_ready[Tq]
            ev = None
            for oc in (range(16) if part == 1 else ()):
                n = cnt["qk"]
                cnt["qk"] += 1
                pb = ps_qk[n % 2]
                for kc in range(8):
                    ev = P.op("pe", lambda e, pb=pb, kc=kc, oc=oc: e.matmul(
                        pb[:, :], lhsT=w_bf[:, kc, oc * 128:(oc + 1) * 128], rhs=hb[:, kc, :],
                        start=(kc == 0), stop=(kc == 7)),
                        (list(rdy) + ev_ws + [ps_qk_free[n % 2]]) if kc == 0 else [], sig=(True if kc == 7 else None))
                stb = qk_st[n % 4]
                if n % 2 == 0:
                    ev2 = P.op("act", lambda e, stb=stb, pb=pb: e.activation(out=stb[:], in_=pb[:, :], func=AF.Copy),
                               [ev, qk_st_free[n % 4]], sig=True)
                else:
                    ev2 = P.op("dve", lambda e, stb=stb, pb=pb: e.tensor_copy(out=stb[:], in_=pb[:, :]),
                               [ev, qk_st_free[n % 4]], sig=True)
                ps_qk_free[n % 2] = ev2
                dst_t = T["QT_d"] if oc < 8 else T["KT_d"]
                r0 = (oc % 8) * 128
                qk_st_free[n % 4] = P.dma("sp", dst_t[r0:r0 + 128, Tq * 512:(Tq + 1) * 512], stb[:], waits=[ev2],
                                          sem=s_qk[n % 4])
            if part == 1:
                return
            for sub in range(4):
                vb = v_st[sub % 2]
                evs = []
                for nh in range(2):
                    n = cnt["v"]
                    cnt["v"] += 1
                    pb = ps_v[n % 2]
                    for kc in range(8):
                        ev = P.op("pe", lambda e, pb=pb, kc=kc, nh=nh, sub=sub: e.matmul(
                            pb[:, :], lhsT=hb[:, kc, sub * 128:(sub + 1) * 128],
                            rhs=w_bf[:, kc, 2048 + nh * 512:2048 + (nh + 1) * 512], start=(kc == 0), stop=(kc == 7)),
                            [ps_v_free[n % 2]] if kc == 0 else [], sig=(True if kc == 7 else None))
                    if nh == 0:
                        ev2 = P.op("act", lambda e, vb=vb, pb=pb: e.activation(out=vb[:, 0:512], in_=pb[:, :], func=AF.Copy),
                                   [ev, v_st_free[sub % 2]], sig=True)
                    else:
                        ev2 = P.op("act", lambda e, vb=vb, pb=pb: e.activation(out=vb[:, 512:1024], in_=pb[:, :], func=AF.Copy),
                                   [ev, v_st_free[sub % 2]], sig=True)
                    ps_v_free[n % 2] = ev2
                    evs.append(ev2)
                kt = Tq * 4 + sub
                v_st_free[sub % 2] = P.dma(
                    "sp", T["V_d"][:, :, kt, :].rearrange("h p d -> p h d"),
                    vb[:].rearrange("p (h d) -> p h d", h=16), waits=evs, sem=s_v[sub % 2])
            for kc in range(8):
                ev = P.op("pe", lambda e, kc=kc: e.matmul(ps_f[0:16, :], lhsT=w_bf[:, kc, 3072:3088], rhs=hb[:, kc, :],
                                                          start=(kc == 0), stop=(kc == 7)),
                          [ps_f_free[0]] if kc == 0 else [], sig=(True if kc == 7 else None))
            hT_free[Tq % 2] = ev
            ps_f_free[0] = P.op("dve", lambda e: e.tensor_copy(out=fl[:, Tq * 512:(Tq + 1) * 512], in_=ps_f[0:16, :]),
                                [ev], sig=True)

        def B2(Tq):
            hb = hT[Tq % 2]
            last = []
            for sub in range(4):
                st = Tq * 4 + sub
                tpb = tp[st % 2]
                ev = None
                for kc in range(8):
                    ev = P.op("pe", lambda e, tpb=tpb, kc=kc, xb2=xn[st % 4]: e.transpose(
                        tpb[:, kc, :], xb2[:, kc * 128:(kc + 1) * 128], ident[:]),
                        ([xn_ready[st], ev_id] + tp_free_evs[st % 2]) if kc == 0 else [],
                        sig=(True if kc == 7 else None))
                xn_rd[st % 4] = ev
                ea = ed = None
                for kc in range(8):
                    dst = hb[:, kc, sub * 128:(sub + 1) * 128]
                    ed = P.op("dve", lambda e, dst=dst, tpb=tpb, kc=kc: e.tensor_scalar(
                        out=dst, in0=tpb[:, kc, :], scalar1=a_m[:, kc:kc + 1], scalar2=b_m[:, kc:kc + 1],
                        op0=ALU.mult, op1=ALU.add), [ev, ev_ab, hT_free[Tq % 2]], sig=True)
                    ea = ed
                tp_free_evs[st % 2] = [ea, ed]
                last = [ea, ed]
            hT_ready[Tq] = last

        A(0)
        if not DBG.get("qkv_noB"):
            B2(0)
        for Tq in range(8):
            if Tq + 1 < 8:
                A(Tq + 1)
            C(Tq, 1)
            if Tq + 1 < 8:
                B2(Tq + 1)
            C(Tq, 2)
        if not DBG.get("qkv_noC") and not DBG.get("qkv_noB"):
            P.dma("sp", T["FL_d"].ap(), fl[:], waits=[ps_f_free[0]], sem=s_f)


def ph_fgate(nc, T):
    with Phase(nc, "fg") as P:
        fl = P.sb("fl", [16, 4096], F32)
        u = P.sb("u", [16, 4096], F32)
        lf = P.sb("lf", [16, 4096], F32)
        ones = P.sb("ones", [16, 4096], F32)
        G = P.sb("G", [16, 4096], F32)
        fb = P.sb("fb", [16, 1], F32)
        nfb = P.sb("nfb", [16, 1], F32)
        parts = [P.sb(f"part{i}", [16, 4096], BF16) for i in range(3)]
        nparts = [P.sb(f"npart{i}", [16, 4096], BF16) for i in range(3)]
        s_in = P.dsem("in")
        s_out = P.dsem("out")
        e_fl = P.dma1("sp", fl[:], T["FL_d"].ap())
        e_fb = P.dma1("sp", fb[:], T["fb_col"].ap())
        e0 = P.op("dve", lambda e: e.tensor_scalar(out=nfb[:], in0=fb[:], scalar1=-1.0, scalar2=None, op0=ALU.mult),
                  [e_fb], sig=True)
        P.op("dve", lambda e: e.memset(ones[:], 1.0))
        e0b = P.op("dve", lambda e: e.tensor_scalar(out=fl[:], in0=fl[:], scalar1=fb[:, 0:1], scalar2=None, op0=ALU.add),
                   [e_fl, e_fb], sig=True)
        e1 = P.op("act", lambda e: e.activation(out=u[:], in_=fl[:], func=AF.Exp, scale=-1.0), [e0b], sig=True)
        e2 = P.op("act", lambda e: e.activation(out=lf[:], in_=u[:], func=AF.Ln, bias=1.0, scale=1.0), [], sig=True)
        P.op("dve", lambda e: e.tensor_tensor_scan(out=G[:], data0=ones[:], data1=lf[:], initial=0.0,
                                                   op0=ALU.mult, op1=ALU.add), [e2])
        P.op("dve", lambda e: e.tensor_scalar(out=u[:], in0=G[:], scalar1=8.0, scalar2=None, op0=ALU.mult))
        P.op("dve", lambda e: e.tensor_copy(out=parts[0][:], in_=u[:]))
        P.op("dve", lambda e: e.tensor_tensor(out=lf[:], in0=u[:], in1=parts[0][:], op=ALU.subtract))
        P.op("dve", lambda e: e.tensor_copy(out=parts[1][:], in_=lf[:]))
        P.op("dve", lambda e: e.tensor_tensor(out=u[:], in0=lf[:], in1=parts[1][:], op=ALU.subtract))
        P.op("dve", lambda e: e.tensor_copy(out=parts[2][:], in_=u[:]))
        ev = None
        for i in range(3):
            ev = P.op("dve", lambda e, i=i: e.tensor_scalar(out=nparts[i][:], in0=parts[i][:], scalar1=-1.0,
                                                            scalar2=None, op0=ALU.mult), [], sig=True)
        for i in range(3):
            P.dma("sp", T["KF_d"][:, i, :], parts[i][:], waits=[ev], sem=s_out)
            P.dma("sp", T["QF_d"][:, i, :], nparts[i][:], waits=[ev], sem=s_out)


def ph_attn(nc, T, OT_all):
    with Phase(nc, "att") as P:
        Qa = [P.sb(f"Qa{i}", [70, 4096], BF16) for i in range(2)]
        Ka = [P.sb(f"Ka{i}", [70, 4096], BF16) for i in range(2)]
        Va = [P.sb(f"Va{i}", [128, 32, 128], BF16) for i in range(2)]
        Pt = [P.sb(f"Pt{i}", [128, 512], BF16) for i in range(3)]
        rec = [P.sb(f"rec{i}", [128, 512], F32) for i in range(2)]
        ident = P.sb("ident", [128, 128], BF16)
        mtri = P.sb("mtri", [128, 128], BF16)
        s_ps = [P.ps(f"s{i}", [128, 512]) for i in range(3)]
        o_ps = [P.ps(f"o{i}", [128, 512]) for i in range(2)]
        s_c = P.dsem("c")
        s_h = [P.dsem(f"h{i}") for i in range(2)]
        e_id = P.dma1("pool", ident[:], T["ident"].ap())
        e_mt = P.dma1("pool", mtri[:], T["mtri"].ap())
        e_ms = None
        for i in range(2):
            P.op("dve", lambda e, i=i: e.memset(Qa[i][64:70, :], 1.0))
            P.op("dve", lambda e, i=i: e.memset(Ka[i][64:70, :], 1.0))
            e_ms = P.op("dve", lambda e, i=i: e.memset(Va[i][:, :, 64:128], 1.0), [], sig=True)

        head_done = [None] * NH

        def load_head(h):
            p = h % 2
            w = [e_ms, head_done[h - 2] if h >= 2 else None]
            P.dma("sp", Qa[p][0:64, :], T["QT_d"][h * 64:(h + 1) * 64, :], waits=w, sem=s_h[p])
            P.dma("sp", Qa[p][64:67, :], T["QF_d"][h, :, :], sem=s_h[p])
            P.dma("sp", Ka[p][0:64, :], T["KT_d"][h * 64:(h + 1) * 64, :], sem=s_h[p])
            P.dma("sp", Ka[p][67:70, :], T["KF_d"][h, :, :], sem=s_h[p])
            return P.dma("sp", Va[p][:, :, 0:64], T["V_d"][h, :, :, :], sem=s_h[p])

        tiles = []
        for h in range(NH):
            for j in range(8):
                for i in range(4 * j + 4):
                    tiles.append((h, j, i))
        NTL = len(tiles)
        head_ev = {0: load_head(0), 1: load_head(1)}
        exp_ev = [None] * NTL
        pv_ev = {}
        s_ev = [None] * NTL
        norm_ev = {}
        last_pv = {}

        def rec_S(n):
            h, j, i = tiles[n]
            p = h % 2
            sb_ = s_ps[n % 3]
            r = i - 4 * j
            w = [head_ev[h], exp_ev[n - 3] if n >= 3 else None, e_id, e_mt]
            if r < 0:
                s_ev[n] = P.op("pe", lambda e: e.matmul(sb_[:, :], lhsT=Ka[p][0:70, i * 128:(i + 1) * 128],
                                                        rhs=Qa[p][0:70, j * 512:(j + 1) * 512], start=True, stop=True),
                               w, sig=True)
            else:
                c0 = 128 * r
                P.op("pe", lambda e: e.matmul(sb_[:, c0:c0 + 128], lhsT=ident[:], rhs=mtri[:], start=True, stop=False), w)
                ev = P.op("pe", lambda e: e.matmul(sb_[:, c0:c0 + 128], lhsT=Ka[p][0:70, i * 128:(i + 1) * 128],
                                                   rhs=Qa[p][0:70, j * 512 + c0:j * 512 + c0 + 128], start=False, stop=True),
                          [], sig=(True if r == 3 else None))
                if r < 3:
                    ev = P.op("pe", lambda e: e.matmul(sb_[:, c0 + 128:512], lhsT=Ka[p][0:70, i * 128:(i + 1) * 128],
                                                       rhs=Qa[p][0:70, j * 512 + c0 + 128:(j + 1) * 512], start=True, stop=True),
                              [], sig=True)
                s_ev[n] = ev

        def rec_exp(n):
            h, j, i = tiles[n]
            r = i - 4 * j
            c0 = 0 if r < 0 else 128 * r
            sb_ = s_ps[n % 3]
            pt = Pt[n % 3]
            exp_ev[n] = P.op("act", lambda e: e.activation(out=pt[:, c0:512], in_=sb_[:, c0:512], func=AF.Exp, scale=0.125),
                             [s_ev[n], pv_ev.get(n - 3)], sig=True, chain=False)

        def rec_PV(n):
            h, j, i = tiles[n]
            p = h % 2
            r = i - 4 * j
            c0 = 0 if r < 0 else 128 * r
            pt = Pt[n % 3]
            qn = h * 8 + j
            ob = o_ps[qn % 2]
            first = (i == 0)
            lastk = (i == 4 * j + 3)
            w = [exp_ev[n]]
            if first and qn >= 2:
                w.append(norm_ev[qn - 2])
            ev = P.op("pe", lambda e: e.matmul(ob[:, c0:512], lhsT=Va[p][:, i, :], rhs=pt[:, c0:512], start=first, stop=lastk),
                      w, sig=(True if lastk else None))
            if lastk:
                rb = rec[qn % 2]
                P.op("dve", lambda e: e.reciprocal(out=rb[64:128, :], in_=ob[64:128, :]), [ev])
                po = (h % 2) * 64
                norm_ev[qn] = P.op("dve", lambda e: e.tensor_tensor(
                    out=OT_all[po:po + 64, h // 2, j * 512:(j + 1) * 512], in0=ob[0:64, :], in1=rb[64:128, :], op=ALU.mult),
                    [], sig=True)
                if j == 7:
                    head_done[h] = ev
                    if h + 2 < NH:
                        head_ev[h + 2] = load_head(h + 2)

        LA = 2
        for n in range(min(LA, NTL)):
            rec_S(n)
        for n in range(NTL):
            rec_exp(n)
            if n + LA < NTL:
                rec_S(n + LA)
            rec_PV(n)


def ph_oproj(nc, T, OT_all):
    with Phase(nc, "op") as P:
        wo = P.sb("wo", [128, 8, 1024], BF16)
        gt = P.sb("gt", [128, 1024], F32)
        xt = [P.sb(f"xt{i}", [128, 1024], F32) for i in range(3)]
        t1 = [P.sb(f"t1{i}", [128, 1024], F32) for i in range(2)]
        x1 = [P.sb(f"x1{i}", [128, 1024], F32) for i in range(2)]
        y_ps = [P.ps(f"y{i}", [128, 1024]) for i in range(2)]
        s_c = P.dsem("c")
        s_x = [P.dsem(f"x{i}") for i in range(3)]
        s_o = [P.dsem(f"o{i}") for i in range(2)]
        e_w = P.dma1("pool", wo[:], T["w_o"].ap().rearrange("(c p) n -> p c n", p=128))
        e_g = P.dma1("sp", gt[:], T["mod_d"][0, 2048:3072].partition_broadcast(128))
        xt_free = [None] * 3
        t1_free = [None] * 2
        x1_free = [None] * 2
        y_free = [None] * 2
        for tt in range(NT):
            xb = xt[tt % 3]
            e_x = P.dma("sp", xb[:], T["x"][tt * 128:(tt + 1) * 128, :], waits=[xt_free[tt % 3]], sem=s_x[tt % 3])
            yb = y_ps[tt % 2]
            ev = None
            for nh in range(2):
                for c in range(8):
                    ev = P.op("pe", lambda e, nh=nh, c=c, yb=yb, tt=tt: e.matmul(
                        yb[:, nh * 512:(nh + 1) * 512], lhsT=OT_all[:, c, tt * 128:(tt + 1) * 128],
                        rhs=wo[:, c, nh * 512:(nh + 1) * 512], start=(c == 0), stop=(c == 7)),
                        [e_w, y_free[tt % 2]] if (c == 0 and nh == 0) else [], sig=(True if (c == 7 and nh == 1) else None))
            tb = t1[tt % 2]
            e_t = P.op("dve", lambda e, tb=tb, yb=yb: e.tensor_tensor(out=tb[:], in0=yb[:, :], in1=gt[:], op=ALU.mult),
                       [ev, e_g, t1_free[tt % 2]], sig=True)
            y_free[tt % 2] = e_t
            ob = x1[tt % 2]
            e_a = P.op("dve", lambda e, tb=tb, xb=xb, ob=ob: e.tensor_tensor(out=ob[:], in0=tb[:], in1=xb[:], op=ALU.add),
                       [e_t, e_x, x1_free[tt % 2]], sig=True)
            t1_free[tt % 2] = e_a
            xt_free[tt % 3] = e_a
            x1_free[tt % 2] = P.dma("sp", T["X1_d"][tt * 128:(tt + 1) * 128, :], ob[:], waits=[e_a], sem=s_o[tt % 2])


def ph_front(nc, T, l, R, Xin):
    with Phase(nc, f"fr{l}") as P:
        s_c = P.dsem("c")
        s_x = [P.dsem(f"x{i}") for i in range(3)]
        s_sc = P.dsem("sc")
        a_f, b_f, ev_ab = make_ab(P, T, l, "gffn_col", 3, 4, s_c)
        rw = P.sb("rw", [128, 8, 32], F32)
        rwp = P.sb("rwp", [128, 8, 32], F32)
        rconst = P.sb("rconst", [1, 32], F32)
        ones_row = P.sb("ones_row", [1, 128], F32)
        identf = P.sb("identf", [128, 128], F32)
        ltri = P.sb("ltri", [128, 128], F32)
        onesf = P.sb("onesf", [128, 128], F32)
        rbias = P.sb("rbias", [128, 32], F32)
        thr = P.sb("thr", [128, 16], F32)
        biota = P.sb("biota", [128, 64], F32)
        ones32 = P.sb("ones32", [128, 32], F32)
        xt = [P.sb(f"xt{i}", [128, 1024], F32) for i in range(3)]
        xnf = [P.sb(f"xnf{i}", [128, 1024], F32) for i in range(2)]
        xnT = [P.sb(f"xnT{i}", [128, 8, 128], F32) for i in range(2)]
        junk = P.sb("junk", [128, 1024], BF16)
        xn_all = P.sb("xn_all", [128, 32, 1024], BF16)
        ss = P.sb("ss", [128, 32], F32)
        sd = P.sb("sd", [128, 32], F32)
        rstd = P.sb("rstd", [128, 32], F32)
        Mall = P.sb("Mall", [128, 32, 32], F32)
        Gall = P.sb("Gall", [128, 32, 32], F32)
        posl = P.sb("posl", [128, 32, 32], F32)
        CSb = P.sb("CSb", [128, 32, 32], F32)
        carry = P.sb("carry", [128, 32, 32], F32)
        W1 = P.sb("W1", [128, 32, 32], F32)
        W2 = P.sb("W2", [128, 32, 32], F32)
        cmpb = P.sb("cmpb", [128, 64, 32], F32)
        scores = [P.sb(f"scores{i}", [128, 32], F32) for i in range(2)]
        sel = P.sb("sel", [128, 32], F32)
        mx = P.sb("mx", [128, 4, 8], F32)
        gs = P.sb("gs", [128, 4], F32)
        gmax = P.sb("gmax", [128, 1], F32)
        gm = P.sb("gm", [128, 4], F32)
        pen = P.sb("pen", [128, 4], F32)
        selm = P.sb("selm", [128, 32], F32)
        t8 = P.sb("t8", [128, 8], F32)
        sm = P.sb("sm", [128, 32], F32)
        den = P.sb("den", [128, 1], F32)
        rden = P.sb("rden", [128, 1], F32)
        cnt = P.sb("cnt", [128, 32], F32)
        nb = P.sb("nb", [128, 32], F32)
        pend = P.sb("pend", [128, 32], F32)
        pst = P.sb("pst", [128, 32], F32)
        da1 = P.sb("da1", [128, 32], F32)
        sumD = P.sb("sumD", [128, 32], F32)
        db1 = P.sb("db1", [128, 32], F32)
        gsum = P.sb("gsum", [128, 32], F32)
        bef = P.sb("bef", [128, 64], F32)
        tpf = P.ps("tpf", [128, 8, 128], F32)
        lg_ps = [P.ps(f"lg{i}", [128, 32]) for i in range(2)]
        pos_ps = [P.ps(f"pos{i}", [128, 32]) for i in range(2)]
        cs_ps = P.ps("cs", [128, 1024])

        zt = P.sb("zt", [128, 8192], BF16)
        s_z = P.dsem("z")
        e_zm = P.op("pool", lambda e: e.memset(zt[:], 0.0), [], sig=True)
        xgv = T["XG_d"].ap().rearrange("(p j n) d -> p j (n d)", p=128, j=16)
        e_z = None
        for j in range(16):
            e_z = P.dma("sp", xgv[:, j, :], zt[:], waits=[e_zm], sem=s_z)
        e_c = []
        for (t_, nm) in ((rw, "rw"), (identf, "identf"), (ltri, "ltri"), (onesf, "ones"), (rbias, "rbias_row"),
                         (thr, "thr"), (biota, "biota")):
            e_c.append(P.dma("sp", t_[:], T[nm].ap(), sem=s_c))
        P.op("dve", lambda e: e.memset(ones_row[:], 1.0))
        P.op("dve", lambda e: e.memset(ones32[:], 1.0))
        ev = None
        for kc in range(8):
            ev = P.op("dve", lambda e, kc=kc: e.tensor_scalar(out=rwp[:, kc, :], in0=rw[:, kc, :], scalar1=a_f[:, kc:kc + 1],
                                                              scalar2=None, op0=ALU.mult), e_c + [ev_ab], sig=True)
        e_rwp = ev
        for kc in range(8):
            ev = P.op("pe", lambda e, kc=kc: e.matmul(cs_ps[0:1, 0:32], lhsT=b_f[:, kc:kc + 1], rhs=rw[:, kc, :],
                                                      start=(kc == 0), stop=(kc == 7)), e_c + [ev_ab] if kc == 0 else [],
                      sig=(True if kc == 7 else None))
        e_rc = P.op("dve", lambda e: e.tensor_copy(out=rconst[:], in_=cs_ps[0:1, 0:32]), [ev], sig=True)

        FS = DBG.get("front_stop", 99)
        xt_free = [[] for _ in range(3)]
        xnf_rd = [None] * 2
        xnT_rd = [None] * 2
        tpf_free = []
        lg_free = [None] * 2
        sc_free = [None] * 2
        pos_free = [None] * 2
        sig_ev = {}
        xnf_rd2 = [None] * 2

        def stageA(tt):
            nonlocal tpf_free
            xb = xt[tt % 3]
            e_x = P.dma("sp", xb[:], Xin[tt * 128:(tt + 1) * 128, :], waits=xt_free[tt % 3], sem=s_x[tt % 3])
            e1 = P.op("act", lambda e: e.activation(out=junk[:], in_=xb[:], func=AF.Square, accum_out=ss[:, tt:tt + 1]),
                      [e_x], sig=True)
            e2 = P.op("act", lambda e: e.activation(out=sd[:, tt:tt + 1], in_=ss[:, tt:tt + 1], func=AF.Sqrt,
                                                    bias=EPS, scale=1.0 / D), [], sig=True)
            P.op("dve", lambda e: e.reciprocal(out=rstd[:, tt:tt + 1], in_=sd[:, tt:tt + 1]), [e2])
            xf = xnf[tt % 2]
            e3 = P.op("dve", lambda e: e.tensor_scalar(out=xf[:], in0=xb[:], scalar1=rstd[:, tt:tt + 1], scalar2=None,
                                                       op0=ALU.mult), [xnf_rd[tt % 2], xnf_rd2[tt % 2]], sig=True)
            e4 = P.op("act", lambda e: e.activation(out=xn_all[:, tt, :], in_=xf[:], func=AF.Copy), [e3], sig=True)
            xt_free[tt % 3] = [e3, e1]
            xnf_rd2[tt % 2] = e4
            ev = None
            for kc in range(8):
                ev = P.op("pe", lambda e, kc=kc: e.transpose(tpf[:, kc, :], xf[:, kc * 128:(kc + 1) * 128], identf[:]),
                          ([e3] + e_c + tpf_free) if kc == 0 else [], sig=(True if kc == 7 else None))
            xnf_rd[tt % 2] = ev
            xT = xnT[tt % 2]
            ea = P.op("act", lambda e: e.activation(out=xT[:, 0:4, :], in_=tpf[:, 0:4, :], func=AF.Copy),
                      [ev, xnT_rd[tt % 2]], sig=True)
            ed = P.op("dve", lambda e: e.tensor_copy(out=xT[:, 4:8, :], in_=tpf[:, 4:8, :]),
                      [ev, xnT_rd[tt % 2]], sig=True)
            tpf_free = [ea, ed]
            lg = lg_ps[tt % 2]
            for kc in range(8):
                P.op("pe", lambda e, kc=kc: e.matmul(lg[:, :], lhsT=xT[:, kc, :], rhs=rwp[:, kc, :], start=(kc == 0), stop=False),
                     [ea, ed, e_rwp, e_rc, lg_free[tt % 2]] if kc == 0 else [])
            ev = P.op("pe", lambda e: e.matmul(lg[:, :], lhsT=ones_row[0:1, :], rhs=rconst[0:1, :], start=False, stop=True),
                      [], sig=True)
            xnT_rd[tt % 2] = ev
            sc = scores[tt % 2]
            e_s = P.op("act", lambda e: e.activation(out=sc[:], in_=lg[:, :], func=AF.Sigmoid), [ev, sc_free[tt % 2]], sig=True)
            lg_free[tt % 2] = e_s
            sig_ev[tt] = e_s

        def stageB(tt):
            sc = scores[tt % 2]
            e_s = sig_ev[tt]
            P.op("dve", lambda e: e.tensor_tensor(out=sel[:], in0=sc[:], in1=rbias[:], op=ALU.add), [e_s])
            for g in range(4):
                P.op("dve", lambda e, g=g: e.max(out=mx[:, g, :], in_=sel[:, g * 8:(g + 1) * 8]))
            P.op("dve", lambda e: e.tensor_tensor(out=gs[:], in0=mx[:, :, 0], in1=mx[:, :, 1], op=ALU.add))
            P.op("dve", lambda e: e.tensor_reduce(out=gmax[:], in_=gs[:], axis=AX.X, op=ALU.max))
            P.op("dve", lambda e: e.tensor_scalar(out=gm[:], in0=gs[:], scalar1=gmax[:, 0:1], scalar2=None, op0=ALU.is_equal))
            P.op("dve", lambda e: e.tensor_scalar(out=pen[:], in0=gm[:], scalar1=-1.0, scalar2=1e9, op0=ALU.add, op1=ALU.mult))
            P.op("dve", lambda e: e.tensor_tensor(out=selm[:].rearrange("p (g k) -> p g k", g=4),
                                                  in0=sel[:].rearrange("p (g k) -> p g k", g=4),
                                                  in1=pen[:].unsqueeze(2).to_broadcast([128, 4, 8]), op=ALU.add))
            P.op("dve", lambda e: e.max(out=t8[:], in_=selm[:]))
            e_M = P.op("dve", lambda e: e.tensor_scalar(out=Mall[:, tt, :], in0=selm[:], scalar1=t8[:, 1:2], scalar2=None,
                                                        op0=ALU.is_ge), [], sig=True)
            P.op("dve", lambda e: e.tensor_tensor(out=sm[:], in0=Mall[:, tt, :], in1=sc[:], op=ALU.mult))
            P.op("dve", lambda e: e.tensor_reduce(out=den[:], in_=sm[:], axis=AX.X, op=ALU.add))
            P.op("dve", lambda e: e.reciprocal(out=rden[:], in_=den[:]))
            sc_free[tt % 2] = P.op("dve", lambda e: e.tensor_scalar(out=Gall[:, tt, :], in0=sm[:], scalar1=rden[:, 0:1],
                                                                  scalar2=None, op0=ALU.mult), [], sig=True)
            pp = pos_ps[tt % 2]
            ev = P.op("pe", lambda e: e.matmul(pp[:, :], lhsT=ltri[:], rhs=Mall[:, tt, :], start=True, stop=True),
                      [e_M, pos_free[tt % 2]], sig=True)
            pos_free[tt % 2] = P.op("act", lambda e: e.activation(out=posl[:, tt, :], in_=pp[:, :], func=AF.Copy), [ev], sig=True)

        stageA(0)
        for tt in range(NT):
            if tt + 1 < NT:
                stageA(tt + 1)
            stageB(tt)
        if FS <= 4:
            return
        e_pos = pos_free[(NT - 1) % 2]
        Mf = Mall[:].rearrange("p a b -> p (a b)")
        ev = None
        for h2 in range(2):
            ev = P.op("pe", lambda e, h2=h2: e.matmul(cs_ps[:, h2 * 512:(h2 + 1) * 512], lhsT=onesf[:],
                                                      rhs=Mf[:, h2 * 512:(h2 + 1) * 512], start=True, stop=True),
                      [sc_free[(NT - 1) % 2]], sig=(True if h2 == 1 else None))
        P.op("dve", lambda e: e.tensor_copy(out=CSb[:].rearrange("p a b -> p (a b)"), in_=cs_ps[:, :]), [ev, e_pos])
        P.op("dve", lambda e: e.memset(carry[:, 0, :], 0.0))
        for tt in range(1, NT):
            P.op("dve", lambda e, tt=tt: e.tensor_tensor(out=carry[:, tt, :], in0=carry[:, tt - 1, :], in1=CSb[:, tt - 1, :],
                                                         op=ALU.add))
        P.op("dve", lambda e: e.tensor_tensor(out=cnt[:], in0=carry[:, NT - 1, :], in1=CSb[:, NT - 1, :], op=ALU.add))
        P.op("dve", lambda e: e.tensor_tensor(out=W1[:, :, 0:16], in0=cnt[:].unsqueeze(2).to_broadcast([128, 32, 16]),
                                              in1=thr[:].unsqueeze(1).to_broadcast([128, 32, 16]), op=ALU.is_gt))
        P.op("dve", lambda e: e.tensor_reduce(out=nb[:], in_=W1[:, :, 0:16], axis=AX.X, op=ALU.add))
        P.op("dve", lambda e: e.tensor_tensor_scan(out=pend[:], data0=ones32[:], data1=nb[:], initial=0.0,
                                                   op0=ALU.mult, op1=ALU.add))
        P.op("dve", lambda e: e.tensor_tensor(out=pst[:], in0=pend[:], in1=nb[:], op=ALU.subtract))
        P.op("dve", lambda e: e.tensor_scalar(out=pst[:], in0=pst[:], scalar1=float(BLK), scalar2=None, op0=ALU.mult))
        P.op("dve", lambda e: e.tensor_tensor(out=W1[:], in0=posl[:], in1=carry[:], op=ALU.add))
        P.op("dve", lambda e: e.tensor_tensor(out=W1[:], in0=W1[:], in1=pst[:].unsqueeze(1).to_broadcast([128, 32, 32]),
                                              op=ALU.add))
        P.op("dve", lambda e: e.scalar_tensor_tensor(out=W1[:], in0=W1[:], scalar=1.0, in1=Mall[:], op0=ALU.add, op1=ALU.mult))
        P.op("dve", lambda e: e.tensor_reduce(out=da1[:], in_=W1[:], axis=AX.X, op=ALU.max))
        P.op("dve", lambda e: e.tensor_reduce(out=sumD[:], in_=W1[:], axis=AX.X, op=ALU.add))
        P.op("dve", lambda e: e.tensor_tensor(out=db1[:], in0=sumD[:], in1=da1[:], op=ALU.subtract))
        dtmp = P.sb("dtmp", [128, 32], F32)
        for (src_, nm_) in ((da1, "destA"), (db1, "destB")):
            P.op("dve", lambda e, src_=src_: e.tensor_scalar(out=dtmp[:], in0=src_[:], scalar1=-1.0, scalar2=0.0,
                                                            op0=ALU.add, op1=ALU.max))
            P.op("dve", lambda e, nm_=nm_: e.tensor_scalar(out=R[nm_][:], in0=dtmp[:], scalar1=float(NBLK * BLK - 1),
                                                         scalar2=None, op0=ALU.min))
        P.op("dve", lambda e: e.tensor_tensor(out=W2[:], in0=W1[:], in1=da1[:].unsqueeze(2).to_broadcast([128, 32, 32]),
                                              op=ALU.is_equal))
        P.op("dve", lambda e: e.tensor_tensor(out=W2[:], in0=W2[:], in1=Gall[:], op=ALU.mult))
        P.op("dve", lambda e: e.tensor_reduce(out=R["gA"][:], in_=W2[:], axis=AX.X, op=ALU.add))
        P.op("dve", lambda e: e.tensor_reduce(out=gsum[:], in_=Gall[:], axis=AX.X, op=ALU.add))
        P.op("dve", lambda e: e.tensor_tensor(out=R["gB"][:], in0=gsum[:], in1=R["gA"][:], op=ALU.subtract))
        P.op("dve", lambda e: e.tensor_tensor(out=cmpb[:], in0=pend[:].unsqueeze(1).to_broadcast([128, 64, 32]),
                                              in1=biota[:].unsqueeze(2).to_broadcast([128, 64, 32]), op=ALU.is_le))
        P.op("dve", lambda e: e.tensor_reduce(out=bef[:], in_=cmpb[:], axis=AX.X, op=ALU.add))
        e_tab = P.op("dve", lambda e: e.tensor_scalar(out=R["blk_e"][:], in0=bef[:], scalar1=31.0, scalar2=None, op0=ALU.min),
                     [], sig=True)
        piota = P.sb("piota", [128, 1], F32)
        e_pi = P.dma1("sp", piota[:], T["piota"].ap())
        e_tab = P.op("dve", lambda e: e.tensor_scalar(out=R["idxW"][:], in0=R["blk_e"][:], scalar1=128.0, scalar2=piota[:, 0:1],
                                                      op0=ALU.mult, op1=ALU.add), [e_pi], sig=True)
        if FS <= 5:
            return
        if "Ri_dbg" in T and l == DBG.get("dump_layer", 0):
            s_dbg = P.dsem("dbg")
            P.dma("sp", T["Ri_dbg"][0, :, :], R["destA"][:], waits=[e_tab], sem=s_dbg)
            P.dma("sp", T["Ri_dbg"][1, :, :], R["destB"][:], waits=[e_tab], sem=s_dbg)
            P.dma("sp", T["Rf_dbg"][0, :, 0:32], R["gA"][:], waits=[e_tab], sem=s_dbg)
            P.dma("sp", T["Rf_dbg"][1, :, 0:32], R["gB"][:], waits=[e_tab], sem=s_dbg)
            P.dma("sp", T["Rf_dbg"][2, :, :], R["blk_e"][:], waits=[e_tab], sem=s_dbg)
        if FS <= 6:
            return
        breg = {}

        def bc(e):
            if "r" not in breg:
                breg["r"] = e.alloc_register("bc_" + P.name)
                e.reg_mov(breg["r"], NBLK * BLK - 1)
            return breg["r"]
        s_sc4 = [s_sc, P.dsem("sc2"), P.dsem("sc3"), P.dsem("sc4")]
        sc_prev = [None] * 4
        for tt in range(NT):
            for i_, nm in enumerate(("destA", "destB")):
                k_ = (tt % 2) * 2 + i_
                sc_prev[k_] = P.op("pool", lambda e, tt=tt, nm=nm: e.indirect_dma_start(
                    out=T["XG_d"][:, :], out_offset=bass.IndirectOffsetOnAxis(ap=R[nm][:, tt:tt + 1], axis=0),
                    in_=xn_all[:, tt, :], in_offset=None),
                    [e_tab, P.last.get("pool"), P.last.get("act"), e_z, sc_prev[k_]], sig=s_sc4[k_], inc=16)


def ph_moe(nc, T, l, R):
    with Phase(nc, f"moe{l}") as P:
        s_c = P.dsem("c")
        s_w = [[P.dsem(f"w{i}_{j}") for j in range(3)] for i in range(2)]
        s_xr = [P.dsem(f"xr{i}") for i in range(2)]
        s_y = [P.dsem(f"y{i}") for i in range(2)]
        sh = P.sb("sh", [128, 8], F32)
        sc = P.sb("sc", [128, 8], F32)
        g = P.sb("g", [128, 8], F32)
        a_f = P.sb("a_f", [128, 8], F32)
        e1 = P.dma("sp", sh[:], T["mod_d"][l, 3 * 1024:4 * 1024].rearrange("(p k) -> p k", k=8), sem=s_c)
        e2 = P.dma("sp", sc[:], T["mod_d"][l, 4 * 1024:5 * 1024].rearrange("(p k) -> p k", k=8), sem=s_c)
        e3 = P.dma("sp", g[:], T["gffn_pk"][:, l, :], sem=s_c)
        ev_ab = P.op("dve", lambda e: e.scalar_tensor_tensor(out=a_f[:], in0=sc[:], scalar=1.0, in1=g[:],
                                                            op0=ALU.add, op1=ALU.mult), [e1, e2, e3], sig=True)
        b_f = sh
        ident = P.sb("ident", [128, 128], BF16)
        e_id = P.dma1("pool", ident[:], T["ident"].ap())
        w1b = [P.sb(f"w1b{i}", [128, 8, 512], BF16) for i in range(2)]
        w3b = [P.sb(f"w3b{i}", [128, 8, 512], BF16) for i in range(2)]
        w2b = [P.sb(f"w2b{i}", [128, 4, 1024], BF16) for i in range(2)]
        xr = [P.sb(f"xr{i}", [128, 2, 1024], BF16) for i in range(2)]
        hT = [P.sb(f"hT{i}", [128, 8, 256], BF16) for i in range(2)]
        sa = [P.sb(f"sa{i}", [128, 256], F32) for i in range(2)]
        hid = [P.sb(f"hid{i}", [128, 4, 256], BF16) for i in range(2)]
        yst = [P.sb(f"yst{i}", [128, 2, 1024], F32) for i in range(2)]
        tp = P.ps("tp", [128, 16, 128], BF16)
        ab_ps = [P.ps(f"ab{i}", [128, 2, 256]) for i in range(2)]
        y_ps = [P.ps(f"y{i}", [128, 512]) for i in range(2)]
        w1v = T[f"w1_{l}"].ap().rearrange("e (p k) n -> (e p) (k n)", k=8)
        w3v = T[f"w3_{l}"].ap().rearrange("e (p k) n -> (e p) (k n)", k=8)
        w2v = T[f"w2_{l}"].ap().rearrange("e (p k) n -> (e p) (k n)", k=4)

        wb_free = [None] * 2
        xr_free = [None] * 2
        hT_free = [None] * 2
        tp_free = []
        ab_free = [None] * 2
        sa_free = [None] * 2
        hid_free = [None] * 2
        yps_free = [None] * 2
        yst_free = [None] * 2
        ny = 0
        nab = 0
        breg = {}

        def bc(e):
            if "r" not in breg:
                breg["r"] = e.alloc_register("bcw_" + P.name)
                e.reg_mov(breg["r"], E * 128 - 1)
            return breg["r"]

        def load_w(b):
            pb = b % 2
            evs_ = []
            prev = w_ev.get(b - 1) or []
            for j_, (src, dst) in enumerate(((w1v, w1b[pb]), (w3v, w3b[pb]), (w2v, w2b[pb]))):
                flat = dst[:].rearrange("p k n -> p (k n)")
                evs_.append(P.op("pool", lambda e, src=src, flat=flat: e.indirect_dma_start(
                    out=flat, out_offset=None, in_=src,
                    in_offset=bass.IndirectOffsetOnAxis(ap=R["idxW"][:, b:b + 1], axis=0),
                    ), [wb_free[pb]] + list(prev), sig=s_w[pb][j_], inc=16))
            return evs_

        def load_x(b):
            pb = b % 2
            return P.dma("sp", xr[pb][:], T["XG_d"][b * BLK:(b + 1) * BLK, :].rearrange("(s p) d -> p s d", p=128),
                         waits=[xr_free[pb]], sem=s_xr[pb])

        w_ev = {}
        w_ev[0] = load_w(0)
        x_ev = {0: load_x(0)}

        def do_block(b):
            nonlocal ny, nab, tp_free
            pb = b % 2
            if b + 1 < NBLK:
                w_ev[b + 1] = load_w(b + 1)
                x_ev[b + 1] = load_x(b + 1)
            xb = xr[pb]
            ev = None
            for s_ in range(2):
                xv = xb[:, s_, :].rearrange("p (q k) -> p k q", k=8)
                for kc in range(8):
                    first = (s_ == 0 and kc == 0)
                    lastt = (s_ == 1 and kc == 7)
                    ev = P.op("pe", lambda e, s_=s_, kc=kc, xv=xv: e.transpose(tp[:, s_ * 8 + kc, :], xv[:, kc, :], ident[:]),
                              ([x_ev[b], e_id] + tp_free) if first else [], sig=(True if lastt else None))
            xr_free[pb] = ev
            hb = hT[pb]
            tpv = tp[:].rearrange("p (s k) t -> p s k t", s=2)
            ea = ed = None
            for kc in range(8):
                dst = hb[:, kc, :].rearrange("p (s t) -> p s t", s=2)
                ed = P.op("dve", lambda e, dst=dst, kc=kc: e.tensor_scalar(out=dst, in0=tpv[:, :, kc, :],
                                                                          scalar1=a_f[:, kc:kc + 1], scalar2=b_f[:, kc:kc + 1],
                                                                          op0=ALU.mult, op1=ALU.add),
                          [ev, ev_ab, hT_free[pb]], sig=True)
                ea = ed
            tp_free = [ea, ed]
            hd = hid[pb]
            w1s = w1b[pb][:].rearrange("p k (q f) -> p k f q", f=4)
            w3s = w3b[pb][:].rearrange("p k (q f) -> p k f q", f=4)
            e_h = None
            for fc in range(4):
                ab = ab_ps[nab % 2]
                for kc in range(8):
                    P.op("pe", lambda e, ab=ab, kc=kc, fc=fc: e.matmul(ab[:, 0, :], lhsT=w1s[:, kc, fc, :],
                                                                      rhs=hb[:, kc, :], start=(kc == 0), stop=(kc == 7)),
                         ([ea, ed, ab_free[nab % 2]] + w_ev[b]) if kc == 0 else [])
                for kc in range(8):
                    ev = P.op("pe", lambda e, ab=ab, kc=kc, fc=fc: e.matmul(ab[:, 1, :], lhsT=w3s[:, kc, fc, :],
                                                                           rhs=hb[:, kc, :], start=(kc == 0), stop=(kc == 7)),
                              [], sig=(True if kc == 7 else None))
                sab = sa[nab % 2]
                e_s = P.op("act", lambda e, sab=sab, ab=ab: e.activation(out=sab[:], in_=ab[:, 0, :], func=AF.Silu),
                           [ev, sa_free[nab % 2]], sig=True)
                e_h = P.op("dve", lambda e, sab=sab, ab=ab, fc=fc: e.tensor_tensor(out=hd[:, fc, :], in0=sab[:], in1=ab[:, 1, :],
                                                                                  op=ALU.mult),
                           [e_s, ev, hid_free[pb]], sig=True)
                ab_free[nab % 2] = e_h
                sa_free[nab % 2] = e_h
                nab += 1
            hT_free[pb] = ev
            ysb = yst[pb]
            evs = []
            for sub in range(2):
                for nh in range(2):
                    yp = y_ps[ny % 2]
                    for fc in range(4):
                        ev = P.op("pe", lambda e, yp=yp, fc=fc, sub=sub, nh=nh: e.matmul(
                            yp[:, :], lhsT=hd[:, fc, sub * 128:(sub + 1) * 128], rhs=w2b[pb][:, fc, nh * 512:(nh + 1) * 512],
                            start=(fc == 0), stop=(fc == 3)),
                            [e_h, yps_free[ny % 2]] if fc == 0 else [], sig=(True if fc == 3 else None))
                    dst = ysb[:, sub, nh * 512:(nh + 1) * 512]
                    if ny % 2 == 0:
                        e_y = P.op("act", lambda e, dst=dst, yp=yp: e.activation(out=dst, in_=yp[:, :], func=AF.Copy),
                                   [ev, yst_free[pb]], sig=True)
                    else:
                        e_y = P.op("dve", lambda e, dst=dst, yp=yp: e.tensor_copy(out=dst, in_=yp[:, :]),
                                   [ev, yst_free[pb]], sig=True)
                    yps_free[ny % 2] = e_y
                    evs.append(e_y)
                    ny += 1
            hid_free[pb] = ev
            wb_free[pb] = ev
            yst_free[pb] = P.dma("sp", T["YG_d"][b * BLK:(b + 1) * BLK, :].rearrange("(s p) d -> p s d", p=128), ysb[:],
                                 waits=evs[-2:], sem=s_y[pb])

        for b in range(NBLK):
            do_block(b)


def ph_comb(nc, T, l, R, Xin, Xout, final):
    with Phase(nc, f"cb{l}") as P:
        s_c = P.dsem("c")
        s_x = [P.dsem(f"x{i}") for i in range(3)]
        s_g = [[P.dsem(f"g{i}_{j}") for j in range(2)] for i in range(4)]
        s_o = [P.dsem(f"o{i}") for i in range(2)]
        gt = P.sb("gt", [128, 1024], F32)
        e_g = P.dma1("sp", gt[:], T["mod_d"][l, 5 * 1024:6 * 1024].partition_broadcast(128))
        gfin = None
        if final:
            gfin = P.sb("gfin", [128, 1024], F32)
            e_g2 = P.dma1("sp", gfin[:], T["gfin_row"].ap())
            junk = P.sb("junk", [128, 1024], BF16)
            ss = P.sb("ss", [128, 32], F32)
            sd = P.sb("sd", [128, 32], F32)
            rstd = P.sb("rstd", [128, 32], F32)
            ob = [P.sb(f"ob{i}", [128, 1024], F32) for i in range(2)]
        xt = [P.sb(f"xt{i}", [128, 1024], F32) for i in range(3)]
        ya = [P.sb(f"ya{i}", [128, 1024], F32) for i in range(4)]
        yb = [P.sb(f"yb{i}", [128, 1024], F32) for i in range(4)]
        tm = [P.sb(f"tm{i}", [128, 1024], F32) for i in range(2)]
        xo = [P.sb(f"xo{i}", [128, 1024], F32) for i in range(2)]
        xt_free = [None] * 3
        y_free = [None] * 4
        tm_free = [None] * 2
        xo_free = [None] * 2
        ob_free = [None] * 2
        gath = {}

        def issue_gather(tt):
            q = tt % 4
            evs_ = []
            for j_, (nm, dst) in enumerate((("destA", ya[q]), ("destB", yb[q]))):
                evs_.append(P.op("pool", lambda e, nm=nm, dst=dst: e.indirect_dma_start(
                    out=dst[:, :], out_offset=None, in_=T["YG_d"][:, :],
                    in_offset=bass.IndirectOffsetOnAxis(ap=R[nm][:, tt:tt + 1], axis=0),
                    ), [y_free[q]], sig=s_g[q][j_], inc=16))
            gath[tt] = evs_

        for t0 in range(3):
            issue_gather(t0)
        breg = {}

        def bc(e):
            if "r" not in breg:
                breg["r"] = e.alloc_register("bc_" + P.name)
                e.reg_mov(breg["r"], NBLK * BLK - 1)
            return breg["r"]
        for tt in range(NT):
            p = tt % 2
            xb = xt[tt % 3]
            e_x = P.dma("sp", xb[:], Xin[tt * 128:(tt + 1) * 128, :], waits=[xt_free[tt % 3]], sem=s_x[tt % 3])
            if tt + 3 < NT:
                issue_gather(tt + 3)
            e_gas = gath[tt]
            q = tt % 4
            tb = tm[p]
            P.op("dve", lambda e, tb=tb, q=q, tt=tt: e.tensor_scalar(out=tb[:], in0=ya[q][:], scalar1=R["gA"][:, tt:tt + 1],
                                                                   scalar2=None, op0=ALU.mult), e_gas + [tm_free[p]])
            P.op("dve", lambda e, tb=tb, q=q, tt=tt: e.scalar_tensor_tensor(out=tb[:], in0=yb[q][:], scalar=R["gB"][:, tt:tt + 1],
                                                                          in1=tb[:], op0=ALU.mult, op1=ALU.add))
            e_t = P.op("dve", lambda e, tb=tb: e.tensor_tensor(out=tb[:], in0=tb[:], in1=gt[:], op=ALU.mult), [e_g], sig=True)
            y_free[q] = e_t
            xob = xo[p]
            e_a = P.op("dve", lambda e, tb=tb, xb=xb, xob=xob: e.tensor_tensor(out=xob[:], in0=tb[:], in1=xb[:], op=ALU.add),
                       [e_t, e_x, xo_free[p]], sig=True)
            tm_free[p] = e_a
            xt_free[tt % 3] = e_a
            if not final:
                xo_free[p] = P.dma("sp", Xout[tt * 128:(tt + 1) * 128, :], xob[:], waits=[e_a], sem=s_o[p])
            else:
                e1 = P.op("act", lambda e, xob=xob, tt=tt: e.activation(out=junk[:], in_=xob[:], func=AF.Square,
                                                                        accum_out=ss[:, tt:tt + 1]), [e_a], sig=True)
                e2 = P.op("act", lambda e, tt=tt: e.activation(out=sd[:, tt:tt + 1], in_=ss[:, tt:tt + 1], func=AF.Sqrt,
                                                               bias=EPS, scale=1.0 / D), [], sig=True)
                P.op("dve", lambda e, tt=tt: e.reciprocal(out=rstd[:, tt:tt + 1], in_=sd[:, tt:tt + 1]), [e2])
                obb = ob[p]
                e_o = P.op("dve", lambda e, obb=obb, xob=xob, tt=tt: e.scalar_tensor_tensor(
                    out=obb[:], in0=xob[:], scalar=rstd[:, tt:tt + 1], in1=gfin[:], op0=ALU.mult, op1=ALU.mult),
                    [e_g2, ob_free[p]], sig=True)
                xo_free[p] = e_o
                ob_free[p] = P.dma("sp", Xout[tt * 128:(tt + 1) * 128, :], obb[:], waits=[e_o], sem=s_o[p])


def ph_pool(nc, T, Xin, Xout):
    with Phase(nc, "pl") as P:
        s_c = P.dsem("c")
        s_x = [P.dsem(f"x{i}") for i in range(6)]
        s_o = [P.dsem(f"o{i}") for i in range(2)]
        a_m, b_m, ev_ab = make_ab(P, T, 1, "gmix_col", 0, 1, s_c)
        identf = P.sb("identf", [128, 128], F32)
        wp = P.sb("wp", [128, 4, 2, 256], BF16)
        cg = P.sb("cg", [128, 1024], F32)
        gtr = P.sb("gtr", [128, 1024], F32)
        invfix = P.sb("invfix", [128, 4, 16], F32)
        fx = P.sb("fx", [128, 2, 16], F32)
        xt = [P.sb(f"xt{i}", [128, 1024], F32) for i in range(6)]
        xnf = [P.sb(f"xnf{i}", [128, 1024], F32) for i in range(2)]
        junk = P.sb("junk", [128, 1024], BF16)
        ss = P.sb("ss", [128, 32], F32)
        sd = P.sb("sd", [128, 32], F32)
        rstd = P.sb("rstd", [128, 32], F32)
        hT = [P.sb(f"hT{i}", [128, 8, 528], F32) for i in range(2)]
        S1 = P.sb("S1", [128, 8, 528], F32)
        S2 = P.sb("S2", [128, 6, 528], F32)
        S3 = P.sb("S3", [128, 4, 528], F32)
        S4 = P.sb("S4", [128, 2, 528], F32)
        pl = [P.sb(f"pl{i}", [128, 8, 512], BF16) for i in range(2)]
        t1 = [P.sb(f"t1{i}", [128, 1024], F32) for i in range(2)]
        x3 = [P.sb(f"x3{i}", [128, 1024], F32) for i in range(2)]
        tpf = [P.ps(f"tpf{i}", [128, 8, 128], F32) for i in range(2)]
        y_ps = [P.ps(f"y{i}", [128, 1024]) for i in range(2)]

        e_id = P.dma1("sp", identf[:], T["identf"].ap())
        e_wp = P.dma1("pool", wp[:], T["pool_w"].ap().rearrange("g (cc p) e -> p g cc e", p=128))
        e_a = P.dma1("sp", cg[:], T["pscale_row"].ap())
        e_b = P.dma1("sp", gtr[:], T["mod_d"][1, 2048:3072].partition_broadcast(128))
        e_if = P.dma1("sp", invfix[:], T["invfix"].ap())
        e_cg = P.op("dve", lambda e: e.tensor_tensor(out=cg[:], in0=cg[:], in1=gtr[:], op=ALU.mult), [e_a, e_b], sig=True)

        xt_free = [None] * 6
        xnf_rd = [None] * 2
        tpf_free = [[], []]
        hT_rd = [[], []]
        pl_rd = [None] * 2
        y_free = [None] * 2
        t1_free = [None] * 2
        x3_free = [None] * 2
        x_ev = {}
        halo_ev = [P.op("pool", lambda e: e.memset(hT[0][:, :, 0:16], 0.0), [], sig=True)]

        def super_tile(Tq):
            H = hT[Tq % 2]
            evs_h = []
            def front_sub(sub):
                nonlocal evs_h
                st = Tq * 4 + sub
                xb = xt[st % 6]
                e_x = P.dma("sp", xb[:], Xin[st * 128:(st + 1) * 128, :], waits=[xt_free[st % 6]], sem=s_x[st % 6])
                x_ev[st] = e_x
                e1 = P.op("act", lambda e: e.activation(out=junk[:], in_=xb[:], func=AF.Square, accum_out=ss[:, st:st + 1]),
                          [e_x], sig=True)
                e2 = P.op("act", lambda e: e.activation(out=sd[:, st:st + 1], in_=ss[:, st:st + 1], func=AF.Sqrt,
                                                        bias=EPS, scale=1.0 / D), [], sig=True)
                P.op("dve", lambda e: e.reciprocal(out=rstd[:, st:st + 1], in_=sd[:, st:st + 1]), [e2])
                xf = xnf[st % 2]
                e3 = P.op("dve", lambda e: e.tensor_scalar(out=xf[:], in0=xb[:], scalar1=rstd[:, st:st + 1], scalar2=None,
                                                           op0=ALU.mult), [xnf_rd[st % 2]], sig=True)
                tpb = tpf[st % 2]
                ev = None
                for kc in range(8):
                    ev = P.op("pe", lambda e, kc=kc: e.transpose(tpb[:, kc, :], xf[:, kc * 128:(kc + 1) * 128], identf[:]),
                              ([e3, e_id] + tpf_free[st % 2]) if kc == 0 else [], sig=(True if kc == 7 else None))
                xnf_rd[st % 2] = ev
                ea = ed = None
                for kc in range(8):
                    dst = H[:, kc, 16 + sub * 128:16 + (sub + 1) * 128]
                    ed = P.op("dve", lambda e, kc=kc, dst=dst: e.tensor_scalar(out=dst, in0=tpb[:, kc, :],
                                                                              scalar1=a_m[:, kc:kc + 1], scalar2=b_m[:, kc:kc + 1],
                                                                              op0=ALU.mult, op1=ALU.add),
                              [ev, ev_ab] + hT_rd[Tq % 2], sig=True)
                    ea = ed
                tpf_free[st % 2] = [ea, ed]
                evs_h = [ea, ed]

            for sub in range(4):
                front_sub(sub)
            P.op("dve", lambda e: e.tensor_tensor(out=S1[:, :, 1:528], in0=H[:, :, 1:528], in1=H[:, :, 0:527], op=ALU.add),
                 evs_h + [halo_ev[0]])
            e_s1 = P.op("dve", lambda e: e.tensor_tensor(out=S2[:, :, 3:528], in0=S1[:, 2:8, 3:528], in1=S1[:, 2:8, 1:526],
                                                         op=ALU.add), [], sig=True)
            P.op("pool", lambda e: e.tensor_tensor(out=S3[:, :, 7:528], in0=S2[:, 2:6, 7:528], in1=S2[:, 2:6, 3:524], op=ALU.add),
                 [e_s1])
            e_s4 = P.op("pool", lambda e: e.tensor_tensor(out=S4[:, :, 15:528], in0=S3[:, 2:4, 15:528], in1=S3[:, 2:4, 7:520],
                                                          op=ALU.add), [], sig=True)
            plb = pl[Tq % 2]
            srcs = [(S1, 0, 0.5), (S2, 0, 0.25), (S3, 0, 0.125), (S4, 0, 0.0625)]
            e_p = None
            for g in range(4):
                Sx, c0, sc_ = srcs[g]
                e_p = P.op("dve", lambda e, g=g, Sx=Sx, sc_=sc_: e.scalar_tensor_tensor(
                    out=plb[:, 2 * g:2 * g + 2, :], in0=Sx[:, 0:2, 16:528], scalar=sc_, in1=H[:, 2 * g:2 * g + 2, 16:528],
                    op0=ALU.mult, op1=ALU.subtract), [e_s4, pl_rd[Tq % 2]], sig=True)
                if Tq == 0:
                    P.op("dve", lambda e, g=g, Sx=Sx: e.tensor_tensor(
                        out=fx[:], in0=Sx[:, 0:2, 16:32], in1=invfix[:, g, :].unsqueeze(1).to_broadcast([128, 2, 16]),
                        op=ALU.mult), [e_if])
                    e_p = P.op("dve", lambda e, g=g: e.tensor_tensor(out=plb[:, 2 * g:2 * g + 2, 0:16], in0=fx[:],
                                                                    in1=H[:, 2 * g:2 * g + 2, 16:32], op=ALU.subtract),
                               [], sig=True)
            if Tq + 1 < 8:
                Hn = hT[(Tq + 1) % 2]
                halo_ev[0] = P.op("pool", lambda e: e.tensor_copy(out=Hn[:, :, 0:16], in_=H[:, :, 512:528]),
                                  evs_h + hT_rd[(Tq + 1) % 2], sig=True)
                hT_rd[Tq % 2] = [e_p, halo_ev[0]]
            else:
                hT_rd[Tq % 2] = [e_p]
            def back_sub(sub):
                st = Tq * 4 + sub
                yp = y_ps[st % 2]
                ev = None
                for g in range(4):
                    for cc in range(2):
                        ev = P.op("pe", lambda e, g=g, cc=cc: e.matmul(yp[:, g * 256:(g + 1) * 256],
                                                                      lhsT=plb[:, 2 * g + cc, sub * 128:(sub + 1) * 128],
                                                                      rhs=wp[:, g, cc, :], start=(cc == 0), stop=(cc == 1)),
                                  [e_p, e_wp, y_free[st % 2]] if (g == 0 and cc == 0) else [],
                                  sig=(True if (g == 3 and cc == 1) else None))
                if sub == 3:
                    pl_rd[Tq % 2] = ev
                tb = t1[st % 2]
                e_t = P.op("dve", lambda e: e.tensor_tensor(out=tb[:], in0=yp[:, :], in1=cg[:], op=ALU.mult),
                           [ev, e_cg, t1_free[st % 2]], sig=True)
                y_free[st % 2] = e_t
                xb = xt[st % 6]
                xo = x3[st % 2]
                e_a2 = P.op("pool", lambda e: e.tensor_tensor(out=xo[:], in0=tb[:], in1=xb[:], op=ALU.add),
                            [e_t, x_ev[st], x3_free[st % 2]], sig=True)
                t1_free[st % 2] = e_a2
                xt_free[st % 6] = e_a2
                x3_free[st % 2] = P.dma("sp", Xout[st * 128:(st + 1) * 128, :], xo[:], waits=[e_a2], sem=s_o[st % 2])

            for sub in range(4):
                back_sub(sub)

        for Tq in range(8):
            super_tile(Tq)


IN_SPECS = {
    "x": ([S, D], F32), "c_col": ([128, 8], F32), "ada_w": ([2, D, 6 * D], F32), "ada_b": ([2, 6 * D], F32),
    "gmix_col": ([128, 2, 8], F32), "gffn_col": ([128, 2, 8], F32), "w_in": ([D, 3 * D + NH], F32),
    "fb_col": ([NH, 1], F32), "w_o": ([D, D], F32), "pool_w": ([4, 256, 256], F32), "pscale_row": ([128, D], F32),
    "rw": ([128, 8, E], F32), "rbias_row": ([128, E], F32),
    "w1_0": ([E, D, FF], F32), "w1_1": ([E, D, FF], F32), "w3_0": ([E, D, FF], F32), "w3_1": ([E, D, FF], F32),
    "w2_0": ([E, FF, D], F32), "w2_1": ([E, FF, D], F32), "gfin_row": ([128, D], F32),
    "ident": ([128, 128], F32), "identf": ([128, 128], F32), "mtri": ([128, 128], F32), "ltri": ([128, 128], F32),
    "ones": ([128, 128], F32), "thr": ([128, 16], F32), "biota": ([128, 64], F32), "invfix": ([128, 4, 16], F32),
    "piota": ([128, 1], F32), "gffn_pk": ([128, 2, 8], F32),
}
SCRATCH = {
    "mod_d": ([2, 6 * D], F32), "QT_d": ([D, S], BF16), "KT_d": ([D, S], BF16), "V_d": ([NH, 128, NT, 64], BF16),
    "FL_d": ([NH, S], F32), "QF_d": ([NH, 3, S], BF16), "KF_d": ([NH, 3, S], BF16),
    "X1_d": ([S, D], F32), "X2_d": ([S, D], F32), "X3_d": ([S, D], F32),
    "XG_d": ([NBLK * BLK, D], BF16), "YG_d": ([NBLK * BLK, D], F32),
}


def build(upto=99, debug=(), only_inputs=None):
    nc = bass.Bass("TRN2", target_bir_lowering=False)
    T = {}
    for k, (shp, dt) in IN_SPECS.items():
        if only_inputs is not None and k not in only_inputs:
            continue
        T[k] = nc.dram_tensor(k, shp, dt, kind="ExternalInput")
    for k, (shp, dt) in SCRATCH.items():
        if k in DBG.get("as_input", ()):
            continue
        T[k] = nc.dram_tensor(k, shp, dt, kind=("ExternalOutput" if k in debug else "Internal"))
    T["out"] = nc.dram_tensor("out", [S, D], F32, kind="ExternalOutput")
    if "Ri_dbg" in debug:
        T["Ri_dbg"] = nc.dram_tensor("Ri_dbg", [2, 128, 32], I32, kind="ExternalOutput")
        T["Rf_dbg"] = nc.dram_tensor("Rf_dbg", [3, 128, 64], F32, kind="ExternalOutput")
    for k in DBG.get("as_input", ()):
        shp, dt = SCRATCH[k]
        T[k] = nc.dram_tensor(k, shp, dt, kind="ExternalInput")
    with ExitStack() as es:
        ph_mod(nc, T)
        if upto < 1:
            return nc
        if not DBG.get("skip_l0mix"):
            ph_qkv(nc, T)
            if not DBG.get("no_fgate"):
                ph_fgate(nc, T)
            if upto < 2:
                return nc
            with nc.sbuf_tensor("OT_all", [128, 8, S], BF16) as OT_all:
                ph_attn(nc, T, OT_all)
                ph_oproj(nc, T, OT_all)
        if upto < 3:
            return nc
        R = {
            "destA": es.enter_context(nc.sbuf_tensor("R_destA", [128, 32], I32)),
            "destB": es.enter_context(nc.sbuf_tensor("R_destB", [128, 32], I32)),
            "gA": es.enter_context(nc.sbuf_tensor("R_gA", [128, 32], F32)),
            "gB": es.enter_context(nc.sbuf_tensor("R_gB", [128, 32], F32)),
            "blk_e": es.enter_context(nc.sbuf_tensor("R_blk_e", [128, 64], F32)),
            "idxW": es.enter_context(nc.sbuf_tensor("R_idxW", [128, 64], I32)),
        }
        if "X2_d" not in DBG.get("as_input", ()) and "X3_d" not in DBG.get("as_input", ()):
            ph_front(nc, T, 0, R, T["X1_d"])
            if upto < 4:
                return nc
            ph_moe(nc, T, 0, R)
            ph_comb(nc, T, 0, R, T["X1_d"], T["X2_d"], final=False)
        if upto < 5:
            return nc
        if "X3_d" not in DBG.get("as_input", ()):
            ph_pool(nc, T, T["X2_d"], T["X3_d"])
        if upto < 6:
            return nc
        ph_front(nc, T, 1, R, T["X3_d"])
        ph_moe(nc, T, 1, R)
        ph_comb(nc, T, 1, R, T["X3_d"], T["out"], final=True)
    return nc


def _col(v):
    return np.ascontiguousarray(np.asarray(v, np.float32).reshape(8, 128).T)


def make_in_maps(inp):
    f = lambda a: np.ascontiguousarray(np.asarray(a, np.float32))
    shared = {
        "ada_w": f(inp["ada_w"]), "ada_b": f(inp["ada_b"]),
        "gmix_col": np.ascontiguousarray(np.stack([_col(inp["norm_mix_g"][l]) for l in range(2)], axis=1)),
        "gffn_col": np.ascontiguousarray(np.stack([_col(inp["norm_ffn_g"][l]) for l in range(2)], axis=1)),
        "w_in": f(inp["attn_w_in"][0]), "fb_col": f(inp["attn_f_bias"][0]).reshape(NH, 1), "w_o": f(inp["attn_w_o"][0]),
        "pool_w": f(inp["pool_w"][0]), "pscale_row": np.ascontiguousarray(np.broadcast_to(f(inp["pool_scale"][0]), (128, D))),
        "rw": np.ascontiguousarray(f(inp["router_w"]).reshape(8, 128, E).transpose(1, 0, 2)),
        "rbias_row": np.ascontiguousarray(np.broadcast_to(f(inp["router_bias"]), (128, E))),
        "w1_0": f(inp["exp_w1"][0]), "w1_1": f(inp["exp_w1"][1]), "w3_0": f(inp["exp_w3"][0]), "w3_1": f(inp["exp_w3"][1]),
        "w2_0": f(inp["exp_w2"][0]), "w2_1": f(inp["exp_w2"][1]),
        "gfin_row": np.ascontiguousarray(np.broadcast_to(f(inp["norm_final_g"]), (128, D))),
    }
    ii = np.arange(128)
    shared["ident"] = np.eye(128, dtype=np.float32)
    shared["identf"] = np.eye(128, dtype=np.float32)
    shared["mtri"] = np.where(ii[None, :] >= ii[:, None], 0.0, MASKV).astype(np.float32)
    shared["ltri"] = (ii[:, None] < ii[None, :]).astype(np.float32)
    shared["ones"] = np.ones((128, 128), np.float32)
    shared["thr"] = np.ascontiguousarray(np.broadcast_to((np.arange(16) * BLK).astype(np.float32), (128, 16)))
    shared["biota"] = np.ascontiguousarray(np.broadcast_to(np.arange(64).astype(np.float32), (128, 64)))
    fix = np.zeros((4, 16), np.float32)
    for g, w in enumerate((2, 4, 8, 16)):
        fix[g] = 1.0 / np.minimum(np.arange(16) + 1, w)
    shared["invfix"] = np.ascontiguousarray(np.broadcast_to(fix, (128, 4, 16)))
    shared["piota"] = np.arange(128, dtype=np.float32).reshape(128, 1)
    shared["gffn_pk"] = np.ascontiguousarray(np.stack([f(inp["norm_ffn_g"][l]).reshape(128, 8) for l in range(2)], axis=1))
    maps = []
    for b in range(8):
        m = dict(shared)
        m["x"] = f(inp["x"][b])
        m["c_col"] = _col(inp["c"][b])
        maps.append(m)
    return maps


_NC_CACHE = {}


def kernel(**inputs):
    if "nc" not in _NC_CACHE:
        _NC_CACHE["nc"] = build()
    nc = _NC_CACHE["nc"]
    in_maps = make_in_maps(inputs)
    res = run_bass_kernel_spmd(nc, in_maps, core_ids=list(range(8)))
    return np.stack([np.asarray(r["out"], np.float32) for r in res.results], axis=0)
```

```python
import numpy as np
from contextlib import ExitStack
import concourse.bass as bass
import concourse.mybir as mybir
from concourse.bass_utils import run_bass_kernel_spmd

F32 = mybir.dt.float32
BF16 = mybir.dt.bfloat16
I32 = mybir.dt.int32
ALU = mybir.AluOpType
AF = mybir.ActivationFunctionType
AX = mybir.AxisListType

S = 4096
D = 1024
NT = S // 128
NH = 16
E = 32
FF = 512
BLK = 256
NBLK = 2 * S // BLK + E
EPS = 1e-6
MASKV = -30000.0
DBG = {}


class SemC:
    def __init__(self, h):
        self.h = h
        self.n = 0


class Phase:
    ENGS = ("pe", "act", "dve", "pool", "sp")

    def __init__(self, nc, name):
        self.nc = nc
        self.name = name
        self.es = ExitStack()
        self.q = {e: [] for e in self.ENGS}
        self.dsems = []
        self.allsems = []
        self.last = {}

    def __enter__(self):
        self.es.__enter__()
        self.esem = {e: self.sem("e_" + e) for e in self.ENGS[:4]}
        return self

    def sem(self, nm):
        sc = SemC(self.es.enter_context(self.nc.semaphore(f"{self.name}_{nm}")))
        self.allsems.append(sc)
        return sc

    def dsem(self, nm):
        s = self.sem(nm)
        self.dsems.append(s)
        return s

    def sb(self, nm, shape, dt):
        return self.es.enter_context(self.nc.sbuf_tensor(f"{self.name}_{nm}", list(shape), dt))

    def ps(self, nm, shape, dt=F32):
        return self.es.enter_context(self.nc.psum_tensor(f"{self.name}_{nm}", list(shape), dt))

    def op(self, eng, fn, waits=(), sig=None, inc=1, chain=True):
        is_compute = eng in ("act", "dve", "pool") and inc == 1
        if is_compute:
            sig = self.esem[eng]
            if chain and self.last.get(eng) is not None:
                waits = list(waits) + [self.last[eng]]
        if sig is True:
            sig = self.esem[eng]
        ev = None
        if sig is not None:
            sig.n += inc
            ev = (sig, sig.n)
        self.q[eng].append((fn, self._collapse(waits), sig, inc))
        if is_compute:
            self.last[eng] = ev
        return ev

    @staticmethod
    def _collapse(waits):
        best = {}
        for w in waits:
            if w is None:
                continue
            s_, v = w
            if id(s_) not in best or best[id(s_)][1] < v:
                best[id(s_)] = (s_, v)
        return list(best.values())

    def dma(self, eng, out, in_, waits=(), sem=None, **kw):
        return self.op(eng, lambda e: e.dma_start(out=out, in_=in_, **kw), waits, sig=sem, inc=16)

    def dma1(self, eng, out, in_, waits=(), **kw):
        self._n1 = getattr(self, "_n1", 0) + 1
        return self.dma(eng, out, in_, waits, sem=self.dsem(f"u{self._n1}"), **kw)

    def wait(self, eng, waits):
        self.q[eng].append((None, self._collapse(waits), None, 0))

    def __exit__(self, *a):
        if a[0] is None:
            self.wait("sp", [(s, s.n) for s in self.dsems if s.n > 0])
            self.run()
            with self.nc.Block() as blk2:
                def clr(e):
                    e.dma_reset()
                    for sc in self.allsems:
                        if sc.n > 0:
                            e.sem_clear(sc.h)
                blk2.gpsimd(clr)
        return self.es.__exit__(*a)

    def run(self):
        nc = self.nc
        with nc.Block() as blk:
            def mk(engname):
                def body(e):
                    waited = {}
                    own = self.esem.get(engname)
                    for fn, waits, sig, inc in self.q[engname]:
                        for (s, v) in waits:
                            if waited.get(id(s), 0) >= v:
                                continue
                            e.wait_ge(s.h, v)
                            waited[id(s)] = v
                        if fn is None:
                            continue
                        ins = fn(e)
                        if sig is not None:
                            ins.then_inc(sig.h, inc)
                return body
            blk.tensor(mk("pe"))
            blk.scalar(mk("act"))
            blk.vector(mk("dve"))
            blk.gpsimd(mk("pool"))
            blk.sync(mk("sp"))


def ph_mod(nc, T):
    with Phase(nc, "mod") as P:
        ccol = P.sb("ccol", [128, 8], F32)
        cact = P.sb("cact", [128, 8], F32)
        bias = [P.sb(f"bias{l}", [1, 6144], F32) for l in range(2)]
        row = [P.sb(f"row{l}", [1, 6144], F32) for l in range(2)]
        wb = [P.sb(f"wb{i}", [128, 8, 512], F32) for i in range(3)]
        pst = [P.ps(f"ps{i}", [128, 512]) for i in range(2)]
        s_w = [P.dsem(f"w{i}") for i in range(3)]
        s_out = P.dsem("out")
        ev_c = P.dma1("sp", ccol[:], T["c_col"].ap())
        ev_b = [P.dma1("sp", bias[l][:], T["ada_b"][l:l + 1, :]) for l in range(2)]
        ev_act = P.op("act", lambda e: e.activation(out=cact[:], in_=ccol[:], func=AF.Silu), [ev_c], sig=True)
        pe_done = [None] * 3
        dve_done = [None] * 2
        n = 0
        for l in range(2):
            for nt in range(12):
                k = n % 3
                pp = n % 2
                src = T["ada_w"][l:l + 1, :, nt * 512:(nt + 1) * 512].rearrange("o (kc p) n -> p (o kc) n", p=128)
                ev_w = P.dma("sp" if n % 2 == 0 else "act", wb[k][:], src, waits=[pe_done[k]], sem=s_w[k])
                ev = None
                for kc in range(8):
                    ev = P.op("pe",
                              lambda e, kc=kc, k=k, pp=pp: e.matmul(pst[pp][0:1, :], lhsT=cact[:, kc:kc + 1],
                                                                   rhs=wb[k][:, kc, :], start=(kc == 0), stop=(kc == 7)),
                              waits=[ev_w, ev_act, dve_done[pp]] if kc == 0 else [],
                              sig=(True if kc == 7 else None))
                pe_done[k] = ev
                dve_done[pp] = P.op("dve",
                                    lambda e, l=l, nt=nt, pp=pp: e.tensor_tensor(
                                        out=row[l][0:1, nt * 512:(nt + 1) * 512], in0=pst[pp][0:1, :],
                                        in1=bias[l][0:1, nt * 512:(nt + 1) * 512], op=ALU.add),
                                    [ev, ev_b[l]], sig=True)
                n += 1
            P.dma("sp", T["mod_d"][l:l + 1, :], row[l][:], waits=[dve_done[(n - 1) % 2]], sem=s_out)


def load_mod_cols(P, T, l, which, sem):
    t = P.sb(f"mc_{l}_{which}", [128, 8], F32)
    src = T["mod_d"][l, which * 1024:(which + 1) * 1024].rearrange("(k p) -> p k", p=128)
    ev = P.dma("sp", t[:], src, sem=sem, allow_slow_non_contiguous=True)
    return t, ev


def make_ab(P, T, l, gname, sh_idx, sc_idx, sem=None):
    sem = P.dsem(f"ab_{gname}{l}")
    sh, ev1 = load_mod_cols(P, T, l, sh_idx, sem)
    sc, ev2 = load_mod_cols(P, T, l, sc_idx, sem)
    g = P.sb(f"g_{gname}{l}", [128, 8], F32)
    ev3 = P.dma("sp", g[:], T[gname][:, l, :], sem=sem)
    a = P.sb(f"a_{gname}{l}", [128, 8], F32)
    ev = P.op("dve", lambda e: e.scalar_tensor_tensor(out=a[:], in0=sc[:], scalar=1.0, in1=g[:],
                                                      op0=ALU.add, op1=ALU.mult), [ev1, ev2, ev3], sig=True)
    return a, sh, ev


def ph_qkv(nc, T):
    with Phase(nc, "qkv") as P:
        w_bf = P.sb("w_bf", [128, 8, 3088], BF16)
        ident = P.sb("ident", [128, 128], BF16)
        xt = [P.sb(f"xt{i}", [128, 1024], F32) for i in range(3)]
        xn = [P.sb(f"xn{i}", [128, 1024], BF16) for i in range(4)]
        junk = P.sb("junk", [128, 1024], BF16)
        ss = P.sb("ss", [128, 32], F32)
        sd = P.sb("sd", [128, 32], F32)
        rstd = P.sb("rstd", [128, 32], F32)
        hT = [P.sb(f"hT{i}", [128, 8, 512], BF16) for i in range(2)]
        qk_st = [P.sb(f"qkst{i}", [128, 512], BF16) for i in range(4)]
        v_st = [P.sb(f"vst{i}", [128, 1024], BF16) for i in range(2)]
        fl = P.sb("fl", [16, 4096], F32)
        tp = [P.ps(f"tp{i}", [128, 8, 128], BF16) for i in range(2)]
        ps_qk = [P.ps(f"psqk{i}", [128, 512]) for i in range(2)]
        ps_v = [P.ps(f"psv{i}", [128, 512]) for i in range(2)]
        ps_f = P.ps("psf", [128, 512])
        s_c = P.dsem("c")
        s_w = P.dsem("w")
        s_x = [P.dsem(f"x{i}") for i in range(3)]
        s_qk = [P.dsem(f"qk{i}") for i in range(4)]
        s_v = [P.dsem(f"v{i}") for i in range(2)]
        s_f = P.dsem("f")

        a_m, b_m, ev_ab = make_ab(P, T, 0, "gmix_col", 0, 1, s_c)
        ev_id = P.dma1("pool", ident[:], T["ident"].ap())
        wsrc = T["w_in"].ap().rearrange("(kc p) n -> p kc n", p=128)
        ev_ws = []
        for kc in range(8):
            ev_ws.append(P.dma1("pool", w_bf[:, kc, :], wsrc[:, kc, :]))

        xt_free = [[] for _ in range(3)]
        xn_rd = [None] * 4
        hT_free = [None] * 2
        hT_ready = {}
        xn_ready = {}

        def A(Tq):
            for sub in range(4):
                st = Tq * 4 + sub
                xb = xt[st % 3]
                ev_x = P.dma("sp", xb[:], T["x"][st * 128:(st + 1) * 128, :], waits=xt_free[st % 3], sem=s_x[st % 3])
                e1 = P.op("act", lambda e, xb=xb, st=st: e.activation(out=junk[:], in_=xb[:], func=AF.Square,
                                                                      accum_out=ss[:, st:st + 1]), [ev_x], sig=True)
                e2 = P.op("act", lambda e, st=st: e.activation(out=sd[:, st:st + 1], in_=ss[:, st:st + 1], func=AF.Sqrt,
                                                               bias=EPS, scale=1.0 / D), [], sig=True)
                P.op("dve", lambda e, st=st: e.reciprocal(out=rstd[:, st:st + 1], in_=sd[:, st:st + 1]), [e2])
                xb2 = xn[st % 4]
                e3 = P.op("dve", lambda e, xb=xb, xb2=xb2, st=st: e.tensor_scalar(
                    out=xb2[:], in0=xb[:], scalar1=rstd[:, st:st + 1], scalar2=None, op0=ALU.mult),
                    [xn_rd[st % 4]], sig=True)
                xt_free[st % 3] = [e1, e3]
                xn_ready[st] = e3

        tp_free_evs = [[], []]
        ps_qk_free = [None] * 2
        ps_v_free = [None] * 2
        ps_f_free = [None]
        qk_st_free = [None] * 4
        v_st_free = [None] * 2
        cnt = {"qk": 0, "v": 0}

        def C(Tq):
            hb = hT[Tq % 2]
            rdy = hT_ready[Tq]
            ev = None
            for oc in range(16):
                n = cnt["qk"]
                cnt["qk"] += 1
                pb = ps_qk[n % 2]
                for kc in range(8):
                    ev = P.op("pe", lambda e, pb=pb, kc=kc, oc=oc: e.matmul(
                        pb[:, :], lhsT=w_bf[:, kc, oc * 128:(oc + 1) * 128], rhs=hb[:, kc, :],
                        start=(kc == 0), stop=(kc == 7)),
                        (list(rdy) + ev_ws + [ps_qk_free[n % 2]]) if kc == 0 else [], sig=(True if kc == 7 else None))
                stb = qk_st[n % 4]
                if n % 2 == 0:
                    ev2 = P.op("act", lambda e, stb=stb, pb=pb: e.activation(out=stb[:], in_=pb[:, :], func=AF.Copy),
                               [ev, qk_st_free[n % 4]], sig=True)
                else:
                    ev2 = P.op("dve", lambda e, stb=stb, pb=pb: e.tensor_copy(out=stb[:], in_=pb[:, :]),
                               [ev, qk_st_free[n % 4]], sig=True)
                ps_qk_free[n % 2] = ev2
                dst_t = T["QT_d"] if oc < 8 else T["KT_d"]
                r0 = (oc % 8) * 128
                qk_st_free[n % 4] = P.dma("sp", dst_t[r0:r0 + 128, Tq * 512:(Tq + 1) * 512], stb[:], waits=[ev2],
                                          sem=s_qk[n % 4])
            for sub in range(4):
                vb = v_st[sub % 2]
                evs = []
                for nh in range(2):
                    n = cnt["v"]
                    cnt["v"] += 1
                    pb = ps_v[n % 2]
                    for kc in range(8):
                        ev = P.op("pe", lambda e, pb=pb, kc=kc, nh=nh, sub=sub: e.matmul(
                            pb[:, :], lhsT=hb[:, kc, sub * 128:(sub + 1) * 128],
                            rhs=w_bf[:, kc, 2048 + nh * 512:2048 + (nh + 1) * 512], start=(kc == 0), stop=(kc == 7)),
                            [ps_v_free[n % 2]] if kc == 0 else [], sig=(True if kc == 7 else None))
                    if nh == 0:
                        ev2 = P.op("act", lambda e, vb=vb, pb=pb: e.activation(out=vb[:, 0:512], in_=pb[:, :], func=AF.Copy),
                                   [ev, v_st_free[sub % 2]], sig=True)
                    else:
                        ev2 = P.op("dve", lambda e, vb=vb, pb=pb: e.tensor_copy(out=vb[:, 512:1024], in_=pb[:, :]),
                                   [ev, v_st_free[sub % 2]], sig=True)
                    ps_v_free[n % 2] = ev2
                    evs.append(ev2)
                kt = Tq * 4 + sub
                v_st_free[sub % 2] = P.dma(
                    "sp", T["V_d"][:, :, kt, :].rearrange("h p d -> p h d"),
                    vb[:].rearrange("p (h d) -> p h d", h=16), waits=evs, sem=s_v[sub % 2])
            for kc in range(8):
                ev = P.op("pe", lambda e, kc=kc: e.matmul(ps_f[0:16, :], lhsT=w_bf[:, kc, 3072:3088], rhs=hb[:, kc, :],
                                                          start=(kc == 0), stop=(kc == 7)),
                          [ps_f_free[0]] if kc == 0 else [], sig=(True if kc == 7 else None))
            hT_free[Tq % 2] = ev
            ps_f_free[0] = P.op("dve", lambda e: e.tensor_copy(out=fl[:, Tq * 512:(Tq + 1) * 512], in_=ps_f[0:16, :]),
                                [ev], sig=True)

        def B2(Tq):
            hb = hT[Tq % 2]
            last = []
            for sub in range(4):
                st = Tq * 4 + sub
                tpb = tp[st % 2]
                ev = None
                for kc in range(8):
                    ev = P.op("pe", lambda e, tpb=tpb, kc=kc, xb2=xn[st % 4]: e.transpose(
                        tpb[:, kc, :], xb2[:, kc * 128:(kc + 1) * 128], ident[:]),
                        ([xn_ready[st], ev_id] + tp_free_evs[st % 2]) if kc == 0 else [],
                        sig=(True if kc == 7 else None))
                xn_rd[st % 4] = ev
                ea = ed = None
                for kc in range(8):
                    dst = hb[:, kc, sub * 128:(sub + 1) * 128]
                    ed = P.op("dve", lambda e, dst=dst, tpb=tpb, kc=kc: e.tensor_scalar(
                        out=dst, in0=tpb[:, kc, :], scalar1=a_m[:, kc:kc + 1], scalar2=b_m[:, kc:kc + 1],
                        op0=ALU.mult, op1=ALU.add), [ev, ev_ab, hT_free[Tq % 2]], sig=True)
                    ea = ed
                tp_free_evs[st % 2] = [ea, ed]
                last = [ea, ed]
            hT_ready[Tq] = last

        A(0)
        if not DBG.get("qkv_noB"):
            B2(0)
        for Tq in range(8):
            if Tq + 1 < 8:
                A(Tq + 1)
            if not DBG.get("qkv_noC") and not DBG.get("qkv_noB"):
                C(Tq)
            if Tq + 1 < 8 and not DBG.get("qkv_noB"):
                B2(Tq + 1)
        if not DBG.get("qkv_noC") and not DBG.get("qkv_noB"):
            P.dma("sp", T["FL_d"].ap(), fl[:], waits=[ps_f_free[0]], sem=s_f)


def ph_fgate(nc, T):
    with Phase(nc, "fg") as P:
        fl = P.sb("fl", [16, 4096], F32)
        u = P.sb("u", [16, 4096], F32)
        lf = P.sb("lf", [16, 4096], F32)
        ones = P.sb("ones", [16, 4096], F32)
        G = P.sb("G", [16, 4096], F32)
        fb = P.sb("fb", [16, 1], F32)
        nfb = P.sb("nfb", [16, 1], F32)
        parts = [P.sb(f"part{i}", [16, 4096], BF16) for i in range(3)]
        nparts = [P.sb(f"npart{i}", [16, 4096], BF16) for i in range(3)]
        s_in = P.dsem("in")
        s_out = P.dsem("out")
        e_fl = P.dma1("sp", fl[:], T["FL_d"].ap())
        e_fb = P.dma1("sp", fb[:], T["fb_col"].ap())
        e0 = P.op("dve", lambda e: e.tensor_scalar(out=nfb[:], in0=fb[:], scalar1=-1.0, scalar2=None, op0=ALU.mult),
                  [e_fb], sig=True)
        P.op("dve", lambda e: e.memset(ones[:], 1.0))
        e0b = P.op("dve", lambda e: e.tensor_scalar(out=fl[:], in0=fl[:], scalar1=fb[:, 0:1], scalar2=None, op0=ALU.add),
                   [e_fl, e_fb], sig=True)
        e1 = P.op("act", lambda e: e.activation(out=u[:], in_=fl[:], func=AF.Exp, scale=-1.0), [e0b], sig=True)
        e2 = P.op("act", lambda e: e.activation(out=lf[:], in_=u[:], func=AF.Ln, bias=1.0, scale=1.0), [], sig=True)
        P.op("dve", lambda e: e.tensor_tensor_scan(out=G[:], data0=ones[:], data1=lf[:], initial=0.0,
                                                   op0=ALU.mult, op1=ALU.add), [e2])
        P.op("dve", lambda e: e.tensor_scalar(out=u[:], in0=G[:], scalar1=8.0, scalar2=None, op0=ALU.mult))
        P.op("dve", lambda e: e.tensor_copy(out=parts[0][:], in_=u[:]))
        P.op("dve", lambda e: e.tensor_tensor(out=lf[:], in0=u[:], in1=parts[0][:], op=ALU.subtract))
        P.op("dve", lambda e: e.tensor_copy(out=parts[1][:], in_=lf[:]))
        P.op("dve", lambda e: e.tensor_tensor(out=u[:], in0=lf[:], in1=parts[1][:], op=ALU.subtract))
        P.op("dve", lambda e: e.tensor_copy(out=parts[2][:], in_=u[:]))
        ev = None
        for i in range(3):
            ev = P.op("dve", lambda e, i=i: e.tensor_scalar(out=nparts[i][:], in0=parts[i][:], scalar1=-1.0,
                                                            scalar2=None, op0=ALU.mult), [], sig=True)
        for i in range(3):
            P.dma("sp", T["KF_d"][:, i, :], parts[i][:], waits=[ev], sem=s_out)
            P.dma("sp", T["QF_d"][:, i, :], nparts[i][:], waits=[ev], sem=s_out)


def ph_attn(nc, T, OT_all):
    with Phase(nc, "att") as P:
        Qa = [P.sb(f"Qa{i}", [70, 4096], BF16) for i in range(2)]
        Ka = [P.sb(f"Ka{i}", [70, 4096], BF16) for i in range(2)]
        Va = [P.sb(f"Va{i}", [128, 32, 128], BF16) for i in range(2)]
        Pt = [P.sb(f"Pt{i}", [128, 512], BF16) for i in range(3)]
        rec = [P.sb(f"rec{i}", [128, 512], F32) for i in range(2)]
        ident = P.sb("ident", [128, 128], BF16)
        mtri = P.sb("mtri", [128, 128], BF16)
        s_ps = [P.ps(f"s{i}", [128, 512]) for i in range(3)]
        o_ps = [P.ps(f"o{i}", [128, 512]) for i in range(2)]
        s_c = P.dsem("c")
        s_h = [P.dsem(f"h{i}") for i in range(2)]
        e_id = P.dma1("pool", ident[:], T["ident"].ap())
        e_mt = P.dma1("pool", mtri[:], T["mtri"].ap())
        e_ms = None
        for i in range(2):
            P.op("dve", lambda e, i=i: e.memset(Qa[i][64:70, :], 1.0))
            P.op("dve", lambda e, i=i: e.memset(Ka[i][64:70, :], 1.0))
            e_ms = P.op("dve", lambda e, i=i: e.memset(Va[i][:, :, 64:128], 1.0), [], sig=True)

        head_done = [None] * NH

        def load_head(h):
            p = h % 2
            w = [e_ms, head_done[h - 2] if h >= 2 else None]
            P.dma("sp", Qa[p][0:64, :], T["QT_d"][h * 64:(h + 1) * 64, :], waits=w, sem=s_h[p])
            P.dma("sp", Qa[p][64:67, :], T["QF_d"][h, :, :], sem=s_h[p])
            P.dma("sp", Ka[p][0:64, :], T["KT_d"][h * 64:(h + 1) * 64, :], sem=s_h[p])
            P.dma("sp", Ka[p][67:70, :], T["KF_d"][h, :, :], sem=s_h[p])
            return P.dma("sp", Va[p][:, :, 0:64], T["V_d"][h, :, :, :], sem=s_h[p])

        tiles = []
        for h in range(NH):
            for j in range(8):
                for i in range(4 * j + 4):
                    tiles.append((h, j, i))
        NTL = len(tiles)
        head_ev = {0: load_head(0), 1: load_head(1)}
        exp_ev = [None] * NTL
        pv_ev = {}
        s_ev = [None] * NTL
        norm_ev = {}
        last_pv = {}

        def rec_S(n):
            h, j, i = tiles[n]
            p = h % 2
            sb_ = s_ps[n % 3]
            r = i - 4 * j
            w = [head_ev[h], exp_ev[n - 3] if n >= 3 else None, e_id, e_mt]
            if r < 0:
                s_ev[n] = P.op("pe", lambda e: e.matmul(sb_[:, :], lhsT=Ka[p][0:70, i * 128:(i + 1) * 128],
                                                        rhs=Qa[p][0:70, j * 512:(j + 1) * 512], start=True, stop=True),
                               w, sig=True)
            else:
                c0 = 128 * r
                P.op("pe", lambda e: e.matmul(sb_[:, c0:c0 + 128], lhsT=ident[:], rhs=mtri[:], start=True, stop=False), w)
                ev = P.op("pe", lambda e: e.matmul(sb_[:, c0:c0 + 128], lhsT=Ka[p][0:70, i * 128:(i + 1) * 128],
                                                   rhs=Qa[p][0:70, j * 512 + c0:j * 512 + c0 + 128], start=False, stop=True),
                          [], sig=(True if r == 3 else None))
                if r < 3:
                    ev = P.op("pe", lambda e: e.matmul(sb_[:, c0 + 128:512], lhsT=Ka[p][0:70, i * 128:(i + 1) * 128],
                                                       rhs=Qa[p][0:70, j * 512 + c0 + 128:(j + 1) * 512], start=True, stop=True),
                              [], sig=True)
                s_ev[n] = ev

        def rec_exp(n):
            h, j, i = tiles[n]
            r = i - 4 * j
            c0 = 0 if r < 0 else 128 * r
            sb_ = s_ps[n % 3]
            pt = Pt[n % 3]
            exp_ev[n] = P.op("act", lambda e: e.activation(out=pt[:, c0:512], in_=sb_[:, c0:512], func=AF.Exp, scale=0.125),
                             [s_ev[n], pv_ev.get(n - 3)], sig=True, chain=False)

        def rec_PV(n):
            h, j, i = tiles[n]
            p = h % 2
            r = i - 4 * j
            c0 = 0 if r < 0 else 128 * r
            pt = Pt[n % 3]
            qn = h * 8 + j
            ob = o_ps[qn % 2]
            first = (i == 0)
            lastk = (i == 4 * j + 3)
            w = [exp_ev[n]]
            if first and qn >= 2:
                w.append(norm_ev[qn - 2])
            ev = P.op("pe", lambda e: e.matmul(ob[:, c0:512], lhsT=Va[p][:, i, :], rhs=pt[:, c0:512], start=first, stop=lastk),
                      w, sig=(True if lastk else None))
            if lastk:
                rb = rec[qn % 2]
                P.op("dve", lambda e: e.reciprocal(out=rb[64:128, :], in_=ob[64:128, :]), [ev])
                po = (h % 2) * 64
                norm_ev[qn] = P.op("dve", lambda e: e.tensor_tensor(
                    out=OT_all[po:po + 64, h // 2, j * 512:(j + 1) * 512], in0=ob[0:64, :], in1=rb[64:128, :], op=ALU.mult),
                    [], sig=True)
                if j == 7:
                    head_done[h] = ev
                    if h + 2 < NH:
                        head_ev[h + 2] = load_head(h + 2)

        LA = 2
        for n in range(min(LA, NTL)):
            rec_S(n)
        for n in range(NTL):
            rec_exp(n)
            if n + LA < NTL:
                rec_S(n + LA)
            rec_PV(n)


def ph_oproj(nc, T, OT_all):
    with Phase(nc, "op") as P:
        wo = P.sb("wo", [128, 8, 1024], BF16)
        gt = P.sb("gt", [128, 1024], F32)
        xt = [P.sb(f"xt{i}", [128, 1024], F32) for i in range(3)]
        t1 = [P.sb(f"t1{i}", [128, 1024], F32) for i in range(2)]
        x1 = [P.sb(f"x1{i}", [128, 1024], F32) for i in range(2)]
        y_ps = [P.ps(f"y{i}", [128, 1024]) for i in range(2)]
        s_c = P.dsem("c")
        s_x = [P.dsem(f"x{i}") for i in range(3)]
        s_o = [P.dsem(f"o{i}") for i in range(2)]
        e_w = P.dma1("pool", wo[:], T["w_o"].ap().rearrange("(c p) n -> p c n", p=128))
        e_g = P.dma1("sp", gt[:], T["mod_d"][0, 2048:3072].partition_broadcast(128))
        xt_free = [None] * 3
        t1_free = [None] * 2
        x1_free = [None] * 2
        y_free = [None] * 2
        for tt in range(NT):
            xb = xt[tt % 3]
            e_x = P.dma("sp", xb[:], T["x"][tt * 128:(tt + 1) * 128, :], waits=[xt_free[tt % 3]], sem=s_x[tt % 3])
            yb = y_ps[tt % 2]
            ev = None
            for nh in range(2):
                for c in range(8):
                    ev = P.op("pe", lambda e, nh=nh, c=c, yb=yb, tt=tt: e.matmul(
                        yb[:, nh * 512:(nh + 1) * 512], lhsT=OT_all[:, c, tt * 128:(tt + 1) * 128],
                        rhs=wo[:, c, nh * 512:(nh + 1) * 512], start=(c == 0), stop=(c == 7)),
                        [e_w, y_free[tt % 2]] if (c == 0 and nh == 0) else [], sig=(True if (c == 7 and nh == 1) else None))
            tb = t1[tt % 2]
            e_t = P.op("dve", lambda e, tb=tb, yb=yb: e.tensor_tensor(out=tb[:], in0=yb[:, :], in1=gt[:], op=ALU.mult),
                       [ev, e_g, t1_free[tt % 2]], sig=True)
            y_free[tt % 2] = e_t
            ob = x1[tt % 2]
            e_a = P.op("dve", lambda e, tb=tb, xb=xb, ob=ob: e.tensor_tensor(out=ob[:], in0=tb[:], in1=xb[:], op=ALU.add),
                       [e_t, e_x, x1_free[tt % 2]], sig=True)
            t1_free[tt % 2] = e_a
            xt_free[tt % 3] = e_a
            x1_free[tt % 2] = P.dma("sp", T["X1_d"][tt * 128:(tt + 1) * 128, :], ob[:], waits=[e_a], sem=s_o[tt % 2])


def ph_front(nc, T, l, R, Xin):
    with Phase(nc, f"fr{l}") as P:
        s_c = P.dsem("c")
        s_x = [P.dsem(f"x{i}") for i in range(3)]
        s_sc = P.dsem("sc")
        a_f, b_f, ev_ab = make_ab(P, T, l, "gffn_col", 3, 4, s_c)
        rw = P.sb("rw", [128, 8, 32], F32)
        rwp = P.sb("rwp", [128, 8, 32], F32)
        rconst = P.sb("rconst", [1, 32], F32)
        ones_row = P.sb("ones_row", [1, 128], F32)
        identf = P.sb("identf", [128, 128], F32)
        ltri = P.sb("ltri", [128, 128], F32)
        onesf = P.sb("onesf", [128, 128], F32)
        rbias = P.sb("rbias", [128, 32], F32)
        thr = P.sb("thr", [128, 16], F32)
        biota = P.sb("biota", [128, 64], F32)
        ones32 = P.sb("ones32", [128, 32], F32)
        xt = [P.sb(f"xt{i}", [128, 1024], F32) for i in range(3)]
        xnf = [P.sb(f"xnf{i}", [128, 1024], F32) for i in range(2)]
        xnT = [P.sb(f"xnT{i}", [128, 8, 128], F32) for i in range(2)]
        junk = P.sb("junk", [128, 1024], BF16)
        xn_all = P.sb("xn_all", [128, 32, 1024], BF16)
        ss = P.sb("ss", [128, 32], F32)
        sd = P.sb("sd", [128, 32], F32)
        rstd = P.sb("rstd", [128, 32], F32)
        Mall = P.sb("Mall", [128, 32, 32], F32)
        Gall = P.sb("Gall", [128, 32, 32], F32)
        posl = P.sb("posl", [128, 32, 32], F32)
        CSb = P.sb("CSb", [128, 32, 32], F32)
        carry = P.sb("carry", [128, 32, 32], F32)
        W1 = P.sb("W1", [128, 32, 32], F32)
        W2 = P.sb("W2", [128, 32, 32], F32)
        cmpb = P.sb("cmpb", [128, 64, 32], F32)
        scores = [P.sb(f"scores{i}", [128, 32], F32) for i in range(2)]
        sel = P.sb("sel", [128, 32], F32)
        mx = P.sb("mx", [128, 4, 8], F32)
        gs = P.sb("gs", [128, 4], F32)
        gmax = P.sb("gmax", [128, 1], F32)
        gm = P.sb("gm", [128, 4], F32)
        pen = P.sb("pen", [128, 4], F32)
        selm = P.sb("selm", [128, 32], F32)
        t8 = P.sb("t8", [128, 8], F32)
        sm = P.sb("sm", [128, 32], F32)
        den = P.sb("den", [128, 1], F32)
        rden = P.sb("rden", [128, 1], F32)
        cnt = P.sb("cnt", [128, 32], F32)
        nb = P.sb("nb", [128, 32], F32)
        pend = P.sb("pend", [128, 32], F32)
        pst = P.sb("pst", [128, 32], F32)
        da1 = P.sb("da1", [128, 32], F32)
        sumD = P.sb("sumD", [128, 32], F32)
        db1 = P.sb("db1", [128, 32], F32)
        gsum = P.sb("gsum", [128, 32], F32)
        bef = P.sb("bef", [128, 64], F32)
        tpf = P.ps("tpf", [128, 8, 128], F32)
        lg_ps = [P.ps(f"lg{i}", [128, 32]) for i in range(2)]
        pos_ps = [P.ps(f"pos{i}", [128, 32]) for i in range(2)]
        cs_ps = P.ps("cs", [128, 1024])

        zt = P.sb("zt", [128, 8192], BF16)
        s_z = P.dsem("z")
        e_zm = P.op("pool", lambda e: e.memset(zt[:], 0.0), [], sig=True)
        xgv = T["XG_d"].ap().rearrange("(p j n) d -> p j (n d)", p=128, j=16)
        e_z = None
        for j in range(16):
            e_z = P.dma("sp", xgv[:, j, :], zt[:], waits=[e_zm], sem=s_z)
        e_c = []
        for (t_, nm) in ((rw, "rw"), (identf, "identf"), (ltri, "ltri"), (onesf, "ones"), (rbias, "rbias_row"),
                         (thr, "thr"), (biota, "biota")):
            e_c.append(P.dma("sp", t_[:], T[nm].ap(), sem=s_c))
        P.op("dve", lambda e: e.memset(ones_row[:], 1.0))
        P.op("dve", lambda e: e.memset(ones32[:], 1.0))
        ev = None
        for kc in range(8):
            ev = P.op("dve", lambda e, kc=kc: e.tensor_scalar(out=rwp[:, kc, :], in0=rw[:, kc, :], scalar1=a_f[:, kc:kc + 1],
                                                              scalar2=None, op0=ALU.mult), e_c + [ev_ab], sig=True)
        e_rwp = ev
        for kc in range(8):
            ev = P.op("pe", lambda e, kc=kc: e.matmul(cs_ps[0:1, 0:32], lhsT=b_f[:, kc:kc + 1], rhs=rw[:, kc, :],
                                                      start=(kc == 0), stop=(kc == 7)), e_c + [ev_ab] if kc == 0 else [],
                      sig=(True if kc == 7 else None))
        e_rc = P.op("dve", lambda e: e.tensor_copy(out=rconst[:], in_=cs_ps[0:1, 0:32]), [ev], sig=True)

        FS = DBG.get("front_stop", 99)
        xt_free = [[] for _ in range(3)]
        xnf_rd = [None] * 2
        xnT_rd = [None] * 2
        tpf_free = []
        lg_free = [None] * 2
        sc_free = [None] * 2
        pos_free = [None] * 2
        sig_ev = {}
        xnf_rd2 = [None] * 2

        def stageA(tt):
            nonlocal tpf_free
            xb = xt[tt % 3]
            e_x = P.dma("sp", xb[:], Xin[tt * 128:(tt + 1) * 128, :], waits=xt_free[tt % 3], sem=s_x[tt % 3])
            e1 = P.op("act", lambda e: e.activation(out=junk[:], in_=xb[:], func=AF.Square, accum_out=ss[:, tt:tt + 1]),
                      [e_x], sig=True)
            e2 = P.op("act", lambda e: e.activation(out=sd[:, tt:tt + 1], in_=ss[:, tt:tt + 1], func=AF.Sqrt,
                                                    bias=EPS, scale=1.0 / D), [], sig=True)
            P.op("dve", lambda e: e.reciprocal(out=rstd[:, tt:tt + 1], in_=sd[:, tt:tt + 1]), [e2])
            xf = xnf[tt % 2]
            e3 = P.op("dve", lambda e: e.tensor_scalar(out=xf[:], in0=xb[:], scalar1=rstd[:, tt:tt + 1], scalar2=None,
                                                       op0=ALU.mult), [xnf_rd[tt % 2], xnf_rd2[tt % 2]], sig=True)
            e4 = P.op("act", lambda e: e.activation(out=xn_all[:, tt, :], in_=xf[:], func=AF.Copy), [e3], sig=True)
            xt_free[tt % 3] = [e3, e1]
            xnf_rd2[tt % 2] = e4
            ev = None
            for kc in range(8):
                ev = P.op("pe", lambda e, kc=kc: e.transpose(tpf[:, kc, :], xf[:, kc * 128:(kc + 1) * 128], identf[:]),
                          ([e3] + e_c + tpf_free) if kc == 0 else [], sig=(True if kc == 7 else None))
            xnf_rd[tt % 2] = ev
            xT = xnT[tt % 2]
            ea = P.op("act", lambda e: e.activation(out=xT[:, :, :], in_=tpf[:, :, :], func=AF.Copy),
                      [ev, xnT_rd[tt % 2]], sig=True)
            ed = ea
            tpf_free = [ea, ed]
            lg = lg_ps[tt % 2]
            for kc in range(8):
                P.op("pe", lambda e, kc=kc: e.matmul(lg[:, :], lhsT=xT[:, kc, :], rhs=rwp[:, kc, :], start=(kc == 0), stop=False),
                     [ea, ed, e_rwp, e_rc, lg_free[tt % 2]] if kc == 0 else [])
            ev = P.op("pe", lambda e: e.matmul(lg[:, :], lhsT=ones_row[0:1, :], rhs=rconst[0:1, :], start=False, stop=True),
                      [], sig=True)
            xnT_rd[tt % 2] = ev
            sc = scores[tt % 2]
            e_s = P.op("act", lambda e: e.activation(out=sc[:], in_=lg[:, :], func=AF.Sigmoid), [ev, sc_free[tt % 2]], sig=True)
            lg_free[tt % 2] = e_s
            sig_ev[tt] = e_s

        def stageB(tt):
            sc = scores[tt % 2]
            e_s = sig_ev[tt]
            P.op("dve", lambda e: e.tensor_tensor(out=sel[:], in0=sc[:], in1=rbias[:], op=ALU.add), [e_s])
            for g in range(4):
                P.op("dve", lambda e, g=g: e.max(out=mx[:, g, :], in_=sel[:, g * 8:(g + 1) * 8]))
            P.op("dve", lambda e: e.tensor_tensor(out=gs[:], in0=mx[:, :, 0], in1=mx[:, :, 1], op=ALU.add))
            P.op("dve", lambda e: e.tensor_reduce(out=gmax[:], in_=gs[:], axis=AX.X, op=ALU.max))
            P.op("dve", lambda e: e.tensor_scalar(out=gm[:], in0=gs[:], scalar1=gmax[:, 0:1], scalar2=None, op0=ALU.is_equal))
            P.op("dve", lambda e: e.tensor_scalar(out=pen[:], in0=gm[:], scalar1=-1.0, scalar2=1e9, op0=ALU.add, op1=ALU.mult))
            P.op("dve", lambda e: e.tensor_tensor(out=selm[:].rearrange("p (g k) -> p g k", g=4),
                                                  in0=sel[:].rearrange("p (g k) -> p g k", g=4),
                                                  in1=pen[:].unsqueeze(2).to_broadcast([128, 4, 8]), op=ALU.add))
            P.op("dve", lambda e: e.max(out=t8[:], in_=selm[:]))
            e_M = P.op("dve", lambda e: e.tensor_scalar(out=Mall[:, tt, :], in0=selm[:], scalar1=t8[:, 1:2], scalar2=None,
                                                        op0=ALU.is_ge), [], sig=True)
            P.op("dve", lambda e: e.tensor_tensor(out=sm[:], in0=Mall[:, tt, :], in1=sc[:], op=ALU.mult))
            P.op("dve", lambda e: e.tensor_reduce(out=den[:], in_=sm[:], axis=AX.X, op=ALU.add))
            P.op("dve", lambda e: e.reciprocal(out=rden[:], in_=den[:]))
            sc_free[tt % 2] = P.op("dve", lambda e: e.tensor_scalar(out=Gall[:, tt, :], in0=sm[:], scalar1=rden[:, 0:1],
                                                                  scalar2=None, op0=ALU.mult), [], sig=True)
            pp = pos_ps[tt % 2]
            ev = P.op("pe", lambda e: e.matmul(pp[:, :], lhsT=ltri[:], rhs=Mall[:, tt, :], start=True, stop=True),
                      [e_M, pos_free[tt % 2]], sig=True)
            pos_free[tt % 2] = P.op("act", lambda e: e.activation(out=posl[:, tt, :], in_=pp[:, :], func=AF.Copy), [ev], sig=True)

        stageA(0)
        for tt in range(NT):
            if tt + 1 < NT:
                stageA(tt + 1)
            stageB(tt)
        if FS <= 4:
            return
        e_pos = pos_free[(NT - 1) % 2]
        Mf = Mall[:].rearrange("p a b -> p (a b)")
        ev = None
        for h2 in range(2):
            ev = P.op("pe", lambda e, h2=h2: e.matmul(cs_ps[:, h2 * 512:(h2 + 1) * 512], lhsT=onesf[:],
                                                      rhs=Mf[:, h2 * 512:(h2 + 1) * 512], start=True, stop=True),
                      [sc_free[(NT - 1) % 2]], sig=(True if h2 == 1 else None))
        P.op("dve", lambda e: e.tensor_copy(out=CSb[:].rearrange("p a b -> p (a b)"), in_=cs_ps[:, :]), [ev, e_pos])
        P.op("dve", lambda e: e.memset(carry[:, 0, :], 0.0))
        for tt in range(1, NT):
            P.op("dve", lambda e, tt=tt: e.tensor_tensor(out=carry[:, tt, :], in0=carry[:, tt - 1, :], in1=CSb[:, tt - 1, :],
                                                         op=ALU.add))
        P.op("dve", lambda e: e.tensor_tensor(out=cnt[:], in0=carry[:, NT - 1, :], in1=CSb[:, NT - 1, :], op=ALU.add))
        P.op("dve", lambda e: e.tensor_tensor(out=W1[:, :, 0:16], in0=cnt[:].unsqueeze(2).to_broadcast([128, 32, 16]),
                                              in1=thr[:].unsqueeze(1).to_broadcast([128, 32, 16]), op=ALU.is_gt))
        P.op("dve", lambda e: e.tensor_reduce(out=nb[:], in_=W1[:, :, 0:16], axis=AX.X, op=ALU.add))
        P.op("dve", lambda e: e.tensor_tensor_scan(out=pend[:], data0=ones32[:], data1=nb[:], initial=0.0,
                                                   op0=ALU.mult, op1=ALU.add))
        P.op("dve", lambda e: e.tensor_tensor(out=pst[:], in0=pend[:], in1=nb[:], op=ALU.subtract))
        P.op("dve", lambda e: e.tensor_scalar(out=pst[:], in0=pst[:], scalar1=float(BLK), scalar2=None, op0=ALU.mult))
        P.op("dve", lambda e: e.tensor_tensor(out=W1[:], in0=posl[:], in1=carry[:], op=ALU.add))
        P.op("dve", lambda e: e.tensor_tensor(out=W1[:], in0=W1[:], in1=pst[:].unsqueeze(1).to_broadcast([128, 32, 32]),
                                              op=ALU.add))
        P.op("dve", lambda e: e.scalar_tensor_tensor(out=W1[:], in0=W1[:], scalar=1.0, in1=Mall[:], op0=ALU.add, op1=ALU.mult))
        P.op("dve", lambda e: e.tensor_reduce(out=da1[:], in_=W1[:], axis=AX.X, op=ALU.max))
        P.op("dve", lambda e: e.tensor_reduce(out=sumD[:], in_=W1[:], axis=AX.X, op=ALU.add))
        P.op("dve", lambda e: e.tensor_tensor(out=db1[:], in0=sumD[:], in1=da1[:], op=ALU.subtract))
        dtmp = P.sb("dtmp", [128, 32], F32)
        for (src_, nm_) in ((da1, "destA"), (db1, "destB")):
            P.op("dve", lambda e, src_=src_: e.tensor_scalar(out=dtmp[:], in0=src_[:], scalar1=-1.0, scalar2=0.0,
                                                            op0=ALU.add, op1=ALU.max))
            P.op("dve", lambda e, nm_=nm_: e.tensor_scalar(out=R[nm_][:], in0=dtmp[:], scalar1=float(NBLK * BLK - 1),
                                                         scalar2=None, op0=ALU.min))
        P.op("dve", lambda e: e.tensor_tensor(out=W2[:], in0=W1[:], in1=da1[:].unsqueeze(2).to_broadcast([128, 32, 32]),
                                              op=ALU.is_equal))
        P.op("dve", lambda e: e.tensor_tensor(out=W2[:], in0=W2[:], in1=Gall[:], op=ALU.mult))
        P.op("dve", lambda e: e.tensor_reduce(out=R["gA"][:], in_=W2[:], axis=AX.X, op=ALU.add))
        P.op("dve", lambda e: e.tensor_reduce(out=gsum[:], in_=Gall[:], axis=AX.X, op=ALU.add))
        P.op("dve", lambda e: e.tensor_tensor(out=R["gB"][:], in0=gsum[:], in1=R["gA"][:], op=ALU.subtract))
        P.op("dve", lambda e: e.tensor_tensor(out=cmpb[:], in0=pend[:].unsqueeze(1).to_broadcast([128, 64, 32]),
                                              in1=biota[:].unsqueeze(2).to_broadcast([128, 64, 32]), op=ALU.is_le))
        P.op("dve", lambda e: e.tensor_reduce(out=bef[:], in_=cmpb[:], axis=AX.X, op=ALU.add))
        e_tab = P.op("dve", lambda e: e.tensor_scalar(out=R["blk_e"][:], in0=bef[:], scalar1=31.0, scalar2=None, op0=ALU.min),
                     [], sig=True)
        piota = P.sb("piota", [128, 1], F32)
        e_pi = P.dma1("sp", piota[:], T["piota"].ap())
        e_tab = P.op("dve", lambda e: e.tensor_scalar(out=R["idxW"][:], in0=R["blk_e"][:], scalar1=128.0, scalar2=piota[:, 0:1],
                                                      op0=ALU.mult, op1=ALU.add), [e_pi], sig=True)
        if FS <= 5:
            return
        if "Ri_dbg" in T and l == DBG.get("dump_layer", 0):
            s_dbg = P.dsem("dbg")
            P.dma("sp", T["Ri_dbg"][0, :, :], R["destA"][:], waits=[e_tab], sem=s_dbg)
            P.dma("sp", T["Ri_dbg"][1, :, :], R["destB"][:], waits=[e_tab], sem=s_dbg)
            P.dma("sp", T["Rf_dbg"][0, :, 0:32], R["gA"][:], waits=[e_tab], sem=s_dbg)
            P.dma("sp", T["Rf_dbg"][1, :, 0:32], R["gB"][:], waits=[e_tab], sem=s_dbg)
            P.dma("sp", T["Rf_dbg"][2, :, :], R["blk_e"][:], waits=[e_tab], sem=s_dbg)
        if FS <= 6:
            return
        breg = {}

        def bc(e):
            if "r" not in breg:
                breg["r"] = e.alloc_register("bc_" + P.name)
                e.reg_mov(breg["r"], NBLK * BLK - 1)
            return breg["r"]
        s_sc4 = [s_sc, P.dsem("sc2"), P.dsem("sc3"), P.dsem("sc4")]
        sc_prev = [None] * 4
        for tt in range(NT):
            for i_, nm in enumerate(("destA", "destB")):
                k_ = (tt % 2) * 2 + i_
                sc_prev[k_] = P.op("pool", lambda e, tt=tt, nm=nm: e.indirect_dma_start(
                    out=T["XG_d"][:, :], out_offset=bass.IndirectOffsetOnAxis(ap=R[nm][:, tt:tt + 1], axis=0),
                    in_=xn_all[:, tt, :], in_offset=None),
                    [e_tab, P.last.get("pool"), P.last.get("act"), e_z, sc_prev[k_]], sig=s_sc4[k_], inc=16)


def ph_moe(nc, T, l, R):
    with Phase(nc, f"moe{l}") as P:
        s_c = P.dsem("c")
        s_w = [[P.dsem(f"w{i}_{j}") for j in range(3)] for i in range(2)]
        s_xr = [P.dsem(f"xr{i}") for i in range(2)]
        s_y = [P.dsem(f"y{i}") for i in range(2)]
        sh = P.sb("sh", [128, 8], F32)
        sc = P.sb("sc", [128, 8], F32)
        g = P.sb("g", [128, 8], F32)
        a_f = P.sb("a_f", [128, 8], F32)
        e1 = P.dma("sp", sh[:], T["mod_d"][l, 3 * 1024:4 * 1024].rearrange("(p k) -> p k", k=8), sem=s_c)
        e2 = P.dma("sp", sc[:], T["mod_d"][l, 4 * 1024:5 * 1024].rearrange("(p k) -> p k", k=8), sem=s_c)
        e3 = P.dma("sp", g[:], T["gffn_pk"][:, l, :], sem=s_c)
        ev_ab = P.op("dve", lambda e: e.scalar_tensor_tensor(out=a_f[:], in0=sc[:], scalar=1.0, in1=g[:],
                                                            op0=ALU.add, op1=ALU.mult), [e1, e2, e3], sig=True)
        b_f = sh
        ident = P.sb("ident", [128, 128], BF16)
        e_id = P.dma1("pool", ident[:], T["ident"].ap())
        w1b = [P.sb(f"w1b{i}", [128, 8, 512], BF16) for i in range(2)]
        w3b = [P.sb(f"w3b{i}", [128, 8, 512], BF16) for i in range(2)]
        w2b = [P.sb(f"w2b{i}", [128, 4, 1024], BF16) for i in range(2)]
        xr = [P.sb(f"xr{i}", [128, 2, 1024], BF16) for i in range(2)]
        hT = [P.sb(f"hT{i}", [128, 8, 256], BF16) for i in range(2)]
        sa = [P.sb(f"sa{i}", [128, 256], F32) for i in range(2)]
        hid = [P.sb(f"hid{i}", [128, 4, 256], BF16) for i in range(2)]
        yst = [P.sb(f"yst{i}", [128, 2, 1024], F32) for i in range(2)]
        tp = P.ps("tp", [128, 16, 128], BF16)
        ab_ps = [P.ps(f"ab{i}", [128, 2, 256]) for i in range(2)]
        y_ps = [P.ps(f"y{i}", [128, 512]) for i in range(2)]
        w1v = T[f"w1_{l}"].ap().rearrange("e (p k) n -> (e p) (k n)", k=8)
        w3v = T[f"w3_{l}"].ap().rearrange("e (p k) n -> (e p) (k n)", k=8)
        w2v = T[f"w2_{l}"].ap().rearrange("e (p k) n -> (e p) (k n)", k=4)

        wb_free = [None] * 2
        xr_free = [None] * 2
        hT_free = [None] * 2
        tp_free = []
        ab_free = [None] * 2
        sa_free = [None] * 2
        hid_free = [None] * 2
        yps_free = [None] * 2
        yst_free = [None] * 2
        ny = 0
        nab = 0
        breg = {}

        def bc(e):
            if "r" not in breg:
                breg["r"] = e.alloc_register("bcw_" + P.name)
                e.reg_mov(breg["r"], E * 128 - 1)
            return breg["r"]

        def load_w(b):
            pb = b % 2
            evs_ = []
            prev = w_ev.get(b - 1) or []
            for j_, (src, dst) in enumerate(((w1v, w1b[pb]), (w3v, w3b[pb]), (w2v, w2b[pb]))):
                flat = dst[:].rearrange("p k n -> p (k n)")
                evs_.append(P.op("pool", lambda e, src=src, flat=flat: e.indirect_dma_start(
                    out=flat, out_offset=None, in_=src,
                    in_offset=bass.IndirectOffsetOnAxis(ap=R["idxW"][:, b:b + 1], axis=0),
                    ), [wb_free[pb]] + list(prev), sig=s_w[pb][j_], inc=16))
            return evs_

        def load_x(b):
            pb = b % 2
            return P.dma("sp", xr[pb][:], T["XG_d"][b * BLK:(b + 1) * BLK, :].rearrange("(s p) d -> p s d", p=128),
                         waits=[xr_free[pb]], sem=s_xr[pb])

        w_ev = {}
        w_ev[0] = load_w(0)
        x_ev = {0: load_x(0)}

        def do_block(b):
            nonlocal ny, nab, tp_free
            pb = b % 2
            if b + 1 < NBLK:
                w_ev[b + 1] = load_w(b + 1)
                x_ev[b + 1] = load_x(b + 1)
            xb = xr[pb]
            ev = None
            for s_ in range(2):
                xv = xb[:, s_, :].rearrange("p (q k) -> p k q", k=8)
                for kc in range(8):
                    first = (s_ == 0 and kc == 0)
                    lastt = (s_ == 1 and kc == 7)
                    ev = P.op("pe", lambda e, s_=s_, kc=kc, xv=xv: e.transpose(tp[:, s_ * 8 + kc, :], xv[:, kc, :], ident[:]),
                              ([x_ev[b], e_id] + tp_free) if first else [], sig=(True if lastt else None))
            xr_free[pb] = ev
            hb = hT[pb]
            tpv = tp[:].rearrange("p (s k) t -> p s k t", s=2)
            ea = ed = None
            for kc in range(8):
                dst = hb[:, kc, :].rearrange("p (s t) -> p s t", s=2)
                ed = P.op("dve", lambda e, dst=dst, kc=kc: e.tensor_scalar(out=dst, in0=tpv[:, :, kc, :],
                                                                          scalar1=a_f[:, kc:kc + 1], scalar2=b_f[:, kc:kc + 1],
                                                                          op0=ALU.mult, op1=ALU.add),
                          [ev, ev_ab, hT_free[pb]], sig=True)
                ea = ed
            tp_free = [ea, ed]
            hd = hid[pb]
            w1s = w1b[pb][:].rearrange("p k (q f) -> p k f q", f=4)
            w3s = w3b[pb][:].rearrange("p k (q f) -> p k f q", f=4)
            e_h = None
            for fc in range(4):
                ab = ab_ps[nab % 2]
                for kc in range(8):
                    P.op("pe", lambda e, ab=ab, kc=kc, fc=fc: e.matmul(ab[:, 0, :], lhsT=w1s[:, kc, fc, :],
                                                                      rhs=hb[:, kc, :], start=(kc == 0), stop=(kc == 7)),
                         ([ea, ed, ab_free[nab % 2]] + w_ev[b]) if kc == 0 else [])
                for kc in range(8):
                    ev = P.op("pe", lambda e, ab=ab, kc=kc, fc=fc: e.matmul(ab[:, 1, :], lhsT=w3s[:, kc, fc, :],
                                                                           rhs=hb[:, kc, :], start=(kc == 0), stop=(kc == 7)),
                              [], sig=(True if kc == 7 else None))
                sab = sa[nab % 2]
                e_s = P.op("act", lambda e, sab=sab, ab=ab: e.activation(out=sab[:], in_=ab[:, 0, :], func=AF.Silu),
                           [ev, sa_free[nab % 2]], sig=True)
                e_h = P.op("dve", lambda e, sab=sab, ab=ab, fc=fc: e.tensor_tensor(out=hd[:, fc, :], in0=sab[:], in1=ab[:, 1, :],
                                                                                  op=ALU.mult),
                           [e_s, ev, hid_free[pb]], sig=True)
                ab_free[nab % 2] = e_h
                sa_free[nab % 2] = e_h
                nab += 1
            hT_free[pb] = ev
            ysb = yst[pb]
            evs = []
            for sub in range(2):
                for nh in range(2):
                    yp = y_ps[ny % 2]
                    for fc in range(4):
                        ev = P.op("pe", lambda e, yp=yp, fc=fc, sub=sub, nh=nh: e.matmul(
                            yp[:, :], lhsT=hd[:, fc, sub * 128:(sub + 1) * 128], rhs=w2b[pb][:, fc, nh * 512:(nh + 1) * 512],
                            start=(fc == 0), stop=(fc == 3)),
                            [e_h, yps_free[ny % 2]] if fc == 0 else [], sig=(True if fc == 3 else None))
                    dst = ysb[:, sub, nh * 512:(nh + 1) * 512]
                    if ny % 2 == 0:
                        e_y = P.op("act", lambda e, dst=dst, yp=yp: e.activation(out=dst, in_=yp[:, :], func=AF.Copy),
                                   [ev, yst_free[pb]], sig=True)
                    else:
                        e_y = P.op("dve", lambda e, dst=dst, yp=yp: e.tensor_copy(out=dst, in_=yp[:, :]),
                                   [ev, yst_free[pb]], sig=True)
                    yps_free[ny % 2] = e_y
                    evs.append(e_y)
                    ny += 1
            hid_free[pb] = ev
            wb_free[pb] = ev
            yst_free[pb] = P.dma("sp", T["YG_d"][b * BLK:(b + 1) * BLK, :].rearrange("(s p) d -> p s d", p=128), ysb[:],
                                 waits=evs[-2:], sem=s_y[pb])

        for b in range(NBLK):
            do_block(b)


def ph_comb(nc, T, l, R, Xin, Xout, final):
    with Phase(nc, f"cb{l}") as P:
        s_c = P.dsem("c")
        s_x = [P.dsem(f"x{i}") for i in range(3)]
        s_g = [[P.dsem(f"g{i}_{j}") for j in range(2)] for i in range(4)]
        s_o = [P.dsem(f"o{i}") for i in range(2)]
        gt = P.sb("gt", [128, 1024], F32)
        e_g = P.dma1("sp", gt[:], T["mod_d"][l, 5 * 1024:6 * 1024].partition_broadcast(128))
        gfin = None
        if final:
            gfin = P.sb("gfin", [128, 1024], F32)
            e_g2 = P.dma1("sp", gfin[:], T["gfin_row"].ap())
            junk = P.sb("junk", [128, 1024], BF16)
            ss = P.sb("ss", [128, 32], F32)
            sd = P.sb("sd", [128, 32], F32)
            rstd = P.sb("rstd", [128, 32], F32)
            ob = [P.sb(f"ob{i}", [128, 1024], F32) for i in range(2)]
        xt = [P.sb(f"xt{i}", [128, 1024], F32) for i in range(3)]
        ya = [P.sb(f"ya{i}", [128, 1024], F32) for i in range(4)]
        yb = [P.sb(f"yb{i}", [128, 1024], F32) for i in range(4)]
        tm = [P.sb(f"tm{i}", [128, 1024], F32) for i in range(2)]
        xo = [P.sb(f"xo{i}", [128, 1024], F32) for i in range(2)]
        xt_free = [None] * 3
        y_free = [None] * 4
        tm_free = [None] * 2
        xo_free = [None] * 2
        ob_free = [None] * 2
        gath = {}

        def issue_gather(tt):
            q = tt % 4
            evs_ = []
            for j_, (nm, dst) in enumerate((("destA", ya[q]), ("destB", yb[q]))):
                evs_.append(P.op("pool", lambda e, nm=nm, dst=dst: e.indirect_dma_start(
                    out=dst[:, :], out_offset=None, in_=T["YG_d"][:, :],
                    in_offset=bass.IndirectOffsetOnAxis(ap=R[nm][:, tt:tt + 1], axis=0),
                    ), [y_free[q]], sig=s_g[q][j_], inc=16))
            gath[tt] = evs_

        for t0 in range(3):
            issue_gather(t0)
        breg = {}

        def bc(e):
            if "r" not in breg:
                breg["r"] = e.alloc_register("bc_" + P.name)
                e.reg_mov(breg["r"], NBLK * BLK - 1)
            return breg["r"]
        for tt in range(NT):
            p = tt % 2
            xb = xt[tt % 3]
            e_x = P.dma("sp", xb[:], Xin[tt * 128:(tt + 1) * 128, :], waits=[xt_free[tt % 3]], sem=s_x[tt % 3])
            if tt + 3 < NT:
                issue_gather(tt + 3)
            e_gas = gath[tt]
            q = tt % 4
            tb = tm[p]
            P.op("dve", lambda e, tb=tb, q=q, tt=tt: e.tensor_scalar(out=tb[:], in0=ya[q][:], scalar1=R["gA"][:, tt:tt + 1],
                                                                   scalar2=None, op0=ALU.mult), e_gas + [tm_free[p]])
            P.op("dve", lambda e, tb=tb, q=q, tt=tt: e.scalar_tensor_tensor(out=tb[:], in0=yb[q][:], scalar=R["gB"][:, tt:tt + 1],
                                                                          in1=tb[:], op0=ALU.mult, op1=ALU.add))
            e_t = P.op("dve", lambda e, tb=tb: e.tensor_tensor(out=tb[:], in0=tb[:], in1=gt[:], op=ALU.mult), [e_g], sig=True)
            y_free[q] = e_t
            xob = xo[p]
            e_a = P.op("dve", lambda e, tb=tb, xb=xb, xob=xob: e.tensor_tensor(out=xob[:], in0=tb[:], in1=xb[:], op=ALU.add),
                       [e_t, e_x, xo_free[p]], sig=True)
            tm_free[p] = e_a
            xt_free[tt % 3] = e_a
            if not final:
                xo_free[p] = P.dma("sp", Xout[tt * 128:(tt + 1) * 128, :], xob[:], waits=[e_a], sem=s_o[p])
            else:
                e1 = P.op("act", lambda e, xob=xob, tt=tt: e.activation(out=junk[:], in_=xob[:], func=AF.Square,
                                                                        accum_out=ss[:, tt:tt + 1]), [e_a], sig=True)
                e2 = P.op("act", lambda e, tt=tt: e.activation(out=sd[:, tt:tt + 1], in_=ss[:, tt:tt + 1], func=AF.Sqrt,
                                                               bias=EPS, scale=1.0 / D), [], sig=True)
                P.op("dve", lambda e, tt=tt: e.reciprocal(out=rstd[:, tt:tt + 1], in_=sd[:, tt:tt + 1]), [e2])
                obb = ob[p]
                e_o = P.op("dve", lambda e, obb=obb, xob=xob, tt=tt: e.scalar_tensor_tensor(
                    out=obb[:], in0=xob[:], scalar=rstd[:, tt:tt + 1], in1=gfin[:], op0=ALU.mult, op1=ALU.mult),
                    [e_g2, ob_free[p]], sig=True)
                xo_free[p] = e_o
                ob_free[p] = P.dma("sp", Xout[tt * 128:(tt + 1) * 128, :], obb[:], waits=[e_o], sem=s_o[p])


def ph_pool(nc, T, Xin, Xout):
    with Phase(nc, "pl") as P:
        s_c = P.dsem("c")
        s_x = [P.dsem(f"x{i}") for i in range(6)]
        s_o = [P.dsem(f"o{i}") for i in range(2)]
        a_m, b_m, ev_ab = make_ab(P, T, 1, "gmix_col", 0, 1, s_c)
        identf = P.sb("identf", [128, 128], F32)
        wp = P.sb("wp", [128, 4, 2, 256], BF16)
        cg = P.sb("cg", [128, 1024], F32)
        gtr = P.sb("gtr", [128, 1024], F32)
        invfix = P.sb("invfix", [128, 4, 16], F32)
        fx = P.sb("fx", [128, 2, 16], F32)
        xt = [P.sb(f"xt{i}", [128, 1024], F32) for i in range(6)]
        xnf = [P.sb(f"xnf{i}", [128, 1024], F32) for i in range(2)]
        junk = P.sb("junk", [128, 1024], BF16)
        ss = P.sb("ss", [128, 32], F32)
        sd = P.sb("sd", [128, 32], F32)
        rstd = P.sb("rstd", [128, 32], F32)
        hT = [P.sb(f"hT{i}", [128, 8, 528], F32) for i in range(2)]
        S1 = P.sb("S1", [128, 8, 528], F32)
        S2 = P.sb("S2", [128, 6, 528], F32)
        S3 = P.sb("S3", [128, 4, 528], F32)
        S4 = P.sb("S4", [128, 2, 528], F32)
        pl = [P.sb(f"pl{i}", [128, 8, 512], BF16) for i in range(2)]
        t1 = [P.sb(f"t1{i}", [128, 1024], F32) for i in range(2)]
        x3 = [P.sb(f"x3{i}", [128, 1024], F32) for i in range(2)]
        tpf = [P.ps(f"tpf{i}", [128, 8, 128], F32) for i in range(2)]
        y_ps = [P.ps(f"y{i}", [128, 1024]) for i in range(2)]

        e_id = P.dma1("sp", identf[:], T["identf"].ap())
        e_wp = P.dma1("pool", wp[:], T["pool_w"].ap().rearrange("g (cc p) e -> p g cc e", p=128))
        e_a = P.dma1("sp", cg[:], T["pscale_row"].ap())
        e_b = P.dma1("sp", gtr[:], T["mod_d"][1, 2048:3072].partition_broadcast(128))
        e_if = P.dma1("sp", invfix[:], T["invfix"].ap())
        e_cg = P.op("dve", lambda e: e.tensor_tensor(out=cg[:], in0=cg[:], in1=gtr[:], op=ALU.mult), [e_a, e_b], sig=True)

        xt_free = [None] * 6
        xnf_rd = [None] * 2
        tpf_free = [[], []]
        hT_rd = [[], []]
        pl_rd = [None] * 2
        y_free = [None] * 2
        t1_free = [None] * 2
        x3_free = [None] * 2
        x_ev = {}
        halo_ev = [P.op("pool", lambda e: e.memset(hT[0][:, :, 0:16], 0.0), [], sig=True)]

        def super_tile(Tq):
            H = hT[Tq % 2]
            evs_h = []
            def front_sub(sub):
                nonlocal evs_h
                st = Tq * 4 + sub
                xb = xt[st % 6]
                e_x = P.dma("sp", xb[:], Xin[st * 128:(st + 1) * 128, :], waits=[xt_free[st % 6]], sem=s_x[st % 6])
                x_ev[st] = e_x
                e1 = P.op("act", lambda e: e.activation(out=junk[:], in_=xb[:], func=AF.Square, accum_out=ss[:, st:st + 1]),
                          [e_x], sig=True)
                e2 = P.op("act", lambda e: e.activation(out=sd[:, st:st + 1], in_=ss[:, st:st + 1], func=AF.Sqrt,
                                                        bias=EPS, scale=1.0 / D), [], sig=True)
                P.op("dve", lambda e: e.reciprocal(out=rstd[:, st:st + 1], in_=sd[:, st:st + 1]), [e2])
                xf = xnf[st % 2]
                e3 = P.op("dve", lambda e: e.tensor_scalar(out=xf[:], in0=xb[:], scalar1=rstd[:, st:st + 1], scalar2=None,
                                                           op0=ALU.mult), [xnf_rd[st % 2]], sig=True)
                tpb = tpf[st % 2]
                ev = None
                for kc in range(8):
                    ev = P.op("pe", lambda e, kc=kc: e.transpose(tpb[:, kc, :], xf[:, kc * 128:(kc + 1) * 128], identf[:]),
                              ([e3, e_id] + tpf_free[st % 2]) if kc == 0 else [], sig=(True if kc == 7 else None))
                xnf_rd[st % 2] = ev
                ea = ed = None
                for kc in range(8):
                    dst = H[:, kc, 16 + sub * 128:16 + (sub + 1) * 128]
                    ed = P.op("dve", lambda e, kc=kc, dst=dst: e.tensor_scalar(out=dst, in0=tpb[:, kc, :],
                                                                              scalar1=a_m[:, kc:kc + 1], scalar2=b_m[:, kc:kc + 1],
                                                                              op0=ALU.mult, op1=ALU.add),
                              [ev, ev_ab] + hT_rd[Tq % 2], sig=True)
                    ea = ed
                tpf_free[st % 2] = [ea, ed]
                evs_h = [ea, ed]

            for sub in range(4):
                front_sub(sub)
            P.op("dve", lambda e: e.tensor_tensor(out=S1[:, :, 1:528], in0=H[:, :, 1:528], in1=H[:, :, 0:527], op=ALU.add),
                 evs_h + [halo_ev[0]])
            e_s1 = P.op("dve", lambda e: e.tensor_tensor(out=S2[:, :, 3:528], in0=S1[:, 2:8, 3:528], in1=S1[:, 2:8, 1:526],
                                                         op=ALU.add), [], sig=True)
            P.op("pool", lambda e: e.tensor_tensor(out=S3[:, :, 7:528], in0=S2[:, 2:6, 7:528], in1=S2[:, 2:6, 3:524], op=ALU.add),
                 [e_s1])
            e_s4 = P.op("pool", lambda e: e.tensor_tensor(out=S4[:, :, 15:528], in0=S3[:, 2:4, 15:528], in1=S3[:, 2:4, 7:520],
                                                          op=ALU.add), [], sig=True)
            plb = pl[Tq % 2]
            srcs = [(S1, 0, 0.5), (S2, 0, 0.25), (S3, 0, 0.125), (S4, 0, 0.0625)]
            e_p = None
            for g in range(4):
                Sx, c0, sc_ = srcs[g]
                e_p = P.op("dve", lambda e, g=g, Sx=Sx, sc_=sc_: e.scalar_tensor_tensor(
                    out=plb[:, 2 * g:2 * g + 2, :], in0=Sx[:, 0:2, 16:528], scalar=sc_, in1=H[:, 2 * g:2 * g + 2, 16:528],
                    op0=ALU.mult, op1=ALU.subtract), [e_s4, pl_rd[Tq % 2]], sig=True)
                if Tq == 0:
                    P.op("dve", lambda e, g=g, Sx=Sx: e.tensor_tensor(
                        out=fx[:], in0=Sx[:, 0:2, 16:32], in1=invfix[:, g, :].unsqueeze(1).to_broadcast([128, 2, 16]),
                        op=ALU.mult), [e_if])
                    e_p = P.op("dve", lambda e, g=g: e.tensor_tensor(out=plb[:, 2 * g:2 * g + 2, 0:16], in0=fx[:],
                                                                    in1=H[:, 2 * g:2 * g + 2, 16:32], op=ALU.subtract),
                               [], sig=True)
            if Tq + 1 < 8:
                Hn = hT[(Tq + 1) % 2]
                halo_ev[0] = P.op("pool", lambda e: e.tensor_copy(out=Hn[:, :, 0:16], in_=H[:, :, 512:528]),
                                  evs_h + hT_rd[(Tq + 1) % 2], sig=True)
                hT_rd[Tq % 2] = [e_p, halo_ev[0]]
            else:
                hT_rd[Tq % 2] = [e_p]
            def back_sub(sub):
                st = Tq * 4 + sub
                yp = y_ps[st % 2]
                ev = None
                for g in range(4):
                    for cc in range(2):
                        ev = P.op("pe", lambda e, g=g, cc=cc: e.matmul(yp[:, g * 256:(g + 1) * 256],
                                                                      lhsT=plb[:, 2 * g + cc, sub * 128:(sub + 1) * 128],
                                                                      rhs=wp[:, g, cc, :], start=(cc == 0), stop=(cc == 1)),
                                  [e_p, e_wp, y_free[st % 2]] if (g == 0 and cc == 0) else [],
                                  sig=(True if (g == 3 and cc == 1) else None))
                if sub == 3:
                    pl_rd[Tq % 2] = ev
                tb = t1[st % 2]
                e_t = P.op("dve", lambda e: e.tensor_tensor(out=tb[:], in0=yp[:, :], in1=cg[:], op=ALU.mult),
                           [ev, e_cg, t1_free[st % 2]], sig=True)
                y_free[st % 2] = e_t
                xb = xt[st % 6]
                xo = x3[st % 2]
                e_a2 = P.op("pool", lambda e: e.tensor_tensor(out=xo[:], in0=tb[:], in1=xb[:], op=ALU.add),
                            [e_t, x_ev[st], x3_free[st % 2]], sig=True)
                t1_free[st % 2] = e_a2
                xt_free[st % 6] = e_a2
                x3_free[st % 2] = P.dma("sp", Xout[st * 128:(st + 1) * 128, :], xo[:], waits=[e_a2], sem=s_o[st % 2])

            for sub in range(4):
                back_sub(sub)

        for Tq in range(8):
            super_tile(Tq)


IN_SPECS = {
    "x": ([S, D], F32), "c_col": ([128, 8], F32), "ada_w": ([2, D, 6 * D], F32), "ada_b": ([2, 6 * D], F32),
    "gmix_col": ([128, 2, 8], F32), "gffn_col": ([128, 2, 8], F32), "w_in": ([D, 3 * D + NH], F32),
    "fb_col": ([NH, 1], F32), "w_o": ([D, D], F32), "pool_w": ([4, 256, 256], F32), "pscale_row": ([128, D], F32),
    "rw": ([128, 8, E], F32), "rbias_row": ([128, E], F32),
    "w1_0": ([E, D, FF], F32), "w1_1": ([E, D, FF], F32), "w3_0": ([E, D, FF], F32), "w3_1": ([E, D, FF], F32),
    "w2_0": ([E, FF, D], F32), "w2_1": ([E, FF, D], F32), "gfin_row": ([128, D], F32),
    "ident": ([128, 128], F32), "identf": ([128, 128], F32), "mtri": ([128, 128], F32), "ltri": ([128, 128], F32),
    "ones": ([128, 128], F32), "thr": ([128, 16], F32), "biota": ([128, 64], F32), "invfix": ([128, 4, 16], F32),
    "piota": ([128, 1], F32), "gffn_pk": ([128, 2, 8], F32),
}
SCRATCH = {
    "mod_d": ([2, 6 * D], F32), "QT_d": ([D, S], BF16), "KT_d": ([D, S], BF16), "V_d": ([NH, 128, NT, 64], BF16),
    "FL_d": ([NH, S], F32), "QF_d": ([NH, 3, S], BF16), "KF_d": ([NH, 3, S], BF16),
    "X1_d": ([S, D], F32), "X2_d": ([S, D], F32), "X3_d": ([S, D], F32),
    "XG_d": ([NBLK * BLK, D], BF16), "YG_d": ([NBLK * BLK, D], F32),
}


def build(upto=99, debug=(), only_inputs=None):
    nc = bass.Bass("TRN2", target_bir_lowering=False)
    T = {}
    for k, (shp, dt) in IN_SPECS.items():
        if only_inputs is not None and k not in only_inputs:
            continue
        T[k] = nc.dram_tensor(k, shp, dt, kind="ExternalInput")
    for k, (shp, dt) in SCRATCH.items():
        if k in DBG.get("as_input", ()):
            continue
        T[k] = nc.dram_tensor(k, shp, dt, kind=("ExternalOutput" if k in debug else "Internal"))
    T["out"] = nc.dram_tensor("out", [S, D], F32, kind="ExternalOutput")
    if "Ri_dbg" in debug:
        T["Ri_dbg"] = nc.dram_tensor("Ri_dbg", [2, 128, 32], I32, kind="ExternalOutput")
        T["Rf_dbg"] = nc.dram_tensor("Rf_dbg", [3, 128, 64], F32, kind="ExternalOutput")
    for k in DBG.get("as_input", ()):
        shp, dt = SCRATCH[k]
        T[k] = nc.dram_tensor(k, shp, dt, kind="ExternalInput")
    with ExitStack() as es:
        ph_mod(nc, T)
        if upto < 1:
            return nc
        if not DBG.get("skip_l0mix"):
            ph_qkv(nc, T)
            if not DBG.get("no_fgate"):
                ph_fgate(nc, T)
            if upto < 2:
                return nc
            with nc.sbuf_tensor("OT_all", [128, 8, S], BF16) as OT_all:
                ph_attn(nc, T, OT_all)
                ph_oproj(nc, T, OT_all)
        if upto < 3:
            return nc
        R = {
            "destA": es.enter_context(nc.sbuf_tensor("R_destA", [128, 32], I32)),
            "destB": es.enter_context(nc.sbuf_tensor("R_destB", [128, 32], I32)),
            "gA": es.enter_context(nc.sbuf_tensor("R_gA", [128, 32], F32)),
            "gB": es.enter_context(nc.sbuf_tensor("R_gB", [128, 32], F32)),
            "blk_e": es.enter_context(nc.sbuf_tensor("R_blk_e", [128, 64], F32)),
            "idxW": es.enter_context(nc.sbuf_tensor("R_idxW", [128, 64], I32)),
        }
        if "X2_d" not in DBG.get("as_input", ()) and "X3_d" not in DBG.get("as_input", ()):
            ph_front(nc, T, 0, R, T["X1_d"])
            if upto < 4:
                return nc
            ph_moe(nc, T, 0, R)
            ph_comb(nc, T, 0, R, T["X1_d"], T["X2_d"], final=False)
        if upto < 5:
            return nc
        if "X3_d" not in DBG.get("as_input", ()):
            ph_pool(nc, T, T["X2_d"], T["X3_d"])
        if upto < 6:
            return nc
        ph_front(nc, T, 1, R, T["X3_d"])
        ph_moe(nc, T, 1, R)
        ph_comb(nc, T, 1, R, T["X3_d"], T["out"], final=True)
    return nc


def _col(v):
    return np.ascontiguousarray(np.asarray(v, np.float32).reshape(8, 128).T)


def make_in_maps(inp):
    f = lambda a: np.ascontiguousarray(np.asarray(a, np.float32))
    shared = {
        "ada_w": f(inp["ada_w"]), "ada_b": f(inp["ada_b"]),
        "gmix_col": np.ascontiguousarray(np.stack([_col(inp["norm_mix_g"][l]) for l in range(2)], axis=1)),
        "gffn_col": np.ascontiguousarray(np.stack([_col(inp["norm_ffn_g"][l]) for l in range(2)], axis=1)),
        "w_in": f(inp["attn_w_in"][0]), "fb_col": f(inp["attn_f_bias"][0]).reshape(NH, 1), "w_o": f(inp["attn_w_o"][0]),
        "pool_w": f(inp["pool_w"][0]), "pscale_row": np.ascontiguousarray(np.broadcast_to(f(inp["pool_scale"][0]), (128, D))),
        "rw": np.ascontiguousarray(f(inp["router_w"]).reshape(8, 128, E).transpose(1, 0, 2)),
        "rbias_row": np.ascontiguousarray(np.broadcast_to(f(inp["router_bias"]), (128, E))),
        "w1_0": f(inp["exp_w1"][0]), "w1_1": f(inp["exp_w1"][1]), "w3_0": f(inp["exp_w3"][0]), "w3_1": f(inp["exp_w3"][1]),
        "w2_0": f(inp["exp_w2"][0]), "w2_1": f(inp["exp_w2"][1]),
        "gfin_row": np.ascontiguousarray(np.broadcast_to(f(inp["norm_final_g"]), (128, D))),
    }
    ii = np.arange(128)
    shared["ident"] = np.eye(128, dtype=np.float32)
    shared["identf"] = np.eye(128, dtype=np.float32)
    shared["mtri"] = np.where(ii[None, :] >= ii[:, None], 0.0, MASKV).astype(np.float32)
    shared["ltri"] = (ii[:, None] < ii[None, :]).astype(np.float32)
    shared["ones"] = np.ones((128, 128), np.float32)
    shared["thr"] = np.ascontiguousarray(np.broadcast_to((np.arange(16) * BLK).astype(np.float32), (128, 16)))
    shared["biota"] = np.ascontiguousarray(np.broadcast_to(np.arange(64).astype(np.float32), (128, 64)))
    fix = np.zeros((4, 16), np.float32)
    for g, w in enumerate((2, 4, 8, 16)):
        fix[g] = 1.0 / np.minimum(np.arange(16) + 1, w)
    shared["invfix"] = np.ascontiguousarray(np.broadcast_to(fix, (128, 4, 16)))
    shared["piota"] = np.arange(128, dtype=np.float32).reshape(128, 1)
    shared["gffn_pk"] = np.ascontiguousarray(np.stack([f(inp["norm_ffn_g"][l]).reshape(128, 8) for l in range(2)], axis=1))
    maps = []
    for b in range(8):
        m = dict(shared)
        m["x"] = f(inp["x"][b])
        m["c_col"] = _col(inp["c"][b])
        maps.append(m)
    return maps


_NC_CACHE = {}


def kernel(**inputs):
    if "nc" not in _NC_CACHE:
        _NC_CACHE["nc"] = build()
    nc = _NC_CACHE["nc"]
    in_maps = make_in_maps(inputs)
    res = run_bass_kernel_spmd(nc, in_maps, core_ids=list(range(8)))
    return np.stack([np.asarray(r["out"], np.float32) for r in res.results], axis=0)
```
